# Optimizing a Trainium2 kernel written in Bass

```python
import jax, jax.numpy as jnp
from jax import lax
import numpy as np

D_MODEL = 1024
BATCH = 8
SEQ = 2048
DEPTH = 1

HEAD_DIM = 64
SCALE = HEAD_DIM ** -0.5
EPS = 1e-6
NEG = -1e30
DIL_PAIRS = ((128, 1), (512, 4), (2048, 16))
N_DIL_GROUPS = len(DIL_PAIRS)
HEADS_A = 8
WIDTH_A = N_DIL_GROUPS * HEADS_A * HEAD_DIM
OUT_A = HEADS_A * HEAD_DIM
ROT_DIM_A = HEAD_DIM // 4
THETA_PARTIAL = 500000.0
HEADS_B_Q = 8
HEADS_B_KV = 2
WIDTH_B_Q = HEADS_B_Q * HEAD_DIM
WIDTH_B_KV = HEADS_B_KV * HEAD_DIM
OUT_B = WIDTH_B_Q
Q_BLOCK = 128
GRID_W = 64
THETA_AXIAL = 10000.0
N_BRANCHES = 2
IN_COLS = 3 * WIDTH_A + WIDTH_B_Q + 2 * WIDTH_B_KV + N_BRANCHES * D_MODEL
N_EXPERTS = 32
TOP_K = 4
D_FF = 1024
SWIGLU_LIMIT = 7.0
SWIGLU_ALPHA = 1.702
EXPERT_BLOCK = 128

kernel_name = "hybrid_dilated_axial_gqa_moe_block"


def rmsnorm(x, g):
    xf = x.astype(jnp.float32)
    y = xf * lax.rsqrt(jnp.mean(xf * xf, axis=-1, keepdims=True) + EPS)
    return (y * g).astype(x.dtype)


def rope(x, pos, theta):
    half = x.shape[-1] // 2
    inv = theta ** (-(jnp.arange(half, dtype=jnp.float32) / half))
    ang = pos.astype(jnp.float32)[:, None] * inv[None, :]
    cos, sin = jnp.cos(ang)[:, None, :], jnp.sin(ang)[:, None, :]
    xf = x.astype(jnp.float32)
    x1, x2 = xf[..., :half], xf[..., half:]
    return jnp.concatenate([x1 * cos - x2 * sin, x2 * cos + x1 * sin], axis=-1).astype(x.dtype)


def partial_rope(x, pos):
    return jnp.concatenate([rope(x[..., :ROT_DIM_A], pos, THETA_PARTIAL), x[..., ROT_DIM_A:]], axis=-1)


def axial_rope(x, row, col):
    h = x.shape[-1] // 2
    return jnp.concatenate([rope(x[..., :h], row, THETA_AXIAL), rope(x[..., h:], col, THETA_AXIAL)], axis=-1)


def _pad_axis3(x, lo, hi):
    return jnp.pad(x, ((0, 0), (0, 0), (0, 0), (lo, hi), (0, 0)))


def dilated_window_attention(q, k, v, window, dil):
    b, s, h, e = q.shape
    half = window // (2 * dil)
    L = s // dil
    nb = -(-L // half)
    lp = nb * half
    pad = lp - L

    def to_sub(t):
        return t.reshape(b, L, dil, h, e).transpose(0, 2, 3, 1, 4)

    qs, ks, vs = to_sub(q), to_sub(k), to_sub(v)
    qb = _pad_axis3(qs, 0, pad).reshape(b, dil, h, nb, half, e)

    def windows(t):
        tp = _pad_axis3(t, half, pad + half).reshape(b, dil, h, nb + 2, half, e)
        return jnp.concatenate([tp[:, :, :, :-2], tp[:, :, :, 1:-1], tp[:, :, :, 2:]], axis=4)

    kw, vw = windows(ks), windows(vs)
    sc = jnp.einsum('bdhnqe,bdhnke->bdhnqk', qb, kw).astype(jnp.float32) * SCALE
    blk = jnp.arange(nb)[:, None, None]
    qi = blk * half + jnp.arange(half)[None, :, None]
    ki = (blk - 1) * half + jnp.arange(3 * half)[None, None, :]
    valid = (jnp.abs(ki - qi) <= half) & (ki >= 0) & (ki < L)
    sc = jnp.where(valid, sc, NEG)
    lse = jax.nn.logsumexp(sc, axis=-1)
    p = jnp.exp(sc - lse[..., None])
    o = jnp.einsum('bdhnqk,bdhnke->bdhnqe', p.astype(v.dtype), vw)
    o = o.reshape(b, dil, h, lp, e)[:, :, :, :L].transpose(0, 3, 1, 2, 4).reshape(b, s, h, e)
    lse = lse.reshape(b, dil, h, lp)[..., :L].transpose(0, 3, 1, 2).reshape(b, s, h)
    return o, lse


def gqa_blocked_attention(q, k, v):
    b, s, hq, e = q.shape
    hkv = k.shape[2]
    g = hq // hkv
    nq = s // Q_BLOCK
    qb = q.reshape(b, nq, Q_BLOCK, hkv, g, e).transpose(1, 0, 3, 4, 2, 5)
    kt = k.transpose(0, 2, 1, 3)
    vt = v.transpose(0, 2, 1, 3)

    def one_block(qblk):
        sc = jnp.einsum('bkgqe,bkse->bkgqs', qblk, kt).astype(jnp.float32) * SCALE
        p = jax.nn.softmax(sc, axis=-1)
        return jnp.einsum('bkgqs,bkse->bkgqe', p.astype(vt.dtype), vt)

    ob = lax.map(one_block, qb)
    return ob.transpose(1, 0, 4, 2, 3, 5).reshape(b, s, hq * e)


def moe_ffn(h, w_router, b_router, w_gate_up, b_gate_up, w_down, b_down):
    t, d = h.shape
    logits = (h @ w_router).astype(jnp.float32) + b_router
    topv, idx = lax.top_k(logits, TOP_K)
    gates = jax.nn.softmax(topv, axis=-1)
    a = t * TOP_K
    flat_e = idx.reshape(a)
    flat_w = gates.reshape(a)
    flat_tok = jnp.arange(a) // TOP_K
    order = jnp.argsort(flat_e)
    e_s, tok_s, w_s = flat_e[order], flat_tok[order], flat_w[order]
    counts = jnp.bincount(flat_e, length=N_EXPERTS)
    starts = jnp.cumsum(counts) - counts
    padded = ((counts + EXPERT_BLOCK - 1) // EXPERT_BLOCK) * EXPERT_BLOCK
    pends = jnp.cumsum(padded)
    pstarts = pends - padded
    dest = pstarts[e_s] + (jnp.arange(a) - starts[e_s])
    p_rows = a + N_EXPERTS * EXPERT_BLOCK
    n_blk = p_rows // EXPERT_BLOCK
    block_e = jnp.clip(jnp.searchsorted(pends, jnp.arange(n_blk) * EXPERT_BLOCK, side='right'), 0, N_EXPERTS - 1)
    xbuf = jnp.zeros((p_rows, d), h.dtype).at[dest].set(h[tok_s])
    wbuf = jnp.zeros((p_rows,), jnp.float32).at[dest].set(w_s)
    tbuf = jnp.zeros((p_rows,), jnp.int32).at[dest].set(tok_s.astype(jnp.int32))

    def expert_block(args):
        xblk, e = args
        gu = xblk @ w_gate_up[e] + b_gate_up[e]
        gate = jnp.minimum(gu[:, :D_FF], SWIGLU_LIMIT)
        up = jnp.clip(gu[:, D_FF:], -SWIGLU_LIMIT, SWIGLU_LIMIT)
        act = (up + 1.0) * (gate * jax.nn.sigmoid(SWIGLU_ALPHA * gate))
        return act @ w_down[e] + b_down[e]

    ybuf = lax.map(expert_block, (xbuf.reshape(n_blk, EXPERT_BLOCK, d), block_e)).reshape(p_rows, d)
    out = jnp.zeros((t, d), jnp.float32).at[tbuf].add(ybuf.astype(jnp.float32) * wbuf[:, None])
    return out


def setup_inputs(seed: int = 0) -> dict:
    key = jax.random.key(seed)
    ks = jax.random.split(key, 18)
    f32 = jnp.float32
    nrm = lambda k, shp: jax.random.normal(k, shp, f32)
    return {
        "x": nrm(ks[0], (BATCH, SEQ, D_MODEL)),
        "norm_mix_g": 1.0 + 0.02 * nrm(ks[1], (D_MODEL,)),
        "w_in": nrm(ks[2], (D_MODEL, IN_COLS)) * D_MODEL ** -0.5,
        "b_gate": 0.1 * nrm(ks[3], (N_BRANCHES, D_MODEL)),
        "qn_a": 1.0 + 0.02 * nrm(ks[4], (N_DIL_GROUPS, HEAD_DIM)),
        "kn_a": 1.0 + 0.02 * nrm(ks[5], (N_DIL_GROUPS, HEAD_DIM)),
        "qn_b": 1.0 + 0.02 * nrm(ks[6], (HEAD_DIM,)),
        "kn_b": 1.0 + 0.02 * nrm(ks[7], (HEAD_DIM,)),
        "w_proj_a": nrm(ks[8], (OUT_A, D_MODEL)) * OUT_A ** -0.5,
        "w_proj_b": nrm(ks[9], (OUT_B, D_MODEL)) * OUT_B ** -0.5,
        "w_out": nrm(ks[10], (D_MODEL, D_MODEL)) * D_MODEL ** -0.5,
        "norm_ffn_g": 1.0 + 0.02 * nrm(ks[11], (D_MODEL,)),
        "w_router": nrm(ks[12], (D_MODEL, N_EXPERTS)) * D_MODEL ** -0.5,
        "b_router": 0.01 * nrm(ks[13], (N_EXPERTS,)),
        "w_gate_up": nrm(ks[14], (N_EXPERTS, D_MODEL, 2 * D_FF)) * D_MODEL ** -0.5,
        "b_gate_up": 0.02 * nrm(ks[15], (N_EXPERTS, 2 * D_FF)),
        "w_down": nrm(ks[16], (N_EXPERTS, D_FF, D_MODEL)) * D_FF ** -0.5,
        "b_down": 0.02 * nrm(ks[17], (N_EXPERTS, D_MODEL)),
    }


def reference(x, norm_mix_g, w_in, b_gate, qn_a, kn_a, qn_b, kn_b, w_proj_a, w_proj_b, w_out,
              norm_ffn_g, w_router, b_router, w_gate_up, b_gate_up, w_down, b_down):
    b, s, d = x.shape
    pos = jnp.arange(s)
    rows = s // GRID_W
    row = jnp.repeat(jnp.arange(rows), GRID_W)
    col = jnp.tile(jnp.arange(GRID_W), rows)
    split_at = list(np.cumsum([3 * WIDTH_A, WIDTH_B_Q, WIDTH_B_KV, WIDTH_B_KV]))

    for _layer in range(DEPTH):
        h = rmsnorm(x, norm_mix_g)
        proj = h @ w_in
        qkv_a, q_b, k_b, v_b, gate_logits = jnp.split(proj, split_at, axis=-1)

        qkv_a = qkv_a.reshape(b, s, 3, N_DIL_GROUPS, HEADS_A, HEAD_DIM)
        outs, lses = [], []
        for gi, (window, dil) in enumerate(DIL_PAIRS):
            qg = partial_rope(rmsnorm(qkv_a[:, :, 0, gi], qn_a[gi]), pos)
            kg = partial_rope(rmsnorm(qkv_a[:, :, 1, gi], kn_a[gi]), pos)
            og, lg = dilated_window_attention(qg, kg, qkv_a[:, :, 2, gi], window, dil)
            outs.append(og)
            lses.append(lg)
        wts = jax.nn.softmax(jnp.stack(lses, axis=0), axis=0)
        o_a = jnp.sum(wts[..., None] * jnp.stack(outs, axis=0).astype(jnp.float32), axis=0)
        o_a = o_a.astype(x.dtype).reshape(b, s, OUT_A)

        qb = axial_rope(rmsnorm(q_b.reshape(b, s, HEADS_B_Q, HEAD_DIM), qn_b), row, col)
        kb = axial_rope(rmsnorm(k_b.reshape(b, s, HEADS_B_KV, HEAD_DIM), kn_b), row, col)
        vb = v_b.reshape(b, s, HEADS_B_KV, HEAD_DIM)
        o_b = gqa_blocked_attention(qb, kb, vb)

        gates = jax.nn.sigmoid(gate_logits.reshape(b, s, N_BRANCHES, d) + b_gate)
        merged = gates[:, :, 0] * (o_a @ w_proj_a) + gates[:, :, 1] * (o_b @ w_proj_b)
        x = x + (merged @ w_out).astype(x.dtype)

        h2 = rmsnorm(x, norm_ffn_g).reshape(b * s, d)
        ff = moe_ffn(h2, w_router, b_router, w_gate_up, b_gate_up, w_down, b_down)
        x = x + ff.reshape(b, s, d).astype(x.dtype)
    return x
```

```python
import contextlib
import os
import numpy as np
import concourse.bass as bass
import concourse.mybir as mybir
from concourse.bass_utils import run_bass_kernel_spmd

F32 = mybir.dt.float32
BF16 = mybir.dt.bfloat16
ALU = mybir.AluOpType
AF = mybir.ActivationFunctionType
AX = mybir.AxisListType

S = 2048
D = 1024
NT = 16
DIL = (1, 4, 16)
NE = 32
EPS = 1e-6
STAGE = int(os.environ.get("KSTAGE", "99"))
NEXP = int(os.environ.get("KNEXP", "32"))
SPARSE = int(os.environ.get("KSPARSE", "1"))
BLK = 384
NBLK = (4 * 2048 + 32 * (BLK - 1)) // BLK
NQ = BLK // 128

ENGS = ("pe", "act", "dve", "pool", "sp")
N_DMA_SEMS = 28
N_DMA_SEMS_HW = 16


class Op:
    __slots__ = ("idx", "eng", "fn", "deps", "dma", "sem", "val", "signal")


class Prog:
    def __init__(self, nc):
        self.nc = nc
        self.ops = []
        self.last_w = {}
        self.readers = {}
        self.dma_rr = 0
        self.dma_rr_sw = 0
        self.dma_last = [None] * N_DMA_SEMS
        self.dma_cnt = [0] * N_DMA_SEMS
        self.barrier_deps = set()
        self.last_on_eng = {}

    def op(self, eng, fn, reads=(), writes=(), dma=False, accum=False):
        o = Op()
        o.idx = len(self.ops)
        o.eng = eng
        o.fn = fn
        o.dma = dma
        o.signal = False
        o.sem = None
        o.val = None
        deps = set(self.barrier_deps)
        for k in reads:
            w = self.last_w.get(k)
            if w is not None:
                deps.add(w)
        for k in writes:
            w = self.last_w.get(k)
            if w is not None:
                if not (accum and self.ops[w].eng == "pe" and eng == "pe"):
                    deps.add(w)
            for r in self.readers.get(k, ()):
                deps.add(r)
        if dma:
            if eng == "sp":
                s = self.dma_rr % N_DMA_SEMS_HW
                self.dma_rr += 1
            else:
                s = N_DMA_SEMS_HW + self.dma_rr_sw % (N_DMA_SEMS - N_DMA_SEMS_HW)
                self.dma_rr_sw += 1
            prev = self.dma_last[s]
            if prev is not None:
                deps.add(prev)
            self.dma_last[s] = o.idx
            self.dma_cnt[s] += 1
            o.sem = ("dma", s)
            o.val = 16 * self.dma_cnt[s]
            o.signal = True
        o.deps = deps
        for k in reads:
            self.readers.setdefault(k, []).append(o.idx)
        for k in writes:
            self.last_w[k] = o.idx
            self.readers[k] = []
        self.ops.append(o)
        self.last_on_eng[("dma", o.sem) if dma else eng] = o.idx
        return o

    def barrier(self):
        self.barrier_deps = set(self.last_on_eng.values())

    def emit(self, final_ops):
        nc = self.nc
        ops = self.ops
        for o in ops:
            for d in o.deps:
                ops[d].signal = True
        cnt = {e: 0 for e in ENGS}
        for o in ops:
            if o.dma:
                continue
            if o.signal:
                cnt[o.eng] += 1
                o.sem = ("eng", o.eng)
                o.val = cnt[o.eng]
        with contextlib.ExitStack() as st:
            sems = {}
            for e in ENGS:
                sems[("eng", e)] = st.enter_context(nc.semaphore("s_" + e))
            for i in range(N_DMA_SEMS):
                sems[("dma", i)] = st.enter_context(nc.semaphore("s_dma%d" % i))
            block = st.enter_context(nc.Block())
            by_eng = {e: [o for o in ops if o.eng == e] for e in ENGS}

            def run(eng_name, engine):
                seen = {}
                for o in by_eng[eng_name]:
                    need = {}
                    for d in o.deps:
                        od = ops[d]
                        if seen.get(od.sem, 0) >= od.val:
                            continue
                        if need.get(od.sem, 0) < od.val:
                            need[od.sem] = od.val
                    for s, v in need.items():
                        engine.wait_ge(sems[s], v)
                        seen[s] = v
                    ins = o.fn(engine)
                    if o.signal:
                        ins.then_inc(sems[o.sem], 16 if o.dma else 1)
                if eng_name == "sp":
                    for fo in final_ops:
                        engine.wait_ge(sems[fo.sem], fo.val)

            @block.tensor
            def _(e):
                run("pe", e)

            @block.scalar
            def _(e):
                run("act", e)

            @block.vector
            def _(e):
                run("dve", e)

            @block.gpsimd
            def _(e):
                run("pool", e)

            @block.sync
            def _(e):
                run("sp", e)


def _consts():
    c = {}
    c["ident"] = np.eye(128, dtype=np.float32)
    bo = np.zeros((128, 128), np.float32)
    bo[:64, :64] = 1.0
    bo[64:, 64:] = 1.0
    c["blockones"] = bo
    pos = np.arange(S, dtype=np.float32)
    inv = (np.float32(500000.0) ** (-(np.arange(8, dtype=np.float32) / np.float32(8)))).astype(np.float32)
    ang = (pos[:, None] * inv[None, :]).astype(np.float32)
    cosA = np.ones((128, S), np.float32)
    sinA = np.zeros((128, S), np.float32)
    RA = np.zeros((128, 128), np.float32)
    for hh in range(2):
        for dd in range(16):
            p = hh * 64 + dd
            cosA[p] = np.cos(ang[:, dd % 8])
            sinA[p] = np.sin(ang[:, dd % 8])
            if dd < 8:
                RA[p + 8, p] = -1.0
            else:
                RA[p - 8, p] = 1.0
    c["cosA"], c["sinA"], c["RA"] = cosA, sinA, RA
    row = (np.arange(S) // 64).astype(np.float32)
    col = (np.arange(S) % 64).astype(np.float32)
    invb = (np.float32(10000.0) ** (-(np.arange(16, dtype=np.float32) / np.float32(16)))).astype(np.float32)
    cosB = np.ones((128, S), np.float32)
    sinB = np.zeros((128, S), np.float32)
    RB = np.zeros((128, 128), np.float32)
    for hh in range(2):
        for dd in range(64):
            p = hh * 64 + dd
            ps_ = row if dd < 32 else col
            a = (ps_ * invb[dd % 16]).astype(np.float32)
            cosB[p] = np.cos(a)
            sinB[p] = np.sin(a)
            if (dd % 32) < 16:
                RB[p + 16, p] = -1.0
            else:
                RB[p - 16, p] = 1.0
    c["cosB"], c["sinB"], c["RB"] = cosB, sinB, RB
    a = np.arange(128)[:, None]
    b = np.arange(128)[None, :]
    m = np.zeros((128, 3, 128), np.float32)
    m[:, 0, :] = (a - b >= 64)
    m[:, 1, :] = (np.abs(a - b) <= 64)
    m[:, 2, :] = (b - a >= 64)
    c["utri"] = np.triu(np.ones((128, 128), np.float32), 1)
    c["blkthr"] = np.tile((np.arange(NBLK, dtype=np.float32) * BLK)[None, :], (128, 1))
    c["basekc"] = (np.arange(8)[None, :] * 128 + np.arange(128)[:, None]).astype(np.float32)
    c["maskA"] = ((1.0 - m) * -30000.0).astype(np.float32).reshape(128, 384)
    return c


def _relayout(inp):
    r = {}
    w_in = inp["w_in"]
    r["w_in"] = w_in
    r["gmix"] = np.ascontiguousarray(inp["norm_mix_g"].reshape(8, 128).T)
    r["gffn"] = np.ascontiguousarray(inp["norm_ffn_g"].reshape(8, 128).T)
    gains = np.zeros((128, 30), np.float32)
    for g in range(3):
        for hp in range(4):
            gains[:, g * 4 + hp] = np.tile(inp["qn_a"][g], 2)
            gains[:, 12 + g * 4 + hp] = np.tile(inp["kn_a"][g], 2)
    for c in range(4):
        gains[:, 24 + c] = np.tile(inp["qn_b"], 2)
    for g in range(2):
        gains[:, 28 + g] = np.tile(inp["kn_b"], 2)
    r["gains"] = gains
    g0c = 3 * 1536 + 512 + 256
    bun = np.empty((8, 128, 24, 128), np.float32)
    for m in range(8):
        cs = slice(m * 128, (m + 1) * 128)
        bun[m, :, 0:4] = inp["w_proj_a"][:, cs].reshape(4, 128, 128).transpose(1, 0, 2)
        bun[m, :, 4:8] = inp["w_proj_b"][:, cs].reshape(4, 128, 128).transpose(1, 0, 2)
        bun[m, :, 8:16] = w_in[:, g0c + m * 128: g0c + (m + 1) * 128].reshape(8, 128, 128).transpose(1, 0, 2)
        bun[m, :, 16:24] = w_in[:, g0c + 1024 + m * 128: g0c + 1024 + (m + 1) * 128].reshape(8, 128, 128).transpose(1, 0, 2)
    r["mbundle"] = bun
    r["bgate"] = np.ascontiguousarray(inp["b_gate"].reshape(2, 8, 128).transpose(2, 0, 1).reshape(128, 16))
    r["w_out"] = inp["w_out"]
    r["w_router"] = inp["w_router"]
    r["b_router"] = inp["b_router"].reshape(1, 32)
    r["w_gate_up"] = inp["w_gate_up"]
    r["w_down"] = inp["w_down"]
    r["bgu"] = np.ascontiguousarray(inp["b_gate_up"].reshape(32, 16, 128).transpose(0, 2, 1).reshape(4096, 16))
    r["b_down"] = inp["b_down"]
    return r


def build(dbg=None):
    nc = bass.Bass("TRN2", target_bir_lowering=False)

    def din(name, shape):
        return nc.dram_tensor(name, list(shape), F32, kind="ExternalInput").ap()

    x_d = din("x", [S, D])
    w_in_d = din("w_in", [D, 7424])
    gmix_d = din("gmix", [128, 8])
    gffn_d = din("gffn", [128, 8])
    gains_d = din("gains", [128, 30])
    mb_d = din("mbundle", [8, 128, 24, 128])
    bgate_d = din("bgate", [128, 16])
    w_out_d = din("w_out", [D, D])
    w_router_d = din("w_router", [D, 32])
    b_router_d = din("b_router", [1, 32])
    if STAGE > 4:
        wgu_d = din("w_gate_up", [NE, D, 2048])
        wd_d = din("w_down", [NE, D, D])
    bgu_d = din("bgu", [4096, 16])
    bd_d = din("b_down", [NE, D])
    cd = {k: din("c_" + k, v.shape) for k, v in _consts().items()}
    out_d = nc.dram_tensor("out", [S, D], F32, kind="ExternalOutput").ap()
    dbg_d = {}
    if dbg:
        for k, shp in dbg.items():
            dbg_d[k] = nc.dram_tensor("dbg_" + k, list(shp), F32, kind="ExternalOutput").ap()

    w_in_v = w_in_d.rearrange("(kc p) n -> p kc n", p=128)

    P = Prog(nc)
    final_ops = []
    with contextlib.ExitStack() as top:
        def sbt(stk, name, shape, dt):
            return stk.enter_context(nc.sbuf_tensor("sb_" + name, list(shape), dt))

        x1 = sbt(top, "x1", [128, NT, D], F32)
        hT = sbt(top, "hT", [128, 8, S], BF16)
        obuf = sbt(top, "obuf", [128, 8, S], BF16)
        ident = sbt(top, "ident", [128, 128], F32)
        blockones = sbt(top, "blockones", [128, 128], F32)
        ones_bf = sbt(top, "ones_bf", [128, 128], BF16)
        gmix = sbt(top, "gmix", [128, 8], F32)
        gffn = sbt(top, "gffn", [128, 8], F32)
        gains = sbt(top, "gains", [128, 30], F32)
        bgate = sbt(top, "bgate", [128, 16], F32)
        epst = sbt(top, "epst", [128, 1], F32)
        ssq = sbt(top, "ssq", [128, NT], F32)
        rstd = sbt(top, "rstd", [128, NT], F32)
        psum = [top.enter_context(nc.psum_tensor("ps%d" % i, [128, 512], F32)) for i in range(8)]

        def PS(i):
            return ("ps", i)

        def dma(out, in_, reads=(), writes=(), eng="sp"):
            return P.op(eng, lambda e: e.dma_start(out=out, in_=in_), reads=reads, writes=writes, dma=True)

        def dbg_out(name, ap, key):
            if name in dbg_d:
                final_ops.append(dma(dbg_d[name], ap, reads=[key]))

        x1f = x1[:].rearrange("p a b -> p (a b)")

        def scr(off, n):
            return x1f[:, off:off + n]

        xs_d = nc.dram_tensor("xsorted", [NBLK * BLK, D], BF16).ap()
        y_d = nc.dram_tensor("ysorted", [NBLK * BLK, D], F32).ap()
        zero_ops = []
        dma(ident[:], cd["ident"][:, :], writes=["ident"])
        dma(blockones[:], cd["blockones"][:, :], writes=["blockones"])
        dma(gmix[:], gmix_d[:, :], writes=["gmix"])
        dma(gffn[:], gffn_d[:, :], writes=["gffn"])
        dma(gains[:], gains_d[:, :], writes=["gains"])
        dma(bgate[:], bgate_d[:, :], writes=["bgate"])
        P.op("pool", lambda e: e.memset(epst[:], EPS), writes=["eps"])
        P.op("pool", lambda e: e.memset(ones_bf[:], 1.0), writes=["ones_bf"])

        def norm_transpose(tt, src_ap, src_key, hn, hn_key, junk, junk_key, dstT, dst_key, psb, extra32=None, scale_ap=None):
            P.op("act", lambda e: e.activation(out=junk, in_=src_ap, func=AF.Square, accum_out=ssq[:, tt:tt + 1]),
                 reads=[src_key], writes=[junk_key, ("ssq", tt)])
            P.op("act", lambda e: e.activation(out=rstd[:, tt:tt + 1], in_=ssq[:, tt:tt + 1], func=AF.Sqrt,
                                               bias=epst[:, 0:1], scale=1.0 / D), reads=[("ssq", tt), "eps"], writes=[("rstd", tt)])
            P.op("dve", lambda e: e.reciprocal(out=rstd[:, tt:tt + 1], in_=rstd[:, tt:tt + 1]), reads=[("rstd", tt)], writes=[("rstd", tt)])
            P.op("dve", lambda e: e.tensor_scalar(out=hn, in0=src_ap, scalar1=rstd[:, tt:tt + 1], scalar2=None, op0=ALU.mult),
                 reads=[src_key, ("rstd", tt)], writes=[hn_key])
            for half in range(2):
                b = psb[half]
                for q in range(4):
                    kc = half * 4 + q
                    P.op("pe", lambda e, kc=kc, q=q, b=b: e.transpose(out=psum[b][:, q * 128:(q + 1) * 128], in_=hn[:, kc * 128:(kc + 1) * 128], identity=ident[:]),
                         reads=[hn_key, "ident"], writes=[PS(b)], accum=(q > 0))
                if dstT is not None and scale_ap is not None:
                    for q in range(4):
                        kc = half * 4 + q
                        P.op("act", lambda e, kc=kc, q=q, b=b: e.activation(out=dstT[:, kc, tt * 128:(tt + 1) * 128], in_=psum[b][:, q * 128:(q + 1) * 128],
                                                                            func=AF.Copy, scale=scale_ap[:, kc:kc + 1]),
                             reads=[PS(b), "gmix"], writes=[PS(b), (dst_key, tt)])
                elif dstT is not None:
                    P.op("act", lambda e, half=half, b=b: e.copy(out=dstT[:, half * 4:half * 4 + 4, tt * 128:(tt + 1) * 128],
                                                                  in_=psum[b][:].rearrange("p (a c) -> p a c", a=4)),
                         reads=[PS(b)], writes=[PS(b), (dst_key, tt)])
                if extra32 is not None:
                    e32, e32_key = extra32
                    P.op("act", lambda e, half=half, b=b: e.copy(out=e32[:, half * 4:half * 4 + 4, :], in_=psum[b][:].rearrange("p (a c) -> p a c", a=4)),
                         reads=[PS(b)], writes=[PS(b), e32_key])

        for tt in range(NT):
            xs = scr((tt % 2) * 1024, 1024)
            hn = scr(2048 + (tt % 2) * 1024, 1024)
            jk = scr(4096, 1024)
            dma(xs, x_d[tt * 128:(tt + 1) * 128, :], writes=[("xs", tt % 2)])
            norm_transpose(tt, xs, ("xs", tt % 2), hn, ("hn", tt % 2), jk, "junk", hT, "hT", (6, 7), scale_ap=gmix)
        hT_keys = [("hT", tt) for tt in range(NT)]
        if STAGE == 0:
            tmp = scr(8192, 2048)
            P.op("dve", lambda e: e.tensor_copy(out=tmp, in_=hT[:, 0, :]), reads=hT_keys, writes=["dbgtmp"])
            dbg_out("hT0", tmp, "dbgtmp")
        P.barrier()

        def load_w_piece(stage, stage_key, wbf, wbf_key, col_specs, fold):
            for (d0, s0, n) in col_specs:
                P.op("pool", lambda e, d0=d0, s0=s0, n=n: e.dma_start(out=wbf[:, :, d0:d0 + n], in_=w_in_v[:, :, s0:s0 + n]), writes=[wbf_key], dma=True)

        class RopeCtx:
            pass

        def qk_chunk(rc, wbf, wbf_key, gain_col, dst, dst_key, dil):
            def proj(tc):
                pb = rc.ps_proj[tc % 2]
                for kc in range(8):
                    P.op("pe", lambda e, kc=kc, pb=pb, tc=tc: e.matmul(psum[pb][:], lhsT=wbf[:, kc, :], rhs=hT[:, kc, tc * 512:(tc + 1) * 512],
                                                                        start=(kc == 0), stop=(kc == 7)),
                         reads=[wbf_key] + hT_keys[tc * 4:tc * 4 + 4], writes=[PS(pb)], accum=(kc > 0))
                sset = tc % 2
                u, sq = rc.u[sset], rc.sq[sset]
                ku, ksq = ("u", sset), ("sq", sset)
                P.op("act", lambda e, pb=pb, u=u: e.activation(out=u, in_=psum[pb][:], func=AF.Copy, scale=gains[:, gain_col:gain_col + 1]),
                     reads=[PS(pb), "gains"], writes=[PS(pb), ku])
                P.op("act", lambda e, pb=pb, sq=sq: e.activation(out=sq, in_=psum[pb][:], func=AF.Square), reads=[PS(pb)], writes=[PS(pb), ksq])

            def aux(tc):
                sset = tc % 2
                u, sq, rs, t1 = rc.u[sset], rc.sq[sset], rc.rs[sset], rc.t1[sset]
                ku, ksq, krs, kt1 = ("u", sset), ("sq", sset), ("rs", sset), ("t1", sset)
                pa = rc.ps_aux[0]
                pr = rc.ps_aux[1]
                P.op("pe", lambda e, pa=pa, sq=sq: e.matmul(psum[pa][:], lhsT=blockones[:], rhs=sq, start=True, stop=True),
                     reads=["blockones", ksq], writes=[PS(pa)])
                P.op("pe", lambda e, pr=pr, u=u: e.matmul(psum[pr][:], lhsT=rc.R[:], rhs=u, start=True, stop=True),
                     reads=[rc.R_key, ku], writes=[PS(pr)])
                P.op("act", lambda e, pa=pa, rs=rs: e.activation(out=rs, in_=psum[pa][:], func=AF.Sqrt, bias=epst[:, 0:1], scale=1.0 / 64),
                     reads=[PS(pa), "eps"], writes=[PS(pa), krs])
                P.op("dve", lambda e, rs=rs: e.reciprocal(out=rs, in_=rs), reads=[krs], writes=[krs])
                P.op("pool", lambda e, u=u, t1=t1, tc=tc: e.tensor_tensor(out=t1, in0=u, in1=rc.cos[:, tc * 512:(tc + 1) * 512], op=ALU.mult),
                     reads=[ku, rc.tab_key], writes=[kt1])
                P.op("dve", lambda e, pr=pr, sq=sq, tc=tc: e.tensor_tensor(out=sq, in0=psum[pr][:], in1=rc.sin[:, tc * 512:(tc + 1) * 512], op=ALU.mult),
                     reads=[PS(pr), rc.tab_key, ksq], writes=[PS(pr), ksq])
                P.op("dve", lambda e, t1=t1, sq=sq: e.tensor_tensor(out=t1, in0=t1, in1=sq, op=ALU.add), reads=[kt1, ksq], writes=[kt1])
                n = 512 // dil
                dv = dst.rearrange("p (r m) -> p r m", r=dil)[:, :, tc * n:(tc + 1) * n]
                P.op("dve", lambda e, t1=t1, rs=rs, dv=dv: e.tensor_tensor(out=dv, in0=t1.rearrange("p (m r) -> p r m", r=dil),
                                                                           in1=rs.rearrange("p (m r) -> p r m", r=dil), op=ALU.mult),
                     reads=[kt1, krs], writes=[dst_key])

            proj(0)
            for tc in range(4):
                if tc + 1 < 4:
                    proj(tc + 1)
                aux(tc)

        def v_tiles(wbf, wbf_key, vdst, v_key, dil, pbanks, evac=None):
            nb = (S // dil) // 128
            hv = hT[:].rearrange("p k (m r) -> p k r m", r=dil)
            for tg in range(4):
                pb = pbanks[tg % 2]
                for q in range(4):
                    ti = tg * 4 + q
                    r_, j = ti // nb, ti % nb
                    for kc in range(8):
                        P.op("pe", lambda e, kc=kc, q=q, r_=r_, j=j, pb=pb: e.matmul(psum[pb][:, q * 128:(q + 1) * 128], lhsT=hv[:, kc, r_, j * 128:(j + 1) * 128],
                                                                                     rhs=wbf[:, kc, :], start=(kc == 0), stop=(kc == 7)),
                             reads=[wbf_key] + hT_keys, writes=[PS(pb)], accum=not (q == 0 and kc == 0))
                if evac is not None:
                    evac(tg, pb)
                    continue
                P.op("act", lambda e, tg=tg, pb=pb: e.copy(out=vdst[:, tg * 4:tg * 4 + 4, :], in_=psum[pb][:].rearrange("p (a c) -> p a c", a=4)),
                     reads=[PS(pb)], writes=[PS(pb), v_key])

        o_aT = obuf[:, 0:4, :]
        o_bT = obuf[:, 4:8, :]

        with contextlib.ExitStack() as sa:
            rc = RopeCtx()
            rc.cos = scr(0, 2048)
            rc.sin = scr(2048, 2048)
            accn = scr(4096, 2048)
            accd = scr(6144, 2048)
            rc.u = [scr(8192 + i * 512, 512) for i in range(2)]
            rc.sq = [scr(9216 + i * 512, 512) for i in range(2)]
            rc.rs = [scr(10240 + i * 512, 512) for i in range(2)]
            rc.t1 = [scr(11264 + i * 512, 512) for i in range(2)]
            wst = [scr(12288 + i * 1024, 1024).rearrange("p (k c) -> p k c", k=8) for i in range(3)]
            rc.R = sbt(sa, "RA", [128, 128], F32)
            rc.R_key = "R"
            rc.tab_key = "tab"
            rc.ps_proj = (0, 1)
            rc.ps_aux = (2, 3)
            maskA = sbt(sa, "maskA", [128, 384], BF16)
            mstage = scr(15360, 384)
            wbf = [sbt(sa, "wbfA%d" % i, [128, 8, 128], BF16) for i in range(6)]
            qT = [sbt(sa, "qT%d" % i, [128, S], BF16) for i in range(2)]
            kT = [sbt(sa, "kT%d" % i, [128, S], BF16) for i in range(2)]
            vA = [[sbt(sa, "vA%d_%d" % (i, hh), [128, 16, 128], BF16) for hh in range(2)] for i in range(2)]
            pT = [sbt(sa, "pTA%d" % i, [128, 384], BF16) for i in range(4)]
            ident_bf = sbt(sa, "ident_bf", [128, 128], BF16)
            P.op("pool", lambda e: e.tensor_copy(out=ident_bf[:], in_=ident[:]), reads=["ident"], writes=["ident_bf"])
            for i in range(2):
                for hh in range(2):
                    P.op("pool", lambda e, i=i, hh=hh: e.memset(vA[i][hh][:], 1.0), writes=[("vA", i)])
            dma(rc.cos, cd["cosA"][:, :], writes=["tab"])
            dma(rc.sin, cd["sinA"][:, :], writes=["tab"])
            dma(rc.R[:], cd["RA"][:, :], writes=["R"])
            dma(mstage, cd["maskA"][:, :], writes=["mstage"])
            P.op("pool", lambda e: e.tensor_copy(out=maskA[:], in_=mstage), reads=["mstage"], writes=["maskA"])
            it = 0
            blk = 0
            itersA = [(hp, g) for hp in range(4) for g in range(3)]

            def loadsA(ix):
                hp_, g_ = itersA[ix]
                o3 = 3 * (ix % 2)
                for j, c0 in enumerate((g_ * 512 + hp_ * 128, 1536 + g_ * 512 + hp_ * 128, 3072 + g_ * 512 + hp_ * 128)):
                    load_w_piece(None, None, wbf[o3 + j][:], ("wbf", o3 + j), [(0, c0, 128)], None)
            loadsA(0)
            for hp in range(4):
                for g in range(3):
                    dil = DIL[g]
                    nb = (S // dil) // 128
                    buf = it % 2
                    if it + 1 < len(itersA):
                        loadsA(it + 1)
                    o3 = 3 * (it % 2)
                    it += 1
                    qk_chunk(rc, wbf[o3 + 0], ("wbf", o3 + 0), g * 4 + hp, qT[buf][:], ("qT", buf), dil)
                    qk_chunk(rc, wbf[o3 + 1], ("wbf", o3 + 1), 12 + g * 4 + hp, kT[buf][:], ("kT", buf), dil)
                    def evacA(tg, pb, buf=buf):
                        for hh in range(2):
                            P.op("act", lambda e, tg=tg, pb=pb, hh=hh, buf=buf: e.copy(out=vA[buf][hh][:, tg * 4:tg * 4 + 4, 64 * hh:64 * hh + 64],
                                                                                      in_=psum[pb][:].rearrange("p (a c) -> p a c", a=4)[:, :, 64 * hh:64 * hh + 64]),
                                 reads=[PS(pb)], writes=[PS(pb), ("vA", buf)])
                    v_tiles(wbf[o3 + 2], ("wbf", o3 + 2), None, ("vA", buf), dil, (0, 1), evac=evacA)
                    if STAGE == 1 and hp == 0:
                        tmp = scr(4096, 1024)
                        for nm, src_, key in (("qT_%d" % g, qT[buf], ("qT", buf)), ("kT_%d" % g, kT[buf], ("kT", buf))):
                            for hf in range(2):
                                P.op("dve", lambda e, src_=src_, hf=hf: e.tensor_copy(out=tmp, in_=src_[:, hf * 1024:(hf + 1) * 1024]), reads=[key], writes=["dbgtmp"])
                                if nm in dbg_d:
                                    final_ops.append(dma(dbg_d[nm][:, hf * 1024:(hf + 1) * 1024], tmp, reads=["dbgtmp"]))
                    if STAGE == 1:
                        continue
                    blocks = [(hh, bi) for hh in range(2) for bi in range(16)]

                    def s_block(hh, bi, blk_):
                        hs = slice(64 * hh, 64 * hh + 64)
                        r_, i = bi // nb, bi % nb
                        sb_ = (4, 5, 3)[blk_ % 3]
                        pbuf = blk_ % 4
                        js = [j for j in (i - 1, i, i + 1) if 0 <= j < nb]
                        qcols = slice(r_ * (S // dil) + i * 128, r_ * (S // dil) + (i + 1) * 128)
                        for j in js:
                            sl = j - i + 1
                            kcols = slice(r_ * (S // dil) + j * 128, r_ * (S // dil) + (j + 1) * 128)
                            P.op("pe", lambda e, sb_=sb_, sl=sl, kcols=kcols, qcols=qcols, hs=hs, buf=buf: e.matmul(
                                psum[sb_][:, sl * 128:(sl + 1) * 128], lhsT=kT[buf][hs, kcols], rhs=qT[buf][hs, qcols], start=True, stop=False),
                                reads=[("kT", buf), ("qT", buf)], writes=[PS(sb_)], accum=(j != js[0]))
                            P.op("pe", lambda e, sb_=sb_, sl=sl: e.matmul(
                                psum[sb_][:, sl * 128:(sl + 1) * 128], lhsT=ident_bf[:], rhs=maskA[:, sl * 128:(sl + 1) * 128], start=False, stop=True),
                                reads=["ident_bf", "maskA"], writes=[PS(sb_)], accum=True)
                        c0, c1 = (js[0] - i + 1) * 128, (js[-1] - i + 2) * 128
                        P.op("act", lambda e, sb_=sb_, pbuf=pbuf, c0=c0, c1=c1: e.activation(out=pT[pbuf][:, c0:c1], in_=psum[sb_][:, c0:c1], func=AF.Exp, scale=0.125),
                             reads=[PS(sb_)], writes=[PS(sb_), ("pT", pbuf)])

                    def pv_block(hh, bi, blk_):
                        hs = slice(64 * hh, 64 * hh + 64)
                        os_ = slice(64 * (1 - hh), 64 * (1 - hh) + 64)
                        r_, i = bi // nb, bi % nb
                        pbuf = blk_ % 4
                        js = [j for j in (i - 1, i, i + 1) if 0 <= j < nb]
                        pn = (6, 7)[(bi // 4) % 2]
                        oc = slice((bi % 4) * 128, (bi % 4 + 1) * 128)
                        for n_, j in enumerate(js):
                            sl = j - i + 1
                            vt = r_ * nb + j
                            first = (bi % 4 == 0 and n_ == 0)
                            P.op("pe", lambda e, pn=pn, oc=oc, vt=vt, sl=sl, pbuf=pbuf, buf=buf, hh=hh, n_=n_, nl=len(js) - 1: e.matmul(
                                psum[pn][:, oc], lhsT=vA[buf][hh][:, vt, :], rhs=pT[pbuf][:, sl * 128:(sl + 1) * 128], start=(n_ == 0), stop=(n_ == nl)),
                                reads=[("vA", buf), ("pT", pbuf)], writes=[PS(pn)], accum=not first)
                        if bi % 4 == 3:
                            b4 = bi // 4
                            if nb >= 4:
                                r0, nr, i0, ni = (4 * b4) // nb, 1, (4 * b4) % nb, 4
                            else:
                                r0, nr, i0, ni = 4 * b4, 4, 0, 1
                            for (acc, ps_rows, kk) in ((accn, hs, "accn"), (accd, os_, "accd")):
                                av = acc.rearrange("p (m r) -> p r m", r=dil)[hs, r0:r0 + nr, i0 * 128:(i0 + ni) * 128]
                                pv = psum[pn][ps_rows, :].rearrange("p (a c) -> p a c", a=nr)
                                if g == 0:
                                    P.op("dve", lambda e, av=av, pv=pv: e.tensor_copy(out=av, in_=pv), reads=[PS(pn)], writes=[PS(pn), (kk, hh)])
                                else:
                                    P.op("dve", lambda e, av=av, pv=pv: e.tensor_tensor(out=av, in0=pv, in1=av, op=ALU.add),
                                         reads=[PS(pn), (kk, hh)], writes=[PS(pn), (kk, hh)])

                    DEPTH = 2
                    for bx in range(min(DEPTH, len(blocks))):
                        s_block(blocks[bx][0], blocks[bx][1], blk + bx)
                    for bx, (hh, bi) in enumerate(blocks):
                        if bx + DEPTH < len(blocks):
                            s_block(blocks[bx + DEPTH][0], blocks[bx + DEPTH][1], blk + DEPTH)
                        pv_block(hh, bi, blk)
                        blk += 1
                if STAGE == 1:
                    continue
                P.op("dve", lambda e: e.reciprocal(out=accd, in_=accd), reads=[("accd", 0), ("accd", 1)], writes=[("accd", 0), ("accd", 1)])
                P.op("dve", lambda e, hp=hp: e.tensor_tensor(out=o_aT[:, hp, :], in0=accn, in1=accd, op=ALU.mult),
                     reads=[("accn", 0), ("accn", 1), ("accd", 0), ("accd", 1)], writes=[("o_aT", hp)])
            if STAGE == 2:
                tmp = scr(8192, 2048)
                for hp in range(4):
                    P.op("dve", lambda e, hp=hp: e.tensor_copy(out=tmp, in_=o_aT[:, hp, :]), reads=[("o_aT", hp)], writes=["dbgtmp"])
                    if "o_aT" in dbg_d:
                        final_ops.append(dma(dbg_d["o_aT"][hp], tmp, reads=["dbgtmp"]))
        P.barrier()

        if STAGE <= 2:
            P.emit(final_ops)
            return nc

        with contextlib.ExitStack() as sbk:
            rc = RopeCtx()
            rc.cos = scr(0, 2048)
            rc.sin = scr(2048, 2048)
            rden = [scr(4096 + i * 512, 512) for i in range(2)]
            rc.u = [scr(8192 + i * 512, 512) for i in range(2)]
            rc.sq = [scr(9216 + i * 512, 512) for i in range(2)]
            rc.rs = [scr(10240 + i * 512, 512) for i in range(2)]
            rc.t1 = [scr(11264 + i * 512, 512) for i in range(2)]
            wst = [scr(12288 + i * 1024, 1024).rearrange("p (k c) -> p k c", k=8) for i in range(2)]
            rc.R = sbt(sbk, "RB", [128, 128], F32)
            rc.R_key = "RBk"
            rc.tab_key = "tabB"
            rc.ps_proj = (0, 1)
            rc.ps_aux = (2, 3)
            wbf = [sbt(sbk, "wbfB%d" % i, [128, 8, 128], BF16) for i in range(3)]
            qbT = sbt(sbk, "qbT", [128, 4, S], BF16)
            kbd = [sbt(sbk, "kbd%d" % g, [128, S], BF16) for g in range(2)]
            vbd = [sbt(sbk, "vbd%d" % g, [128, 16, 128], BF16) for g in range(2)]
            vb1 = [[sbt(sbk, "vb1_%d_%d" % (g, hh), [128, 16, 128], BF16) for hh in range(2)] for g in range(2)]
            pTB = [sbt(sbk, "pTB%d" % i, [128, 512], BF16) for i in range(3)]
            dma(rc.cos, cd["cosB"][:, :], writes=["tabB"])
            dma(rc.sin, cd["sinB"][:, :], writes=["tabB"])
            dma(rc.R[:], cd["RB"][:, :], writes=["RBk"])
            cqb, ckb, cvb = 4608, 5120, 5248
            piecesB = [[(0, cqb + c * 128, 128)] for c in range(4)]
            for g in range(2):
                piecesB.append([(0, ckb + g * 64, 64), (64, ckb + g * 64, 64)])
                piecesB.append([(0, cvb + g * 64, 64), (64, cvb + g * 64, 64)])

            def loadB(j):
                load_w_piece(None, None, wbf[j % 3][:], ("wbfB", j % 3), piecesB[j], None)
            loadB(0)
            loadB(1)
            for c in range(4):
                if c + 2 < len(piecesB):
                    loadB(c + 2)
                qk_chunk(rc, wbf[c % 3], ("wbfB", c % 3), 24 + c, qbT[:, c, :], ("qbT", c), 1)
            for g in range(2):
                j = 4 + 2 * g
                if j + 2 < len(piecesB):
                    loadB(j + 2)
                qk_chunk(rc, wbf[j % 3], ("wbfB", j % 3), 28 + g, kbd[g][:], ("kbd", g), 1)
                if j + 3 < len(piecesB):
                    loadB(j + 3)
                v_tiles(wbf[(j + 1) % 3], ("wbfB", (j + 1) % 3), vbd[g], ("vbd", g), 1, (0, 1))
                for hh in range(2):
                    oh = 1 - hh
                    P.op("pool", lambda e, g=g, hh=hh, oh=oh: e.memset(vb1[g][hh][:, :, 64 * oh:64 * oh + 64], 1.0), writes=[("vb1", g, hh)])
                    P.op("pool", lambda e, g=g, hh=hh: e.tensor_copy(out=vb1[g][hh][:, :, 64 * hh:64 * hh + 64], in_=vbd[g][:, :, 64 * hh:64 * hh + 64]),
                         reads=[("vbd", g), ("vb1", g, hh)], writes=[("vb1", g, hh)])
            zt = scr(12288, 512).bitcast(BF16)
            P.op("pool", lambda e: e.memset(zt, 0.0), writes=["zt"])
            xs_flat = xs_d.rearrange("(p r) d -> p (r d)", p=128)
            ZR = NQ * 2 if (NBLK * BLK // 128) % (NQ * 2) == 0 else NQ
            assert (NBLK * BLK // 128) % ZR == 0
            for i in range(NBLK * BLK // 128 // ZR):
                zero_ops.append(P.op("pool", lambda e, i=i: e.dma_start(out=xs_flat[:, i * ZR * 1024:(i + 1) * ZR * 1024].rearrange("p (a c) -> p a c", a=ZR),
                                                                        in_=zt.unsqueeze(1).broadcast_to([128, ZR, 1024])), reads=["zt"], writes=[("xs0", i)], dma=True))
            tasks = [(h, qc, kt) for h in range(8) for qc in range(4) for kt in range(16)]

            def s_task(ix):
                h, qc, kt = tasks[ix]
                c, hh, g = h // 2, h % 2, h // 4
                hs = slice(64 * hh, 64 * hh + 64)
                qs = slice(qc * 512, (qc + 1) * 512)
                sb_ = 4 + ix % 2
                pb = ix % 3
                P.op("pe", lambda e, sb_=sb_, g=g, hs=hs, kt=kt, c=c, qs=qs: e.matmul(psum[sb_][:], lhsT=kbd[g][hs, kt * 128:(kt + 1) * 128], rhs=qbT[hs, c, qs], start=True, stop=True),
                     reads=[("kbd", g), ("qbT", c)], writes=[PS(sb_)])
                P.op("act", lambda e, sb_=sb_, pb=pb: e.activation(out=pTB[pb][:], in_=psum[sb_][:], func=AF.Exp, scale=0.125),
                     reads=[PS(sb_)], writes=[PS(sb_), ("pTB", pb)])

            def pv_task(ix):
                h, qc, kt = tasks[ix]
                c, hh, g = h // 2, h % 2, h // 4
                hs = slice(64 * hh, 64 * hh + 64)
                os_ = slice(64 * (1 - hh), 64 * (1 - hh) + 64)
                qs = slice(qc * 512, (qc + 1) * 512)
                par = (h * 4 + qc) % 2
                pn = (6, 7)[par]
                pb = ix % 3
                P.op("pe", lambda e, pn=pn, g=g, hh=hh, kt=kt, pb=pb: e.matmul(psum[pn][:], lhsT=vb1[g][hh][:, kt, :], rhs=pTB[pb][:], start=(kt == 0), stop=(kt == 15)),
                     reads=[("vb1", g, hh), ("pTB", pb)], writes=[PS(pn)], accum=(kt > 0))
                if kt == 15:
                    rd = rden[par]
                    P.op("dve", lambda e, rd=rd, hs=hs, os_=os_, pn=pn: e.reciprocal(out=rd[hs, :], in_=psum[pn][os_, :]), reads=[PS(pn)], writes=[PS(pn), ("rden", par)])
                    P.op("dve", lambda e, rd=rd, hs=hs, pn=pn, c=c, qs=qs: e.tensor_tensor(out=o_bT[hs, c, qs], in0=psum[pn][hs, :], in1=rd[hs, :], op=ALU.mult),
                         reads=[PS(pn), ("rden", par)], writes=[PS(pn), ("o_bT", c)])

            s_task(0)
            for ix in range(len(tasks)):
                if ix + 1 < len(tasks):
                    s_task(ix + 1)
                pv_task(ix)
            if STAGE == 3:
                tmp = scr(8192, 2048)
                for c in range(4):
                    P.op("dve", lambda e, c=c: e.tensor_copy(out=tmp, in_=o_bT[:, c, :]), reads=[("o_bT", c)], writes=["dbgtmp"])
                    if "o_bT" in dbg_d:
                        final_ops.append(dma(dbg_d["o_bT"][c], tmp, reads=["dbgtmp"]))
        P.barrier()
        if STAGE == 3:
            P.emit(final_ops)
            return nc

        o_keys_a = [("o_aT", c) for c in range(4)]
        o_keys_b = [("o_bT", c) for c in range(4)]
        with contextlib.ExitStack() as sm:
            bbf2 = [sbt(sm, "bbf%d" % i, [128, 24, 128], BF16) for i in range(2)]
            woutbf = sbt(sm, "woutbf", [128, 8, D], BF16)
            mergedT = sbt(sm, "mergedT", [128, 8, S], BF16)
            sg0 = [sbt(sm, "sg0_%d" % i, [128, 512], F32) for i in range(2)]
            sg1 = [sbt(sm, "sg1_%d" % i, [128, 512], F32) for i in range(2)]
            for tt in range(NT):
                dma(x1[:, tt, :], x_d[tt * 128:(tt + 1) * 128, :], writes=[("x1", tt)])
            w_out_v = w_out_d.rearrange("(kc p) n -> p kc n", p=128)
            for q in range(4):
                P.op("pool", lambda e, q=q: e.dma_start(out=woutbf[:, 2 * q:2 * q + 2, :], in_=w_out_v[:, 2 * q:2 * q + 2, :]), writes=[("woutbf", q)], dma=True)
            it = 0

            def loadM(m_):
                P.op("pool", lambda e: e.dma_start(out=bbf2[m_ % 2][:], in_=mb_d[m_]), writes=[("bbf", m_ % 2)], dma=True)
            loadM(0)
            for m in range(8):
                bbf = bbf2[m % 2]
                kbb = ("bbf", m % 2)
                for tc in range(4):
                    if tc == 1 and m + 1 < 8:
                        loadM(m + 1)
                    ts_ = slice(tc * 512, (tc + 1) * 512)
                    par = it % 2
                    it += 1
                    bA, bB, bG0, bG1 = (0, 1, 2, 3) if par == 0 else (4, 5, 6, 7)
                    for c in range(4):
                        P.op("pe", lambda e, c=c, bA=bA, ts_=ts_, bbf=bbf: e.matmul(psum[bA][:], lhsT=bbf[:, c, :], rhs=o_aT[:, c, ts_], start=(c == 0), stop=(c == 3)),
                             reads=[kbb] + o_keys_a, writes=[PS(bA)], accum=(c > 0))
                    for c in range(4):
                        P.op("pe", lambda e, c=c, bB=bB, ts_=ts_, bbf=bbf: e.matmul(psum[bB][:], lhsT=bbf[:, 4 + c, :], rhs=o_bT[:, c, ts_], start=(c == 0), stop=(c == 3)),
                             reads=[kbb] + o_keys_b, writes=[PS(bB)], accum=(c > 0))
                    for kc in range(8):
                        P.op("pe", lambda e, kc=kc, bG0=bG0, ts_=ts_, bbf=bbf: e.matmul(psum[bG0][:], lhsT=bbf[:, 8 + kc, :], rhs=hT[:, kc, ts_], start=(kc == 0), stop=(kc == 7)),
                             reads=[kbb] + hT_keys, writes=[PS(bG0)], accum=(kc > 0))
                    for kc in range(8):
                        P.op("pe", lambda e, kc=kc, bG1=bG1, ts_=ts_, bbf=bbf: e.matmul(psum[bG1][:], lhsT=bbf[:, 16 + kc, :], rhs=hT[:, kc, ts_], start=(kc == 0), stop=(kc == 7)),
                             reads=[kbb] + hT_keys, writes=[PS(bG1)], accum=(kc > 0))
                    P.op("act", lambda e, m=m, bG0=bG0, par=par: e.activation(out=sg0[par][:], in_=psum[bG0][:], func=AF.Sigmoid, bias=bgate[:, m:m + 1]),
                         reads=[PS(bG0), "bgate"], writes=[PS(bG0), ("sg0", par)])
                    P.op("act", lambda e, m=m, bG1=bG1, par=par: e.activation(out=sg1[par][:], in_=psum[bG1][:], func=AF.Sigmoid, bias=bgate[:, 8 + m:9 + m]),
                         reads=[PS(bG1), "bgate"], writes=[PS(bG1), ("sg1", par)])
                    P.op("dve", lambda e, bA=bA, par=par: e.tensor_tensor(out=sg0[par][:], in0=psum[bA][:], in1=sg0[par][:], op=ALU.mult),
                         reads=[PS(bA), ("sg0", par)], writes=[PS(bA), ("sg0", par)])
                    P.op("dve", lambda e, bB=bB, par=par: e.tensor_tensor(out=sg1[par][:], in0=psum[bB][:], in1=sg1[par][:], op=ALU.mult),
                         reads=[PS(bB), ("sg1", par)], writes=[PS(bB), ("sg1", par)])
                    P.op("pool", lambda e, m=m, par=par, ts_=ts_: e.tensor_tensor(out=mergedT[:, m, ts_], in0=sg0[par][:], in1=sg1[par][:], op=ALU.add),
                         reads=[("sg0", par), ("sg1", par)], writes=[("mergedT", m, tc)])
            for tt in range(NT):
                for nh in range(2):
                    b = (0, 1, 4, 5)[(tt * 2 + nh) % 4]
                    ns = slice(nh * 512, (nh + 1) * 512)
                    for m in range(8):
                        P.op("pe", lambda e, m=m, b=b, tt=tt, ns=ns: e.matmul(psum[b][:], lhsT=mergedT[:, m, tt * 128:(tt + 1) * 128], rhs=woutbf[:, m, ns], start=(m == 0), stop=(m == 7)),
                             reads=[("mergedT", m, tt // 4), ("woutbf", m // 2)], writes=[PS(b)], accum=(m > 0))
                    P.op("dve", lambda e, b=b, tt=tt, ns=ns: e.tensor_tensor(out=x1[:, tt, ns], in0=psum[b][:], in1=x1[:, tt, ns], op=ALU.add),
                         reads=[PS(b), ("x1", tt)], writes=[PS(b), ("x1", tt)])
        x1_keys = [("x1", tt) for tt in range(NT)]
        if STAGE == 4:
            for tt in range(NT):
                if "x1" in dbg_d:
                    final_ops.append(dma(dbg_d["x1"][tt * 128:(tt + 1) * 128, :], x1[:, tt, :], reads=[("x1", tt)]))
            P.emit(final_ops)
            return nc
        P.barrier()

        I32 = mybir.dt.int32
        wgu_rows = wgu_d.rearrange("e k n -> (e k) n")
        wd_rows = wd_d.rearrange("e k n -> (e k) n")
        IOA = bass.IndirectOffsetOnAxis

        def idma(out, out_off, in_, in_off, bound, reads=(), writes=()):
            def f(e):
                if bound is None:
                    return e.indirect_dma_start(out=out, out_offset=out_off, in_=in_, in_offset=in_off)
                return e.indirect_dma_start(out=out, out_offset=out_off, in_=in_, in_offset=in_off, bounds_check=pregs[bound], oob_is_err=False)
            return P.op("pool", f, reads=reads, writes=writes, dma=True)

        pregs = {}

        def mkreg(name, val):
            def f(e):
                pregs[name] = e.alloc_register(name)
                return e.reg_mov(pregs[name], val)
            P.op("pool", f)
        mkreg("bw", NE * D - 1)
        mkreg("bb", NE * 128 - 1)

        with contextlib.ExitStack() as se:
            print("SBUF remaining before moe scope", nc.sbuf_bytes_remaining)
            wgubf = obuf
            wdbf = sbt(se, "wdbf", [128, 8, D], BF16)
            gwk = sbt(se, "gwk", [128, NT, 4], F32)
            desti = sbt(se, "desti", [128, NT * 4], I32)
            be = sbt(se, "be", [128, NBLK], F32)
            widx = sbt(se, "widx", [128, NBLK, 8], I32)
            bidx = sbt(se, "bidx", [128, NBLK], I32)
            h2tok = hT[:].rearrange("p k s -> p (k s)").rearrange("p (t f) -> p t f", t=NT)
            with contextlib.ExitStack() as sr:
                stg = [sbt(sr, "stg%d" % i, [128, 2048], F32) for i in range(2)]
                hn2s = [stg[i][:, 0:1024] for i in range(2)]
                h32s = [stg[i][:, 1024:2048].rearrange("p (k c) -> p k c", k=8) for i in range(2)]
                wr = sbt(sr, "wr", [128, 8, 32], F32)
                wrf = sbt(sr, "wrf", [128, 8, 32], F32)
                brt = sbt(sr, "brt", [128, 32], F32)
                tiny = []
                for nm_, shp_, dt_ in (("logit", [128, 32], F32), ("mx8", [128, 8], F32), ("negmax", [128, 1], F32), ("msk", [128, 32], F32), ("ex", [128, 32], F32),
                                       ("ssum", [128, 1], F32), ("gw", [128, 32], F32), ("gwT", [32, 128], F32), ("mskb", [128, 32], BF16), ("e4", [128, 4], F32), ("s4", [128, 1], F32)):
                    tiny.append([sbt(sr, "%s_%d" % (nm_, i), shp_, dt_) for i in range(2)])
                bd32 = sbt(sr, "bd32", [32, D], F32)
                utri = sbt(sr, "utri", [128, 128], BF16)
                basekc = sbt(sr, "basekc", [128, 8], F32)
                cum = sbt(sr, "cum", [128, 32], F32)
                rank_all = sbt(sr, "rank_all", [128, NT, 32], F32)
                logit_all = sbt(sr, "logit_all", [128, NT, 32], F32)
                mx8_all = sbt(sr, "mx8_all", [128, NT, 8], F32)
                pada = sbt(sr, "pada", [128, 32], F32)
                padb = sbt(sr, "padb", [128, 32], F32)
                padded = sbt(sr, "padded", [128, 32], F32)
                pstart = sbt(sr, "pstart", [128, 32], F32)
                dest_all = sbt(sr, "dest_all", [128, NT, 32], F32)
                junk32 = sbt(sr, "junk32", [128, 32], F32)
                destk = sbt(sr, "destk", [128, NT * 4], F32)
                widf = sbt(sr, "widf", [128, NBLK, 8], F32)
                bidf = sbt(sr, "bidf", [128, NBLK], F32)
                dma(wr[:], w_router_d.rearrange("(kc p) n -> p kc n", p=128), writes=["wr"])
                dma(brt[:], b_router_d[0:1, :].broadcast_to([128, 32]), writes=["brt"])
                dma(bd32[:], bd_d[:, :], writes=["bd32"])
                dma(basekc[:], cd["basekc"][:, :], writes=["basekc"])
                blkthr = sbt(sr, "blkthr", [128, NBLK], F32)
                dma(blkthr[:], cd["blkthr"][:, :], writes=["blkthr"])
                dma(stg[1][:, 1024:1152], cd["utri"][:, :], writes=["h32b"])
                P.op("pool", lambda e: e.tensor_copy(out=utri[:], in_=stg[1][:, 1024:1152]), reads=["h32b"], writes=["utri"])
                P.op("pool", lambda e: e.memset(cum[:], 0.0), writes=["cum"])
                P.op("pool", lambda e: e.tensor_tensor(out=wrf[:], in0=wr[:], in1=gffn[:, 0:8].unsqueeze(2).broadcast_to([128, 8, 32]), op=ALU.mult),
                     reads=["wr", "gffn"], writes=["wrf"])
                def route_tile(tt):
                    par = tt % 2
                    hn2, junk2, h32 = hn2s[par], h32s[par].rearrange("p k c -> p (k c)"), h32s[par]
                    khn, kh32 = ("hn2a", "hn2b")[par], ("h32a", "h32b")[par]
                    logit, mx8, negmax, msk, ex, ssum, gw, gwT, mskb, e4, s4 = [t_[par] for t_ in tiny]
                    kk = lambda n: (n, par)
                    pl, pg = (5, 4) if par == 0 else (1, 0)
                    norm_transpose(tt, x1[:, tt, :], ("x1", tt), hn2, khn, junk2, kh32, None, None, (6, 7), extra32=(h32, kh32))
                    P.op("act", lambda e, tt=tt: e.copy(out=h2tok[:, tt, :], in_=hn2), reads=[khn], writes=[("h2tok", tt)])
                    for kc in range(8):
                        P.op("pe", lambda e, kc=kc: e.matmul(psum[pl][:, 0:32], lhsT=h32[:, kc, :], rhs=wrf[:, kc, :], start=(kc == 0), stop=(kc == 7)),
                             reads=[kh32, "wrf"], writes=[PS(pl)], accum=(kc > 0))
                    P.op("dve", lambda e: e.tensor_tensor(out=logit[:], in0=psum[pl][:, 0:32], in1=brt[:], op=ALU.add), reads=[PS(pl), "brt"], writes=[PS(pl), kk("logit")])
                    P.op("dve", lambda e: e.max(out=mx8[:], in_=logit[:]), reads=[kk("logit")], writes=[kk("mx8")])
                    P.op("dve", lambda e: e.tensor_scalar(out=msk[:], in0=logit[:], scalar1=mx8[:, 3:4], scalar2=None, op0=ALU.is_ge), reads=[kk("logit"), kk("mx8")], writes=[kk("msk")])
                    P.op("dve", lambda e: e.tensor_scalar(out=negmax[:], in0=mx8[:, 0:1], scalar1=-1.0, scalar2=None, op0=ALU.mult), reads=[kk("mx8")], writes=[kk("negmax")])
                    P.op("act", lambda e: e.activation(out=ex[:], in_=logit[:], func=AF.Exp, bias=negmax[:, 0:1], scale=1.0), reads=[kk("logit"), kk("negmax")], writes=[kk("ex")])
                    P.op("dve", lambda e: e.tensor_tensor(out=ex[:], in0=ex[:], in1=msk[:], op=ALU.mult), reads=[kk("ex"), kk("msk")], writes=[kk("ex")])
                    P.op("dve", lambda e: e.reduce_sum(out=ssum[:], in_=ex[:], axis=AX.X), reads=[kk("ex")], writes=[kk("ssum")])
                    P.op("dve", lambda e: e.reciprocal(out=ssum[:], in_=ssum[:]), reads=[kk("ssum")], writes=[kk("ssum")])
                    P.op("dve", lambda e: e.tensor_scalar(out=gw[:], in0=ex[:], scalar1=ssum[:, 0:1], scalar2=None, op0=ALU.mult), reads=[kk("ex"), kk("ssum")], writes=[kk("gw")])
                    P.op("pool", lambda e, tt=tt: e.tensor_copy(out=logit_all[:, tt, :], in_=logit[:]), reads=[kk("logit")], writes=[("logit_all", tt)])
                    P.op("pool", lambda e, tt=tt: e.tensor_copy(out=mx8_all[:, tt, :], in_=mx8[:]), reads=[kk("mx8")], writes=[("mx8_all", tt)])
                    P.op("pool", lambda e: e.tensor_copy(out=mskb[:], in_=msk[:]), reads=[kk("msk")], writes=[kk("mskb")])
                    P.op("act", lambda e: e.activation(out=e4[:], in_=mx8[:, 0:4], func=AF.Exp, bias=negmax[:, 0:1], scale=1.0), reads=[kk("mx8"), kk("negmax")], writes=[kk("e4")])
                    P.op("dve", lambda e: e.reduce_sum(out=s4[:], in_=e4[:], axis=AX.X), reads=[kk("e4")], writes=[kk("s4")])
                    P.op("dve", lambda e: e.reciprocal(out=s4[:], in_=s4[:]), reads=[kk("s4")], writes=[kk("s4")])
                    P.op("dve", lambda e, tt=tt: e.tensor_scalar(out=gwk[:, tt, :], in0=e4[:], scalar1=s4[:, 0:1], scalar2=None, op0=ALU.mult), reads=[kk("e4"), kk("s4")], writes=[("gwk", tt)])
                    P.op("pe", lambda e: e.matmul(psum[pl][:, 32:64], lhsT=utri[:], rhs=mskb[:], start=True, stop=True), reads=["utri", kk("mskb")], writes=[PS(pl)])
                    P.op("pe", lambda e: e.matmul(psum[pl][:, 64:96], lhsT=ones_bf[:], rhs=mskb[:], start=True, stop=True), reads=["ones_bf", kk("mskb")], writes=[PS(pl)], accum=True)
                    P.op("dve", lambda e, tt=tt: e.tensor_tensor(out=rank_all[:, tt, :], in0=psum[pl][:, 32:64], in1=cum[:], op=ALU.add), reads=[PS(pl), "cum"], writes=[PS(pl), ("rank_all", tt)])
                    P.op("dve", lambda e: e.tensor_tensor(out=cum[:], in0=psum[pl][:, 64:96], in1=cum[:], op=ALU.add), reads=[PS(pl), "cum"], writes=[PS(pl), "cum"])
                    P.op("pe", lambda e: e.transpose(out=psum[pg][0:32, 0:128], in_=gw[:], identity=ident[:]), reads=[kk("gw"), "ident"], writes=[PS(pg)])
                    P.op("act", lambda e: e.copy(out=gwT[:], in_=psum[pg][0:32, 0:128]), reads=[PS(pg)], writes=[PS(pg), kk("gwT")])
                    for nh in range(2):
                        ns = slice(nh * 512, (nh + 1) * 512)
                        b = 2 + nh
                        P.op("pe", lambda e, b=b, ns=ns: e.matmul(psum[b][:], lhsT=gwT[:], rhs=bd32[:, ns], start=True, stop=True), reads=[kk("gwT"), "bd32"], writes=[PS(b)])
                        P.op("dve", lambda e, b=b, tt=tt, ns=ns: e.tensor_tensor(out=x1[:, tt, ns], in0=psum[b][:], in1=x1[:, tt, ns], op=ALU.add),
                             reads=[PS(b), ("x1", tt)], writes=[PS(b), ("x1", tt)])

                for tt in range(NT):
                    route_tile(tt)
                rk_keys = [("rank_all", tt) for tt in range(NT)]
                P.op("pool", lambda e: e.memset(padb[:], 0.0), writes=["padb"])
                for j in range(-(-S // BLK)):
                    P.op("dve", lambda e, j=j: e.scalar_tensor_tensor(out=padb[:], in0=cum[:], scalar=float(BLK * j), in1=padb[:], op0=ALU.is_gt, op1=ALU.add),
                         reads=["cum", "padb"], writes=["padb"])
                P.op("dve", lambda e: e.tensor_scalar(out=padded[:], in0=padb[:], scalar1=float(BLK), scalar2=None, op0=ALU.mult), reads=["padb"], writes=["padded"])
                P.op("dve", lambda e: e.tensor_copy(out=pada[:], in_=padded[:]), reads=["padded", "padb"], writes=["pada"])
                src_, dst_ = pada, padb
                for st_ in (1, 2, 4, 8, 16):
                    P.op("dve", lambda e, src_=src_, dst_=dst_, st_=st_: e.tensor_copy(out=dst_[:, 0:st_], in_=src_[:, 0:st_]), reads=["pada", "padb"], writes=["pada", "padb"])
                    P.op("dve", lambda e, src_=src_, dst_=dst_, st_=st_: e.tensor_tensor(out=dst_[:, st_:32], in0=src_[:, st_:32], in1=src_[:, 0:32 - st_], op=ALU.add),
                         reads=["pada", "padb"], writes=["pada", "padb"])
                    src_, dst_ = dst_, src_
                pend = src_
                P.op("dve", lambda e: e.tensor_tensor(out=pstart[:], in0=pend[:], in1=padded[:], op=ALU.subtract), reads=["pada", "padb", "padded"], writes=["pstart"])
                P.op("dve", lambda e: e.tensor_tensor(out=dest_all[:], in0=rank_all[:], in1=pstart[:].unsqueeze(1).broadcast_to([128, NT, 32]), op=ALU.add),
                     reads=rk_keys + ["pstart"], writes=["dest_all"])
                big = stg[0][:].rearrange("p (t k e) -> p t k e", t=NT, k=4)
                la_keys = [("logit_all", tt) for tt in range(NT)] + [("mx8_all", tt) for tt in range(NT)]
                P.op("dve", lambda e: e.tensor_tensor(out=big, in0=logit_all[:].unsqueeze(2).broadcast_to([128, NT, 4, 32]),
                                                      in1=mx8_all[:, :, 0:4].unsqueeze(3).broadcast_to([128, NT, 4, 32]), op=ALU.is_equal),
                     reads=la_keys + ["hn2a", "h32a"], writes=["big"])
                P.op("dve", lambda e: e.tensor_tensor(out=big, in0=big, in1=dest_all[:].unsqueeze(2).broadcast_to([128, NT, 4, 32]), op=ALU.mult),
                     reads=["big", "dest_all"], writes=["big"])
                P.op("dve", lambda e: e.reduce_sum(out=destk[:], in_=stg[0][:].rearrange("p (c e) -> p c e", e=32), axis=AX.X), reads=["big"], writes=["destk"])
                P.op("dve", lambda e: e.tensor_copy(out=desti[:], in_=destk[:]), reads=["destk"], writes=["desti"])
                big2 = stg[1][:, 0:NBLK * 32].rearrange("p (b e) -> p b e", e=32)
                P.op("dve", lambda e: e.tensor_tensor(out=big2, in0=pend[:].unsqueeze(1).broadcast_to([128, NBLK, 32]),
                                                      in1=blkthr[:].unsqueeze(2).broadcast_to([128, NBLK, 32]), op=ALU.is_le),
                     reads=["pada", "padb", "blkthr", "h32a", "h32b", "hn2b"], writes=["big2"])
                P.op("dve", lambda e: e.reduce_sum(out=be[:], in_=big2, axis=AX.X), reads=["big2"], writes=["be"])
                P.op("dve", lambda e: e.tensor_scalar(out=bidf[:], in0=be[:], scalar1=1024.0, scalar2=None, op0=ALU.mult), reads=["be"], writes=["bidf"])
                P.op("dve", lambda e: e.tensor_tensor(out=widf[:], in0=bidf[:].unsqueeze(2).broadcast_to([128, NBLK, 8]), in1=basekc[:].unsqueeze(1).broadcast_to([128, NBLK, 8]), op=ALU.add),
                     reads=["bidf", "basekc"], writes=["widf"])
                P.op("dve", lambda e: e.tensor_copy(out=widx[:], in_=widf[:]), reads=["widf"], writes=["widx"])
                P.op("dve", lambda e: e.tensor_scalar(out=bidf[:], in0=be[:], scalar1=128.0, scalar2=basekc[:, 0:1], op0=ALU.mult, op1=ALU.add), reads=["be", "basekc", "widf"], writes=["bidf"])
                P.op("dve", lambda e: e.tensor_copy(out=bidx[:], in_=bidf[:]), reads=["bidf"], writes=["bidx"])
                if STAGE == 5:
                    gwk_keys = [("gwk", tt) for tt in range(NT)]
                    final_ops.append(dma(dbg_d["destk"][:, :], destk[:], reads=["destk"]))
                    final_ops.append(dma(dbg_d["be"][:, :], be[:], reads=["be"]))
                    final_ops.append(dma(dbg_d["gwk"][:, :], gwk[:].rearrange("p a b -> p (a b)"), reads=gwk_keys))
                    final_ops.append(dma(dbg_d["widf"][:, :], widf[:].rearrange("p a b -> p (a b)"), reads=["widf"]))
            if STAGE == 5:
                P.emit(final_ops)
                return nc
            P.barrier()
            nrow = NBLK * BLK
            xs0_keys = [("xs0", i) for i in range(len(zero_ops))]
            sc_ops = []
            for tt in range(NT):
                for k in range(4):
                    c_ = tt * 4 + k
                    sc_ops.append(idma(xs_d[:, :], IOA(ap=desti[:, c_:c_ + 1], axis=0), h2tok[:, tt, :], None, None,
                                       reads=[("h2tok", tt), "desti"] + (xs0_keys if c_ == 0 else []), writes=[("xs", c_)]))
            xs_keys = [("xs", c_) for c_ in range(NT * 4)]
            P.barrier()
            with contextlib.ExitStack() as sb_:
                print("SBUF remaining before block scope", nc.sbuf_bytes_remaining)
                wgu2 = [obuf, hT]
                identb = sbt(sb_, "identb", [128, 128], BF16)
                xtok = sbt(sb_, "xtok", [128, NQ, D], BF16)
                xT = [sbt(sb_, "xT%d" % i, [128, 8, BLK], BF16) for i in range(2)]
                actT = [sbt(sb_, "actT%d" % i, [128, 8, BLK], BF16) for i in range(2)]
                gtb = [sbt(sb_, "gtb%d" % i, [128, BLK], F32) for i in range(2)]
                sgb = [sbt(sb_, "sgb%d" % i, [128, BLK], F32) for i in range(2)]
                u1b = [sbt(sb_, "u1b%d" % i, [128, BLK], F32) for i in range(2)]
                ysb = [sbt(sb_, "ysb%d" % i, [128, 512], F32) for i in range(3)]
                bblk = [sbt(sb_, "bblk%d" % i, [128, 16], F32) for i in range(2)]
                P.op("dve", lambda e: e.tensor_copy(out=identb[:], in_=ident[:]), reads=["ident"], writes=["identb"])
                for i in range(2):
                    P.op("dve", lambda e, i=i: e.memset(bblk[i][:], 0.0), writes=[("bblk", i)])
                itc = [0]
                ycnt = [0]

                half = NBLK - 32
                order = []
                for i in range(half):
                    order += [i, 32 + i]
                order += list(range(half, 32))
                assert sorted(order) == list(range(NBLK))

                def load_wgu(b):
                    blk_ = order[b]
                    wb_ = wgu2[b % 2]
                    for kc in range(8):
                        idma(wb_[:, kc, :], None, wgu_rows[:, :], IOA(ap=widx[:, blk_, kc:kc + 1], axis=0), "bw", reads=["widx"], writes=[("wgu", b % 2, kc)])
                    idma(bblk[b % 2][:], None, bgu_d[:, :], IOA(ap=bidx[:, blk_:blk_ + 1], axis=0), "bb", reads=["bidx"], writes=[("bblk", b % 2)])
                    P.op("dve", lambda e, b=b: e.tensor_scalar(out=bblk[b % 2][:, 8:16], in0=bblk[b % 2][:, 8:16], scalar1=1.0, scalar2=None, op0=ALU.add),
                         reads=[("bblk", b % 2)], writes=[("bblk", b % 2)])

                def load_wd(b):
                    blk_ = order[b]
                    for kc in range(8):
                        idma(wdbf[:, kc, :], None, wd_rows[:, :], IOA(ap=widx[:, blk_, kc:kc + 1], axis=0), "bw", reads=["widx"], writes=[("wd", kc)])

                def load_x(b):
                    blk_ = order[b]
                    for q in range(NQ):
                        dma(xtok[:, q, :], xs_d[blk_ * BLK + q * 128: blk_ * BLK + (q + 1) * 128, :], reads=xs_keys if q == 0 else [], writes=[("xtok", q)])
                    xt_ = xT[b % 2]
                    for kc in range(8):
                        bk = 6 + kc % 2
                        pv = psum[bk][:].bitcast(BF16)
                        for q in range(NQ):
                            P.op("pe", lambda e, kc=kc, q=q, pv=pv: e.transpose(out=pv[:, q * 128:(q + 1) * 128], in_=xtok[:, q, kc * 128:(kc + 1) * 128], identity=identb[:]),
                                 reads=[("xtok", q), "identb"], writes=[PS(bk)], accum=(q > 0))
                        P.op("act", lambda e, kc=kc, pv=pv, xt_=xt_: e.activation(out=xt_[:, kc, :], in_=pv[:, 0:BLK], func=AF.Copy, scale=gffn[:, kc:kc + 1]),
                             reads=[PS(bk), "gffn"], writes=[PS(bk), ("xT", b % 2, kc)])

                pend_fin = []

                def gu_phase(b):
                    wb_ = wgu2[b % 2]
                    xt_ = xT[b % 2]
                    ap_ = b % 2
                    for m in range(8):
                        bpar = itc[0] % 2
                        par = itc[0] % 2
                        itc[0] += 1
                        bg_, bu_ = (0, 1) if bpar == 0 else (2, 3)
                        for kc in range(8):
                            P.op("pe", lambda e, kc=kc, m=m, bg_=bg_: e.matmul(psum[bg_][:, 0:BLK], lhsT=wb_[:, kc, m * 128:(m + 1) * 128], rhs=xt_[:, kc, :], start=(kc == 0), stop=(kc == 7)),
                                 reads=[("wgu", b % 2, kc), ("xT", b % 2, kc)], writes=[PS(bg_)], accum=(kc > 0))
                        for kc in range(8):
                            P.op("pe", lambda e, kc=kc, m=m, bu_=bu_: e.matmul(psum[bu_][:, 0:BLK], lhsT=wb_[:, kc, 1024 + m * 128:1024 + (m + 1) * 128], rhs=xt_[:, kc, :], start=(kc == 0), stop=(kc == 7)),
                                 reads=[("wgu", b % 2, kc), ("xT", b % 2, kc)], writes=[PS(bu_)], accum=(kc > 0))
                        gt, sg, u1 = gtb[bpar], sgb[par], u1b[bpar]
                        bb_ = bblk[b % 2]
                        P.op("dve", lambda e, gt=gt, bg_=bg_, m=m, bb_=bb_: e.tensor_scalar(out=gt[:], in0=psum[bg_][:, 0:BLK], scalar1=bb_[:, m:m + 1], scalar2=7.0, op0=ALU.add, op1=ALU.min),
                             reads=[PS(bg_), ("bblk", b % 2)], writes=[PS(bg_), ("gt", bpar)])
                        P.op("act", lambda e, gt=gt, sg=sg: e.activation(out=sg[:], in_=gt[:], func=AF.Sigmoid, scale=1.702), reads=[("gt", bpar)], writes=[("sg", par)])
                        P.op("dve", lambda e, u1=u1, bu_=bu_, m=m, bb_=bb_: e.tensor_scalar(out=u1[:], in0=psum[bu_][:, 0:BLK], scalar1=bb_[:, 8 + m:9 + m], scalar2=8.0, op0=ALU.add, op1=ALU.min),
                             reads=[PS(bu_), ("bblk", b % 2)], writes=[PS(bu_), ("u1", bpar)])
                        P.op("dve", lambda e, gt=gt, u1=u1: e.scalar_tensor_tensor(out=u1[:], in0=u1[:], scalar=-6.0, in1=gt[:], op0=ALU.max, op1=ALU.mult),
                             reads=[("gt", bpar), ("u1", bpar)], writes=[("u1", bpar)])

                        def fin(sg=sg, u1=u1, ap_=ap_, m=m, par=par, bpar=bpar):
                            P.op("dve", lambda e: e.tensor_tensor(out=actT[ap_][:, m, :], in0=u1[:], in1=sg[:], op=ALU.mult),
                                 reads=[("sg", par), ("u1", bpar)], writes=[("actT", ap_, m)])
                        if pend_fin:
                            pend_fin.pop()()
                        pend_fin.append(fin)
                    if pend_fin:
                        pend_fin.pop()()

                def down_phase(b):
                    ap_ = b % 2
                    blk_ = order[b]
                    for q in range(NQ):
                        for nh in range(2):
                            yi = ycnt[0] % 3
                            ycnt[0] += 1
                            yb = ysb[yi]
                            yk = ("ysb", yi)
                            bk = 4 + nh
                            ns = slice(nh * 512, (nh + 1) * 512)
                            for m in range(8):
                                P.op("pe", lambda e, m=m, bk=bk, q=q, ns=ns: e.matmul(psum[bk][:], lhsT=actT[ap_][:, m, q * 128:(q + 1) * 128], rhs=wdbf[:, m, ns], start=(m == 0), stop=(m == 7)),
                                     reads=[("actT", ap_, m), ("wd", m)], writes=[PS(bk)], accum=(m > 0))
                            if nh == 0:
                                P.op("act", lambda e, bk=bk, yb=yb: e.copy(out=yb[:], in_=psum[bk][:]), reads=[PS(bk)], writes=[PS(bk), yk])
                            else:
                                P.op("dve", lambda e, bk=bk, yb=yb: e.tensor_copy(out=yb[:], in_=psum[bk][:]), reads=[PS(bk)], writes=[PS(bk), yk])
                            dma(y_d[blk_ * BLK + q * 128: blk_ * BLK + (q + 1) * 128, ns], yb[:], reads=[yk], writes=[("y", b, q, nh)])

                load_wgu(0)
                load_wgu(1)
                load_wd(0)
                load_x(0)
                gu_phase(0)
                for b in range(NBLK):
                    if b + 1 < NBLK:
                        load_x(b + 1)
                        gu_phase(b + 1)
                    if b + 2 < NBLK:
                        load_wgu(b + 2)
                    down_phase(b)
                    if b + 1 < NBLK:
                        load_wd(b + 1)
            P.barrier()
            with contextlib.ExitStack() as sc:
                yg = [sbt(sc, "yg%d" % i, [128, D], F32) for i in range(4)]
                for tt in range(NT):
                    for k in range(4):
                        c_ = tt * 4 + k
                        gi = c_ % 4
                        idma(yg[gi][:], None, y_d[:, :], IOA(ap=desti[:, c_:c_ + 1], axis=0), None, reads=["desti"], writes=[("yg", gi)])
                        P.op("dve", lambda e, tt=tt, k=k, gi=gi: e.scalar_tensor_tensor(out=x1[:, tt, :], in0=yg[gi][:], scalar=gwk[:, tt, k:k + 1], in1=x1[:, tt, :], op0=ALU.mult, op1=ALU.add),
                             reads=[("yg", gi), ("gwk", tt), ("x1", tt)], writes=[("x1", tt)])
                    final_ops.append(dma(out_d[tt * 128:(tt + 1) * 128, :], x1[:, tt, :], reads=[("x1", tt)]))
        P.emit(final_ops)
    return nc


def _in_maps(inputs):
    inp = {k: np.asarray(v) for k, v in inputs.items()}
    r = _relayout(inp)
    c = _consts()
    base = dict(r)
    if STAGE <= 4:
        base.pop("w_gate_up")
        base.pop("w_down")
    for k, v in c.items():
        base["c_" + k] = v
    maps = []
    for b in range(inp["x"].shape[0]):
        m = dict(base)
        m["x"] = np.ascontiguousarray(inp["x"][b])
        maps.append(m)
    return maps


def kernel(**inputs):
    maps = _in_maps(inputs)
    nc = build()
    res = run_bass_kernel_spmd(nc, maps, core_ids=list(range(len(maps))))
    return np.stack([r["out"] for r in res.results], axis=0).astype(np.float32)
```

```python
import contextlib
import os
import numpy as np
import concourse.bass as bass
import concourse.mybir as mybir
from concourse.bass_utils import run_bass_kernel_spmd

F32 = mybir.dt.float32
BF16 = mybir.dt.bfloat16
ALU = mybir.AluOpType
AF = mybir.ActivationFunctionType
AX = mybir.AxisListType

S = 2048
D = 1024
NT = 16
DIL = (1, 4, 16)
NE = 32
EPS = 1e-6
STAGE = int(os.environ.get("KSTAGE", "99"))
NEXP = int(os.environ.get("KNEXP", "32"))
SPARSE = int(os.environ.get("KSPARSE", "1"))
BLK = 384
NBLK = (4 * 2048 + 32 * (BLK - 1)) // BLK
NQ = BLK // 128

ENGS = ("pe", "act", "dve", "pool", "sp")
N_DMA_SEMS = 28
N_DMA_SEMS_HW = 16


class Op:
    __slots__ = ("idx", "eng", "fn", "deps", "dma", "sem", "val", "signal")


class Prog:
    def __init__(self, nc):
        self.nc = nc
        self.ops = []
        self.last_w = {}
        self.readers = {}
        self.dma_rr = 0
        self.dma_rr_sw = 0
        self.dma_last = [None] * N_DMA_SEMS
        self.dma_cnt = [0] * N_DMA_SEMS
        self.barrier_deps = set()
        self.last_on_eng = {}

    def op(self, eng, fn, reads=(), writes=(), dma=False, accum=False):
        o = Op()
        o.idx = len(self.ops)
        o.eng = eng
        o.fn = fn
        o.dma = dma
        o.signal = False
        o.sem = None
        o.val = None
        deps = set(self.barrier_deps)
        for k in reads:
            w = self.last_w.get(k)
            if w is not None:
                deps.add(w)
        for k in writes:
            w = self.last_w.get(k)
            if w is not None:
                if not (accum and self.ops[w].eng == "pe" and eng == "pe"):
                    deps.add(w)
            for r in self.readers.get(k, ()):
                deps.add(r)
        if dma:
            if eng == "sp":
                s = self.dma_rr % N_DMA_SEMS_HW
                self.dma_rr += 1
            else:
                s = N_DMA_SEMS_HW + self.dma_rr_sw % (N_DMA_SEMS - N_DMA_SEMS_HW)
                self.dma_rr_sw += 1
            prev = self.dma_last[s]
            if prev is not None:
                deps.add(prev)
            self.dma_last[s] = o.idx
            self.dma_cnt[s] += 1
            o.sem = ("dma", s)
            o.val = 16 * self.dma_cnt[s]
            o.signal = True
        o.deps = deps
        for k in reads:
            self.readers.setdefault(k, []).append(o.idx)
        for k in writes:
            self.last_w[k] = o.idx
            self.readers[k] = []
        self.ops.append(o)
        self.last_on_eng[("dma", o.sem) if dma else eng] = o.idx
        return o

    def barrier(self):
        self.barrier_deps = set(self.last_on_eng.values())

    def emit(self, final_ops):
        nc = self.nc
        ops = self.ops
        for o in ops:
            for d in o.deps:
                ops[d].signal = True
        cnt = {e: 0 for e in ENGS}
        for o in ops:
            if o.dma:
                continue
            if o.signal:
                cnt[o.eng] += 1
                o.sem = ("eng", o.eng)
                o.val = cnt[o.eng]
        with contextlib.ExitStack() as st:
            sems = {}
            for e in ENGS:
                sems[("eng", e)] = st.enter_context(nc.semaphore("s_" + e))
            for i in range(N_DMA_SEMS):
                sems[("dma", i)] = st.enter_context(nc.semaphore("s_dma%d" % i))
            block = st.enter_context(nc.Block())
            by_eng = {e: [o for o in ops if o.eng == e] for e in ENGS}

            def run(eng_name, engine):
                seen = {}
                for o in by_eng[eng_name]:
                    need = {}
                    for d in o.deps:
                        od = ops[d]
                        if seen.get(od.sem, 0) >= od.val:
                            continue
                        if need.get(od.sem, 0) < od.val:
                            need[od.sem] = od.val
                    for s, v in need.items():
                        engine.wait_ge(sems[s], v)
                        seen[s] = v
                    ins = o.fn(engine)
                    if o.signal:
                        ins.then_inc(sems[o.sem], 16 if o.dma else 1)
                if eng_name == "sp":
                    for fo in final_ops:
                        engine.wait_ge(sems[fo.sem], fo.val)

            @block.tensor
            def _(e):
                run("pe", e)

            @block.scalar
            def _(e):
                run("act", e)

            @block.vector
            def _(e):
                run("dve", e)

            @block.gpsimd
            def _(e):
                run("pool", e)

            @block.sync
            def _(e):
                run("sp", e)


def _consts():
    c = {}
    c["ident"] = np.eye(128, dtype=np.float32)
    bo = np.zeros((128, 128), np.float32)
    bo[:64, :64] = 1.0
    bo[64:, 64:] = 1.0
    c["blockones"] = bo
    pos = np.arange(S, dtype=np.float32)
    inv = (np.float32(500000.0) ** (-(np.arange(8, dtype=np.float32) / np.float32(8)))).astype(np.float32)
    ang = (pos[:, None] * inv[None, :]).astype(np.float32)
    cosA = np.ones((128, S), np.float32)
    sinA = np.zeros((128, S), np.float32)
    RA = np.zeros((128, 128), np.float32)
    for hh in range(2):
        for dd in range(16):
            p = hh * 64 + dd
            cosA[p] = np.cos(ang[:, dd % 8])
            sinA[p] = np.sin(ang[:, dd % 8])
            if dd < 8:
                RA[p + 8, p] = -1.0
            else:
                RA[p - 8, p] = 1.0
    c["cosA"], c["sinA"], c["RA"] = cosA, sinA, RA
    row = (np.arange(S) // 64).astype(np.float32)
    col = (np.arange(S) % 64).astype(np.float32)
    invb = (np.float32(10000.0) ** (-(np.arange(16, dtype=np.float32) / np.float32(16)))).astype(np.float32)
    cosB = np.ones((128, S), np.float32)
    sinB = np.zeros((128, S), np.float32)
    RB = np.zeros((128, 128), np.float32)
    for hh in range(2):
        for dd in range(64):
            p = hh * 64 + dd
            ps_ = row if dd < 32 else col
            a = (ps_ * invb[dd % 16]).astype(np.float32)
            cosB[p] = np.cos(a)
            sinB[p] = np.sin(a)
            if (dd % 32) < 16:
                RB[p + 16, p] = -1.0
            else:
                RB[p - 16, p] = 1.0
    c["cosB"], c["sinB"], c["RB"] = cosB, sinB, RB
    a = np.arange(128)[:, None]
    b = np.arange(128)[None, :]
    m = np.zeros((128, 3, 128), np.float32)
    m[:, 0, :] = (a - b >= 64)
    m[:, 1, :] = (np.abs(a - b) <= 64)
    m[:, 2, :] = (b - a >= 64)
    c["utri"] = np.triu(np.ones((128, 128), np.float32), 1)
    c["blkthr"] = np.tile((np.arange(NBLK, dtype=np.float32) * BLK)[None, :], (128, 1))
    c["basekc"] = (np.arange(8)[None, :] * 128 + np.arange(128)[:, None]).astype(np.float32)
    c["maskA"] = ((1.0 - m) * -30000.0).astype(np.float32).reshape(128, 384)
    return c


def _relayout(inp):
    r = {}
    w_in = inp["w_in"]
    r["w_in"] = w_in
    r["gmix"] = np.ascontiguousarray(inp["norm_mix_g"].reshape(8, 128).T)
    r["gffn"] = np.ascontiguousarray(inp["norm_ffn_g"].reshape(8, 128).T)
    gains = np.zeros((128, 30), np.float32)
    for g in range(3):
        for hp in range(4):
            gains[:, g * 4 + hp] = np.tile(inp["qn_a"][g], 2)
            gains[:, 12 + g * 4 + hp] = np.tile(inp["kn_a"][g], 2)
    for c in range(4):
        gains[:, 24 + c] = np.tile(inp["qn_b"], 2)
    for g in range(2):
        gains[:, 28 + g] = np.tile(inp["kn_b"], 2)
    r["gains"] = gains
    g0c = 3 * 1536 + 512 + 256
    bun = np.empty((8, 128, 24, 128), np.float32)
    for m in range(8):
        cs = slice(m * 128, (m + 1) * 128)
        bun[m, :, 0:4] = inp["w_proj_a"][:, cs].reshape(4, 128, 128).transpose(1, 0, 2)
        bun[m, :, 4:8] = inp["w_proj_b"][:, cs].reshape(4, 128, 128).transpose(1, 0, 2)
        bun[m, :, 8:16] = w_in[:, g0c + m * 128: g0c + (m + 1) * 128].reshape(8, 128, 128).transpose(1, 0, 2)
        bun[m, :, 16:24] = w_in[:, g0c + 1024 + m * 128: g0c + 1024 + (m + 1) * 128].reshape(8, 128, 128).transpose(1, 0, 2)
    r["mbundle"] = bun
    r["bgate"] = np.ascontiguousarray(inp["b_gate"].reshape(2, 8, 128).transpose(2, 0, 1).reshape(128, 16))
    r["w_out"] = inp["w_out"]
    r["w_router"] = inp["w_router"]
    r["b_router"] = inp["b_router"].reshape(1, 32)
    r["w_gate_up"] = inp["w_gate_up"]
    r["w_down"] = inp["w_down"]
    r["bgu"] = np.ascontiguousarray(inp["b_gate_up"].reshape(32, 16, 128).transpose(0, 2, 1).reshape(4096, 16))
    r["b_down"] = inp["b_down"]
    return r


def build(dbg=None):
    nc = bass.Bass("TRN2", target_bir_lowering=False)

    def din(name, shape):
        return nc.dram_tensor(name, list(shape), F32, kind="ExternalInput").ap()

    x_d = din("x", [S, D])
    w_in_d = din("w_in", [D, 7424])
    gmix_d = din("gmix", [128, 8])
    gffn_d = din("gffn", [128, 8])
    gains_d = din("gains", [128, 30])
    mb_d = din("mbundle", [8, 128, 24, 128])
    bgate_d = din("bgate", [128, 16])
    w_out_d = din("w_out", [D, D])
    w_router_d = din("w_router", [D, 32])
    b_router_d = din("b_router", [1, 32])
    if STAGE > 4:
        wgu_d = din("w_gate_up", [NE, D, 2048])
        wd_d = din("w_down", [NE, D, D])
    bgu_d = din("bgu", [4096, 16])
    bd_d = din("b_down", [NE, D])
    cd = {k: din("c_" + k, v.shape) for k, v in _consts().items()}
    out_d = nc.dram_tensor("out", [S, D], F32, kind="ExternalOutput").ap()
    dbg_d = {}
    if dbg:
        for k, shp in dbg.items():
            dbg_d[k] = nc.dram_tensor("dbg_" + k, list(shp), F32, kind="ExternalOutput").ap()

    w_in_v = w_in_d.rearrange("(kc p) n -> p kc n", p=128)

    P = Prog(nc)
    final_ops = []
    with contextlib.ExitStack() as top:
        def sbt(stk, name, shape, dt):
            return stk.enter_context(nc.sbuf_tensor("sb_" + name, list(shape), dt))

        x1 = sbt(top, "x1", [128, NT, D], F32)
        hT = sbt(top, "hT", [128, 8, S], BF16)
        obuf = sbt(top, "obuf", [128, 8, S], BF16)
        ident = sbt(top, "ident", [128, 128], F32)
        blockones = sbt(top, "blockones", [128, 128], F32)
        ones_bf = sbt(top, "ones_bf", [128, 128], BF16)
        gmix = sbt(top, "gmix", [128, 8], F32)
        gffn = sbt(top, "gffn", [128, 8], F32)
        gains = sbt(top, "gains", [128, 30], F32)
        bgate = sbt(top, "bgate", [128, 16], F32)
        epst = sbt(top, "epst", [128, 1], F32)
        ssq = sbt(top, "ssq", [128, NT], F32)
        rstd = sbt(top, "rstd", [128, NT], F32)
        psum = [top.enter_context(nc.psum_tensor("ps%d" % i, [128, 512], F32)) for i in range(8)]

        def PS(i):
            return ("ps", i)

        def dma(out, in_, reads=(), writes=(), eng="sp"):
            return P.op(eng, lambda e: e.dma_start(out=out, in_=in_), reads=reads, writes=writes, dma=True)

        def dbg_out(name, ap, key):
            if name in dbg_d:
                final_ops.append(dma(dbg_d[name], ap, reads=[key]))

        x1f = x1[:].rearrange("p a b -> p (a b)")

        def scr(off, n):
            return x1f[:, off:off + n]

        xs_d = nc.dram_tensor("xsorted", [NBLK * BLK, D], BF16).ap()
        y_d = nc.dram_tensor("ysorted", [NBLK * BLK, D], F32).ap()
        zero_ops = []
        dma(ident[:], cd["ident"][:, :], writes=["ident"])
        dma(blockones[:], cd["blockones"][:, :], writes=["blockones"])
        dma(gmix[:], gmix_d[:, :], writes=["gmix"])
        dma(gffn[:], gffn_d[:, :], writes=["gffn"])
        dma(gains[:], gains_d[:, :], writes=["gains"])
        dma(bgate[:], bgate_d[:, :], writes=["bgate"])
        P.op("pool", lambda e: e.memset(epst[:], EPS), writes=["eps"])
        P.op("pool", lambda e: e.memset(ones_bf[:], 1.0), writes=["ones_bf"])

        def norm_transpose(tt, src_ap, src_key, hn, hn_key, junk, junk_key, dstT, dst_key, psb, extra32=None, scale_ap=None):
            P.op("act", lambda e: e.activation(out=junk, in_=src_ap, func=AF.Square, accum_out=ssq[:, tt:tt + 1]),
                 reads=[src_key], writes=[junk_key, ("ssq", tt)])
            P.op("act", lambda e: e.activation(out=rstd[:, tt:tt + 1], in_=ssq[:, tt:tt + 1], func=AF.Sqrt,
                                               bias=epst[:, 0:1], scale=1.0 / D), reads=[("ssq", tt), "eps"], writes=[("rstd", tt)])
            P.op("dve", lambda e: e.reciprocal(out=rstd[:, tt:tt + 1], in_=rstd[:, tt:tt + 1]), reads=[("rstd", tt)], writes=[("rstd", tt)])
            P.op("dve", lambda e: e.tensor_scalar(out=hn, in0=src_ap, scalar1=rstd[:, tt:tt + 1], scalar2=None, op0=ALU.mult),
                 reads=[src_key, ("rstd", tt)], writes=[hn_key])
            for half in range(2):
                b = psb[half]
                for q in range(4):
                    kc = half * 4 + q
                    P.op("pe", lambda e, kc=kc, q=q, b=b: e.transpose(out=psum[b][:, q * 128:(q + 1) * 128], in_=hn[:, kc * 128:(kc + 1) * 128], identity=ident[:]),
                         reads=[hn_key, "ident"], writes=[PS(b)], accum=(q > 0))
                if dstT is not None and scale_ap is not None:
                    for q in range(4):
                        kc = half * 4 + q
                        P.op("act", lambda e, kc=kc, q=q, b=b: e.activation(out=dstT[:, kc, tt * 128:(tt + 1) * 128], in_=psum[b][:, q * 128:(q + 1) * 128],
                                                                            func=AF.Copy, scale=scale_ap[:, kc:kc + 1]),
                             reads=[PS(b), "gmix"], writes=[PS(b), (dst_key, tt)])
                elif dstT is not None:
                    P.op("act", lambda e, half=half, b=b: e.copy(out=dstT[:, half * 4:half * 4 + 4, tt * 128:(tt + 1) * 128],
                                                                  in_=psum[b][:].rearrange("p (a c) -> p a c", a=4)),
                         reads=[PS(b)], writes=[PS(b), (dst_key, tt)])
                if extra32 is not None:
                    e32, e32_key = extra32
                    P.op("act", lambda e, half=half, b=b: e.copy(out=e32[:, half * 4:half * 4 + 4, :], in_=psum[b][:].rearrange("p (a c) -> p a c", a=4)),
                         reads=[PS(b)], writes=[PS(b), e32_key])

        for tt in range(NT):
            xs = scr((tt % 2) * 1024, 1024)
            hn = scr(2048 + (tt % 2) * 1024, 1024)
            jk = scr(4096, 1024)
            dma(xs, x_d[tt * 128:(tt + 1) * 128, :], writes=[("xs", tt % 2)])
            norm_transpose(tt, xs, ("xs", tt % 2), hn, ("hn", tt % 2), jk, "junk", hT, "hT", (6, 7), scale_ap=gmix)
        hT_keys = [("hT", tt) for tt in range(NT)]
        if STAGE == 0:
            tmp = scr(8192, 2048)
            P.op("dve", lambda e: e.tensor_copy(out=tmp, in_=hT[:, 0, :]), reads=hT_keys, writes=["dbgtmp"])
            dbg_out("hT0", tmp, "dbgtmp")
        P.barrier()

        def load_w_piece(stage, stage_key, wbf, wbf_key, col_specs, fold):
            for (d0, s0, n) in col_specs:
                P.op("pool", lambda e, d0=d0, s0=s0, n=n: e.dma_start(out=wbf[:, :, d0:d0 + n], in_=w_in_v[:, :, s0:s0 + n]), writes=[wbf_key], dma=True)

        class RopeCtx:
            pass

        def qk_chunk(rc, wbf, wbf_key, gain_col, dst, dst_key, dil):
            def proj(tc):
                pb = rc.ps_proj[tc % 2]
                for kc in range(8):
                    P.op("pe", lambda e, kc=kc, pb=pb, tc=tc: e.matmul(psum[pb][:], lhsT=wbf[:, kc, :], rhs=hT[:, kc, tc * 512:(tc + 1) * 512],
                                                                        start=(kc == 0), stop=(kc == 7)),
                         reads=[wbf_key] + hT_keys[tc * 4:tc * 4 + 4], writes=[PS(pb)], accum=(kc > 0))
                sset = tc % 2
                u, sq = rc.u[sset], rc.sq[sset]
                ku, ksq = ("u", sset), ("sq", sset)
                P.op("act", lambda e, pb=pb, u=u: e.activation(out=u, in_=psum[pb][:], func=AF.Copy, scale=gains[:, gain_col:gain_col + 1]),
                     reads=[PS(pb), "gains"], writes=[PS(pb), ku])
                P.op("act", lambda e, pb=pb, sq=sq: e.activation(out=sq, in_=psum[pb][:], func=AF.Square), reads=[PS(pb)], writes=[PS(pb), ksq])

            def aux(tc):
                sset = tc % 2
                u, sq, rs, t1 = rc.u[sset], rc.sq[sset], rc.rs[sset], rc.t1[sset]
                ku, ksq, krs, kt1 = ("u", sset), ("sq", sset), ("rs", sset), ("t1", sset)
                pa = rc.ps_aux[0]
                pr = rc.ps_aux[1]
                P.op("pe", lambda e, pa=pa, sq=sq: e.matmul(psum[pa][:], lhsT=blockones[:], rhs=sq, start=True, stop=True),
                     reads=["blockones", ksq], writes=[PS(pa)])
                P.op("pe", lambda e, pr=pr, u=u: e.matmul(psum[pr][:], lhsT=rc.R[:], rhs=u, start=True, stop=True),
                     reads=[rc.R_key, ku], writes=[PS(pr)])
                P.op("act", lambda e, pa=pa, rs=rs: e.activation(out=rs, in_=psum[pa][:], func=AF.Sqrt, bias=epst[:, 0:1], scale=1.0 / 64),
                     reads=[PS(pa), "eps"], writes=[PS(pa), krs])
                P.op("dve", lambda e, rs=rs: e.reciprocal(out=rs, in_=rs), reads=[krs], writes=[krs])
                P.op("pool", lambda e, u=u, t1=t1, tc=tc: e.tensor_tensor(out=t1, in0=u, in1=rc.cos[:, tc * 512:(tc + 1) * 512], op=ALU.mult),
                     reads=[ku, rc.tab_key], writes=[kt1])
                P.op("dve", lambda e, pr=pr, sq=sq, tc=tc: e.tensor_tensor(out=sq, in0=psum[pr][:], in1=rc.sin[:, tc * 512:(tc + 1) * 512], op=ALU.mult),
                     reads=[PS(pr), rc.tab_key, ksq], writes=[PS(pr), ksq])
                P.op("dve", lambda e, t1=t1, sq=sq: e.tensor_tensor(out=t1, in0=t1, in1=sq, op=ALU.add), reads=[kt1, ksq], writes=[kt1])
                n = 512 // dil
                dv = dst.rearrange("p (r m) -> p r m", r=dil)[:, :, tc * n:(tc + 1) * n]
                P.op("dve", lambda e, t1=t1, rs=rs, dv=dv: e.tensor_tensor(out=dv, in0=t1.rearrange("p (m r) -> p r m", r=dil),
                                                                           in1=rs.rearrange("p (m r) -> p r m", r=dil), op=ALU.mult),
                     reads=[kt1, krs], writes=[dst_key])

            proj(0)
            for tc in range(4):
                if tc + 1 < 4:
                    proj(tc + 1)
                aux(tc)

        def v_tiles(wbf, wbf_key, vdst, v_key, dil, pbanks, evac=None):
            nb = (S // dil) // 128
            hv = hT[:].rearrange("p k (m r) -> p k r m", r=dil)
            for tg in range(4):
                pb = pbanks[tg % 2]
                for q in range(4):
                    ti = tg * 4 + q
                    r_, j = ti // nb, ti % nb
                    for kc in range(8):
                        P.op("pe", lambda e, kc=kc, q=q, r_=r_, j=j, pb=pb: e.matmul(psum[pb][:, q * 128:(q + 1) * 128], lhsT=hv[:, kc, r_, j * 128:(j + 1) * 128],
                                                                                     rhs=wbf[:, kc, :], start=(kc == 0), stop=(kc == 7)),
                             reads=[wbf_key] + hT_keys, writes=[PS(pb)], accum=not (q == 0 and kc == 0))
                if evac is not None:
                    evac(tg, pb)
                    continue
                P.op("act", lambda e, tg=tg, pb=pb: e.copy(out=vdst[:, tg * 4:tg * 4 + 4, :], in_=psum[pb][:].rearrange("p (a c) -> p a c", a=4)),
                     reads=[PS(pb)], writes=[PS(pb), v_key])

        o_aT = obuf[:, 0:4, :]
        o_bT = obuf[:, 4:8, :]

        with contextlib.ExitStack() as sa:
            rc = RopeCtx()
            rc.cos = scr(0, 2048)
            rc.sin = scr(2048, 2048)
            accn = scr(4096, 2048)
            accd = scr(6144, 2048)
            rc.u = [scr(8192 + i * 512, 512) for i in range(2)]
            rc.sq = [scr(9216 + i * 512, 512) for i in range(2)]
            rc.rs = [scr(10240 + i * 512, 512) for i in range(2)]
            rc.t1 = [scr(11264 + i * 512, 512) for i in range(2)]
            wst = [scr(12288 + i * 1024, 1024).rearrange("p (k c) -> p k c", k=8) for i in range(3)]
            rc.R = sbt(sa, "RA", [128, 128], F32)
            rc.R_key = "R"
            rc.tab_key = "tab"
            rc.ps_proj = (0, 1)
            rc.ps_aux = (2, 3)
            maskA = sbt(sa, "maskA", [128, 384], BF16)
            mstage = scr(15360, 384)
            wbf = [sbt(sa, "wbfA%d" % i, [128, 8, 128], BF16) for i in range(6)]
            qT = [sbt(sa, "qT%d" % i, [128, S], BF16) for i in range(2)]
            kT = [sbt(sa, "kT%d" % i, [128, S], BF16) for i in range(2)]
            vA = [[sbt(sa, "vA%d_%d" % (i, hh), [128, 16, 128], BF16) for hh in range(2)] for i in range(2)]
            pT = [sbt(sa, "pTA%d" % i, [128, 384], BF16) for i in range(4)]
            ident_bf = sbt(sa, "ident_bf", [128, 128], BF16)
            P.op("pool", lambda e: e.tensor_copy(out=ident_bf[:], in_=ident[:]), reads=["ident"], writes=["ident_bf"])
            for i in range(2):
                for hh in range(2):
                    P.op("pool", lambda e, i=i, hh=hh: e.memset(vA[i][hh][:], 1.0), writes=[("vA", i)])
            dma(rc.cos, cd["cosA"][:, :], writes=["tab"])
            dma(rc.sin, cd["sinA"][:, :], writes=["tab"])
            dma(rc.R[:], cd["RA"][:, :], writes=["R"])
            dma(mstage, cd["maskA"][:, :], writes=["mstage"])
            P.op("pool", lambda e: e.tensor_copy(out=maskA[:], in_=mstage), reads=["mstage"], writes=["maskA"])
            it = 0
            blk = 0
            itersA = [(hp, g) for hp in range(4) for g in range(3)]

            def loadsA(ix):
                hp_, g_ = itersA[ix]
                o3 = 3 * (ix % 2)
                for j, c0 in enumerate((g_ * 512 + hp_ * 128, 1536 + g_ * 512 + hp_ * 128, 3072 + g_ * 512 + hp_ * 128)):
                    load_w_piece(None, None, wbf[o3 + j][:], ("wbf", o3 + j), [(0, c0, 128)], None)
            loadsA(0)
            for hp in range(4):
                for g in range(3):
                    dil = DIL[g]
                    nb = (S // dil) // 128
                    buf = it % 2
                    if it + 1 < len(itersA):
                        loadsA(it + 1)
                    o3 = 3 * (it % 2)
                    it += 1
                    qk_chunk(rc, wbf[o3 + 0], ("wbf", o3 + 0), g * 4 + hp, qT[buf][:], ("qT", buf), dil)
                    qk_chunk(rc, wbf[o3 + 1], ("wbf", o3 + 1), 12 + g * 4 + hp, kT[buf][:], ("kT", buf), dil)
                    def evacA(tg, pb, buf=buf):
                        for hh in range(2):
                            P.op("act", lambda e, tg=tg, pb=pb, hh=hh, buf=buf: e.copy(out=vA[buf][hh][:, tg * 4:tg * 4 + 4, 64 * hh:64 * hh + 64],
                                                                                      in_=psum[pb][:].rearrange("p (a c) -> p a c", a=4)[:, :, 64 * hh:64 * hh + 64]),
                                 reads=[PS(pb)], writes=[PS(pb), ("vA", buf)])
                    v_tiles(wbf[o3 + 2], ("wbf", o3 + 2), None, ("vA", buf), dil, (0, 1), evac=evacA)
                    if STAGE == 1 and hp == 0:
                        tmp = scr(4096, 1024)
                        for nm, src_, key in (("qT_%d" % g, qT[buf], ("qT", buf)), ("kT_%d" % g, kT[buf], ("kT", buf))):
                            for hf in range(2):
                                P.op("dve", lambda e, src_=src_, hf=hf: e.tensor_copy(out=tmp, in_=src_[:, hf * 1024:(hf + 1) * 1024]), reads=[key], writes=["dbgtmp"])
                                if nm in dbg_d:
                                    final_ops.append(dma(dbg_d[nm][:, hf * 1024:(hf + 1) * 1024], tmp, reads=["dbgtmp"]))
                    if STAGE == 1:
                        continue
                    blocks = [(hh, bi) for hh in range(2) for bi in range(16)]

                    def s_block(hh, bi, blk_):
                        hs = slice(64 * hh, 64 * hh + 64)
                        r_, i = bi // nb, bi % nb
                        sb_ = (4, 5, 3)[blk_ % 3]
                        pbuf = blk_ % 4
                        js = [j for j in (i - 1, i, i + 1) if 0 <= j < nb]
                        qcols = slice(r_ * (S // dil) + i * 128, r_ * (S // dil) + (i + 1) * 128)
                        for j in js:
                            sl = j - i + 1
                            kcols = slice(r_ * (S // dil) + j * 128, r_ * (S // dil) + (j + 1) * 128)
                            P.op("pe", lambda e, sb_=sb_, sl=sl, kcols=kcols, qcols=qcols, hs=hs, buf=buf: e.matmul(
                                psum[sb_][:, sl * 128:(sl + 1) * 128], lhsT=kT[buf][hs, kcols], rhs=qT[buf][hs, qcols], start=True, stop=False),
                                reads=[("kT", buf), ("qT", buf)], writes=[PS(sb_)], accum=(j != js[0]))
                            P.op("pe", lambda e, sb_=sb_, sl=sl: e.matmul(
                                psum[sb_][:, sl * 128:(sl + 1) * 128], lhsT=ident_bf[:], rhs=maskA[:, sl * 128:(sl + 1) * 128], start=False, stop=True),
                                reads=["ident_bf", "maskA"], writes=[PS(sb_)], accum=True)
                        c0, c1 = (js[0] - i + 1) * 128, (js[-1] - i + 2) * 128
                        P.op("act", lambda e, sb_=sb_, pbuf=pbuf, c0=c0, c1=c1: e.activation(out=pT[pbuf][:, c0:c1], in_=psum[sb_][:, c0:c1], func=AF.Exp, scale=0.125),
                             reads=[PS(sb_)], writes=[PS(sb_), ("pT", pbuf)])

                    def pv_block(hh, bi, blk_):
                        hs = slice(64 * hh, 64 * hh + 64)
                        os_ = slice(64 * (1 - hh), 64 * (1 - hh) + 64)
                        r_, i = bi // nb, bi % nb
                        pbuf = blk_ % 4
                        js = [j for j in (i - 1, i, i + 1) if 0 <= j < nb]
                        pn = (6, 7)[(bi // 4) % 2]
                        oc = slice((bi % 4) * 128, (bi % 4 + 1) * 128)
                        for n_, j in enumerate(js):
                            sl = j - i + 1
                            vt = r_ * nb + j
                            first = (bi % 4 == 0 and n_ == 0)
                            P.op("pe", lambda e, pn=pn, oc=oc, vt=vt, sl=sl, pbuf=pbuf, buf=buf, hh=hh, n_=n_, nl=len(js) - 1: e.matmul(
                                psum[pn][:, oc], lhsT=vA[buf][hh][:, vt, :], rhs=pT[pbuf][:, sl * 128:(sl + 1) * 128], start=(n_ == 0), stop=(n_ == nl)),
                                reads=[("vA", buf), ("pT", pbuf)], writes=[PS(pn)], accum=not first)
                        if bi % 4 == 3:
                            b4 = bi // 4
                            if nb >= 4:
                                r0, nr, i0, ni = (4 * b4) // nb, 1, (4 * b4) % nb, 4
                            else:
                                r0, nr, i0, ni = 4 * b4, 4, 0, 1
                            for (acc, ps_rows, kk) in ((accn, hs, "accn"), (accd, os_, "accd")):
                                av = acc.rearrange("p (m r) -> p r m", r=dil)[hs, r0:r0 + nr, i0 * 128:(i0 + ni) * 128]
                                pv = psum[pn][ps_rows, :].rearrange("p (a c) -> p a c", a=nr)
                                if g == 0:
                                    P.op("dve", lambda e, av=av, pv=pv: e.tensor_copy(out=av, in_=pv), reads=[PS(pn)], writes=[PS(pn), (kk, hh)])
                                else:
                                    P.op("dve", lambda e, av=av, pv=pv: e.tensor_tensor(out=av, in0=pv, in1=av, op=ALU.add),
                                         reads=[PS(pn), (kk, hh)], writes=[PS(pn), (kk, hh)])

                    DEPTH = 2
                    for bx in range(min(DEPTH, len(blocks))):
                        s_block(blocks[bx][0], blocks[bx][1], blk + bx)
                    for bx, (hh, bi) in enumerate(blocks):
                        if bx + DEPTH < len(blocks):
                            s_block(blocks[bx + DEPTH][0], blocks[bx + DEPTH][1], blk + DEPTH)
                        pv_block(hh, bi, blk)
                        blk += 1
                if STAGE == 1:
                    continue
                P.op("dve", lambda e: e.reciprocal(out=accd, in_=accd), reads=[("accd", 0), ("accd", 1)], writes=[("accd", 0), ("accd", 1)])
                P.op("dve", lambda e, hp=hp: e.tensor_tensor(out=o_aT[:, hp, :], in0=accn, in1=accd, op=ALU.mult),
                     reads=[("accn", 0), ("accn", 1), ("accd", 0), ("accd", 1)], writes=[("o_aT", hp)])
            if STAGE == 2:
                tmp = scr(8192, 2048)
                for hp in range(4):
                    P.op("dve", lambda e, hp=hp: e.tensor_copy(out=tmp, in_=o_aT[:, hp, :]), reads=[("o_aT", hp)], writes=["dbgtmp"])
                    if "o_aT" in dbg_d:
                        final_ops.append(dma(dbg_d["o_aT"][hp], tmp, reads=["dbgtmp"]))
        P.barrier()

        if STAGE <= 2:
            P.emit(final_ops)
            return nc

        with contextlib.ExitStack() as sbk:
            rc = RopeCtx()
            rc.cos = scr(0, 2048)
            rc.sin = scr(2048, 2048)
            rden = [scr(4096 + i * 512, 512) for i in range(2)]
            rc.u = [scr(8192 + i * 512, 512) for i in range(2)]
            rc.sq = [scr(9216 + i * 512, 512) for i in range(2)]
            rc.rs = [scr(10240 + i * 512, 512) for i in range(2)]
            rc.t1 = [scr(11264 + i * 512, 512) for i in range(2)]
            wst = [scr(12288 + i * 1024, 1024).rearrange("p (k c) -> p k c", k=8) for i in range(2)]
            rc.R = sbt(sbk, "RB", [128, 128], F32)
            rc.R_key = "RBk"
            rc.tab_key = "tabB"
            rc.ps_proj = (0, 1)
            rc.ps_aux = (2, 3)
            wbf = [sbt(sbk, "wbfB%d" % i, [128, 8, 128], BF16) for i in range(3)]
            qbT = sbt(sbk, "qbT", [128, 4, S], BF16)
            kbd = [sbt(sbk, "kbd%d" % g, [128, S], BF16) for g in range(2)]
            vbd = [sbt(sbk, "vbd%d" % g, [128, 16, 128], BF16) for g in range(2)]
            vb1 = [[sbt(sbk, "vb1_%d_%d" % (g, hh), [128, 16, 128], BF16) for hh in range(2)] for g in range(2)]
            pTB = [sbt(sbk, "pTB%d" % i, [128, 512], BF16) for i in range(3)]
            dma(rc.cos, cd["cosB"][:, :], writes=["tabB"])
            dma(rc.sin, cd["sinB"][:, :], writes=["tabB"])
            dma(rc.R[:], cd["RB"][:, :], writes=["RBk"])
            cqb, ckb, cvb = 4608, 5120, 5248
            piecesB = [[(0, cqb + c * 128, 128)] for c in range(4)]
            for g in range(2):
                piecesB.append([(0, ckb + g * 64, 64), (64, ckb + g * 64, 64)])
                piecesB.append([(0, cvb + g * 64, 64), (64, cvb + g * 64, 64)])

            def loadB(j):
                load_w_piece(None, None, wbf[j % 3][:], ("wbfB", j % 3), piecesB[j], None)
            loadB(0)
            loadB(1)
            for c in range(4):
                if c + 2 < len(piecesB):
                    loadB(c + 2)
                qk_chunk(rc, wbf[c % 3], ("wbfB", c % 3), 24 + c, qbT[:, c, :], ("qbT", c), 1)
            for g in range(2):
                j = 4 + 2 * g
                if j + 2 < len(piecesB):
                    loadB(j + 2)
                qk_chunk(rc, wbf[j % 3], ("wbfB", j % 3), 28 + g, kbd[g][:], ("kbd", g), 1)
                if j + 3 < len(piecesB):
                    loadB(j + 3)
                v_tiles(wbf[(j + 1) % 3], ("wbfB", (j + 1) % 3), vbd[g], ("vbd", g), 1, (0, 1))
                for hh in range(2):
                    oh = 1 - hh
                    P.op("pool", lambda e, g=g, hh=hh, oh=oh: e.memset(vb1[g][hh][:, :, 64 * oh:64 * oh + 64], 1.0), writes=[("vb1", g, hh)])
                    P.op("pool", lambda e, g=g, hh=hh: e.tensor_copy(out=vb1[g][hh][:, :, 64 * hh:64 * hh + 64], in_=vbd[g][:, :, 64 * hh:64 * hh + 64]),
                         reads=[("vbd", g), ("vb1", g, hh)], writes=[("vb1", g, hh)])
            zt = scr(12288, 512).bitcast(BF16)
            P.op("pool", lambda e: e.memset(zt, 0.0), writes=["zt"])
            xs_flat = xs_d.rearrange("(p r) d -> p (r d)", p=128)
            ZR = NQ * 2 if (NBLK * BLK // 128) % (NQ * 2) == 0 else NQ
            assert (NBLK * BLK // 128) % ZR == 0
            for i in range(NBLK * BLK // 128 // ZR):
                zero_ops.append(P.op("pool", lambda e, i=i: e.dma_start(out=xs_flat[:, i * ZR * 1024:(i + 1) * ZR * 1024].rearrange("p (a c) -> p a c", a=ZR),
                                                                        in_=zt.unsqueeze(1).broadcast_to([128, ZR, 1024])), reads=["zt"], writes=[("xs0", i)], dma=True))
            tasks = [(h, qc, kt) for h in range(8) for qc in range(4) for kt in range(16)]

            def s_task(ix):
                h, qc, kt = tasks[ix]
                c, hh, g = h // 2, h % 2, h // 4
                hs = slice(64 * hh, 64 * hh + 64)
                qs = slice(qc * 512, (qc + 1) * 512)
                sb_ = 4 + ix % 2
                pb = ix % 3
                P.op("pe", lambda e, sb_=sb_, g=g, hs=hs, kt=kt, c=c, qs=qs: e.matmul(psum[sb_][:], lhsT=kbd[g][hs, kt * 128:(kt + 1) * 128], rhs=qbT[hs, c, qs], start=True, stop=True),
                     reads=[("kbd", g), ("qbT", c)], writes=[PS(sb_)])
                P.op("act", lambda e, sb_=sb_, pb=pb: e.activation(out=pTB[pb][:], in_=psum[sb_][:], func=AF.Exp, scale=0.125),
                     reads=[PS(sb_)], writes=[PS(sb_), ("pTB", pb)])

            def pv_task(ix):
                h, qc, kt = tasks[ix]
                c, hh, g = h // 2, h % 2, h // 4
                hs = slice(64 * hh, 64 * hh + 64)
                os_ = slice(64 * (1 - hh), 64 * (1 - hh) + 64)
                qs = slice(qc * 512, (qc + 1) * 512)
                par = (h * 4 + qc) % 2
                pn = (6, 7)[par]
                pb = ix % 3
                P.op("pe", lambda e, pn=pn, g=g, hh=hh, kt=kt, pb=pb: e.matmul(psum[pn][:], lhsT=vb1[g][hh][:, kt, :], rhs=pTB[pb][:], start=(kt == 0), stop=(kt == 15)),
                     reads=[("vb1", g, hh), ("pTB", pb)], writes=[PS(pn)], accum=(kt > 0))
                if kt == 15:
                    rd = rden[par]
                    P.op("dve", lambda e, rd=rd, hs=hs, os_=os_, pn=pn: e.reciprocal(out=rd[hs, :], in_=psum[pn][os_, :]), reads=[PS(pn)], writes=[PS(pn), ("rden", par)])
                    P.op("dve", lambda e, rd=rd, hs=hs, pn=pn, c=c, qs=qs: e.tensor_tensor(out=o_bT[hs, c, qs], in0=psum[pn][hs, :], in1=rd[hs, :], op=ALU.mult),
                         reads=[PS(pn), ("rden", par)], writes=[PS(pn), ("o_bT", c)])

            s_task(0)
            for ix in range(len(tasks)):
                if ix + 1 < len(tasks):
                    s_task(ix + 1)
                pv_task(ix)
            if STAGE == 3:
                tmp = scr(8192, 2048)
                for c in range(4):
                    P.op("dve", lambda e, c=c: e.tensor_copy(out=tmp, in_=o_bT[:, c, :]), reads=[("o_bT", c)], writes=["dbgtmp"])
                    if "o_bT" in dbg_d:
                        final_ops.append(dma(dbg_d["o_bT"][c], tmp, reads=["dbgtmp"]))
        P.barrier()
        if STAGE == 3:
            P.emit(final_ops)
            return nc

        o_keys_a = [("o_aT", c) for c in range(4)]
        o_keys_b = [("o_bT", c) for c in range(4)]
        with contextlib.ExitStack() as sm:
            bbf2 = [sbt(sm, "bbf%d" % i, [128, 24, 128], BF16) for i in range(2)]
            woutbf = sbt(sm, "woutbf", [128, 8, D], BF16)
            mergedT = sbt(sm, "mergedT", [128, 8, S], BF16)
            sg0 = [sbt(sm, "sg0_%d" % i, [128, 512], F32) for i in range(2)]
            sg1 = [sbt(sm, "sg1_%d" % i, [128, 512], F32) for i in range(2)]
            for tt in range(NT):
                dma(x1[:, tt, :], x_d[tt * 128:(tt + 1) * 128, :], writes=[("x1", tt)])
            w_out_v = w_out_d.rearrange("(kc p) n -> p kc n", p=128)
            for q in range(4):
                P.op("pool", lambda e, q=q: e.dma_start(out=woutbf[:, 2 * q:2 * q + 2, :], in_=w_out_v[:, 2 * q:2 * q + 2, :]), writes=[("woutbf", q)], dma=True)
            it = 0

            def loadM(m_):
                P.op("pool", lambda e: e.dma_start(out=bbf2[m_ % 2][:], in_=mb_d[m_]), writes=[("bbf", m_ % 2)], dma=True)
            loadM(0)
            for m in range(8):
                bbf = bbf2[m % 2]
                kbb = ("bbf", m % 2)
                for tc in range(4):
                    if tc == 1 and m + 1 < 8:
                        loadM(m + 1)
                    ts_ = slice(tc * 512, (tc + 1) * 512)
                    par = it % 2
                    it += 1
                    bA, bB, bG0, bG1 = (0, 1, 2, 3) if par == 0 else (4, 5, 6, 7)
                    for c in range(4):
                        P.op("pe", lambda e, c=c, bA=bA, ts_=ts_, bbf=bbf: e.matmul(psum[bA][:], lhsT=bbf[:, c, :], rhs=o_aT[:, c, ts_], start=(c == 0), stop=(c == 3)),
                             reads=[kbb] + o_keys_a, writes=[PS(bA)], accum=(c > 0))
                    for c in range(4):
                        P.op("pe", lambda e, c=c, bB=bB, ts_=ts_, bbf=bbf: e.matmul(psum[bB][:], lhsT=bbf[:, 4 + c, :], rhs=o_bT[:, c, ts_], start=(c == 0), stop=(c == 3)),
                             reads=[kbb] + o_keys_b, writes=[PS(bB)], accum=(c > 0))
                    for kc in range(8):
                        P.op("pe", lambda e, kc=kc, bG0=bG0, ts_=ts_, bbf=bbf: e.matmul(psum[bG0][:], lhsT=bbf[:, 8 + kc, :], rhs=hT[:, kc, ts_], start=(kc == 0), stop=(kc == 7)),
                             reads=[kbb] + hT_keys, writes=[PS(bG0)], accum=(kc > 0))
                    for kc in range(8):
                        P.op("pe", lambda e, kc=kc, bG1=bG1, ts_=ts_, bbf=bbf: e.matmul(psum[bG1][:], lhsT=bbf[:, 16 + kc, :], rhs=hT[:, kc, ts_], start=(kc == 0), stop=(kc == 7)),
                             reads=[kbb] + hT_keys, writes=[PS(bG1)], accum=(kc > 0))
                    P.op("act", lambda e, m=m, bG0=bG0, par=par: e.activation(out=sg0[par][:], in_=psum[bG0][:], func=AF.Sigmoid, bias=bgate[:, m:m + 1]),
                         reads=[PS(bG0), "bgate"], writes=[PS(bG0), ("sg0", par)])
                    P.op("act", lambda e, m=m, bG1=bG1, par=par: e.activation(out=sg1[par][:], in_=psum[bG1][:], func=AF.Sigmoid, bias=bgate[:, 8 + m:9 + m]),
                         reads=[PS(bG1), "bgate"], writes=[PS(bG1), ("sg1", par)])
                    P.op("dve", lambda e, bA=bA, par=par: e.tensor_tensor(out=sg0[par][:], in0=psum[bA][:], in1=sg0[par][:], op=ALU.mult),
                         reads=[PS(bA), ("sg0", par)], writes=[PS(bA), ("sg0", par)])
                    P.op("dve", lambda e, bB=bB, par=par: e.tensor_tensor(out=sg1[par][:], in0=psum[bB][:], in1=sg1[par][:], op=ALU.mult),
                         reads=[PS(bB), ("sg1", par)], writes=[PS(bB), ("sg1", par)])
                    P.op("pool", lambda e, m=m, par=par, ts_=ts_: e.tensor_tensor(out=mergedT[:, m, ts_], in0=sg0[par][:], in1=sg1[par][:], op=ALU.add),
                         reads=[("sg0", par), ("sg1", par)], writes=[("mergedT", m, tc)])
            for tt in range(NT):
                for nh in range(2):
                    b = (0, 1, 4, 5)[(tt * 2 + nh) % 4]
                    ns = slice(nh * 512, (nh + 1) * 512)
                    for m in range(8):
                        P.op("pe", lambda e, m=m, b=b, tt=tt, ns=ns: e.matmul(psum[b][:], lhsT=mergedT[:, m, tt * 128:(tt + 1) * 128], rhs=woutbf[:, m, ns], start=(m == 0), stop=(m == 7)),
                             reads=[("mergedT", m, tt // 4), ("woutbf", m // 2)], writes=[PS(b)], accum=(m > 0))
                    P.op("dve", lambda e, b=b, tt=tt, ns=ns: e.tensor_tensor(out=x1[:, tt, ns], in0=psum[b][:], in1=x1[:, tt, ns], op=ALU.add),
                         reads=[PS(b), ("x1", tt)], writes=[PS(b), ("x1", tt)])
        x1_keys = [("x1", tt) for tt in range(NT)]
        if STAGE == 4:
            for tt in range(NT):
                if "x1" in dbg_d:
                    final_ops.append(dma(dbg_d["x1"][tt * 128:(tt + 1) * 128, :], x1[:, tt, :], reads=[("x1", tt)]))
            P.emit(final_ops)
            return nc
        P.barrier()

        I32 = mybir.dt.int32
        wgu_rows = wgu_d.rearrange("e k n -> (e k) n")
        wd_rows = wd_d.rearrange("e k n -> (e k) n")
        IOA = bass.IndirectOffsetOnAxis

        def idma(out, out_off, in_, in_off, bound, reads=(), writes=()):
            def f(e):
                if bound is None:
                    return e.indirect_dma_start(out=out, out_offset=out_off, in_=in_, in_offset=in_off)
                return e.indirect_dma_start(out=out, out_offset=out_off, in_=in_, in_offset=in_off, bounds_check=pregs[bound], oob_is_err=False)
            return P.op("pool", f, reads=reads, writes=writes, dma=True)

        pregs = {}

        def mkreg(name, val):
            def f(e):
                pregs[name] = e.alloc_register(name)
                return e.reg_mov(pregs[name], val)
            P.op("pool", f)
        mkreg("bw", NE * D - 1)
        mkreg("bb", NE * 128 - 1)

        with contextlib.ExitStack() as se:
            print("SBUF remaining before moe scope", nc.sbuf_bytes_remaining)
            wgubf = obuf
            wdbf = sbt(se, "wdbf", [128, 8, D], BF16)
            gwk = sbt(se, "gwk", [128, NT, 4], F32)
            desti = sbt(se, "desti", [128, NT * 4], I32)
            be = sbt(se, "be", [128, NBLK], F32)
            widx = sbt(se, "widx", [128, NBLK, 8], I32)
            bidx = sbt(se, "bidx", [128, NBLK], I32)
            h2tok = hT[:].rearrange("p k s -> p (k s)").rearrange("p (t f) -> p t f", t=NT)
            with contextlib.ExitStack() as sr:
                stg = [sbt(sr, "stg%d" % i, [128, 2048], F32) for i in range(2)]
                hn2s = [stg[i][:, 0:1024] for i in range(2)]
                h32s = [stg[i][:, 1024:2048].rearrange("p (k c) -> p k c", k=8) for i in range(2)]
                wr = sbt(sr, "wr", [128, 8, 32], F32)
                wrf = sbt(sr, "wrf", [128, 8, 32], F32)
                brt = sbt(sr, "brt", [128, 32], F32)
                tiny = []
                for nm_, shp_, dt_ in (("logit", [128, 32], F32), ("mx8", [128, 8], F32), ("negmax", [128, 1], F32), ("msk", [128, 32], F32), ("ex", [128, 32], F32),
                                       ("ssum", [128, 1], F32), ("gw", [128, 32], F32), ("gwT", [32, 128], F32), ("mskb", [128, 32], BF16), ("e4", [128, 4], F32), ("s4", [128, 1], F32)):
                    tiny.append([sbt(sr, "%s_%d" % (nm_, i), shp_, dt_) for i in range(2)])
                bd32 = sbt(sr, "bd32", [32, D], F32)
                utri = sbt(sr, "utri", [128, 128], BF16)
                basekc = sbt(sr, "basekc", [128, 8], F32)
                cum = sbt(sr, "cum", [128, 32], F32)
                rank_all = sbt(sr, "rank_all", [128, NT, 32], F32)
                logit_all = sbt(sr, "logit_all", [128, NT, 32], F32)
                mx8_all = sbt(sr, "mx8_all", [128, NT, 8], F32)
                pada = sbt(sr, "pada", [128, 32], F32)
                padb = sbt(sr, "padb", [128, 32], F32)
                padded = sbt(sr, "padded", [128, 32], F32)
                pstart = sbt(sr, "pstart", [128, 32], F32)
                dest_all = sbt(sr, "dest_all", [128, NT, 32], F32)
                junk32 = sbt(sr, "junk32", [128, 32], F32)
                destk = sbt(sr, "destk", [128, NT * 4], F32)
                widf = sbt(sr, "widf", [128, NBLK, 8], F32)
                bidf = sbt(sr, "bidf", [128, NBLK], F32)
                dma(wr[:], w_router_d.rearrange("(kc p) n -> p kc n", p=128), writes=["wr"])
                dma(brt[:], b_router_d[0:1, :].broadcast_to([128, 32]), writes=["brt"])
                dma(bd32[:], bd_d[:, :], writes=["bd32"])
                dma(basekc[:], cd["basekc"][:, :], writes=["basekc"])
                blkthr = sbt(sr, "blkthr", [128, NBLK], F32)
                dma(blkthr[:], cd["blkthr"][:, :], writes=["blkthr"])
                dma(stg[1][:, 1024:1152], cd["utri"][:, :], writes=["h32b"])
                P.op("pool", lambda e: e.tensor_copy(out=utri[:], in_=stg[1][:, 1024:1152]), reads=["h32b"], writes=["utri"])
                P.op("pool", lambda e: e.memset(cum[:], 0.0), writes=["cum"])
                P.op("pool", lambda e: e.tensor_tensor(out=wrf[:], in0=wr[:], in1=gffn[:, 0:8].unsqueeze(2).broadcast_to([128, 8, 32]), op=ALU.mult),
                     reads=["wr", "gffn"], writes=["wrf"])
                def route_tile(tt):
                    par = tt % 2
                    hn2, junk2, h32 = hn2s[par], h32s[par].rearrange("p k c -> p (k c)"), h32s[par]
                    khn, kh32 = ("hn2a", "hn2b")[par], ("h32a", "h32b")[par]
                    logit, mx8, negmax, msk, ex, ssum, gw, gwT, mskb, e4, s4 = [t_[par] for t_ in tiny]
                    kk = lambda n: (n, par)
                    pl, pg = (5, 4) if par == 0 else (1, 0)
                    norm_transpose(tt, x1[:, tt, :], ("x1", tt), hn2, khn, junk2, kh32, None, None, (6, 7), extra32=(h32, kh32))
                    P.op("act", lambda e, tt=tt: e.copy(out=h2tok[:, tt, :], in_=hn2), reads=[khn], writes=[("h2tok", tt)])
                    for kc in range(8):
                        P.op("pe", lambda e, kc=kc: e.matmul(psum[pl][:, 0:32], lhsT=h32[:, kc, :], rhs=wrf[:, kc, :], start=(kc == 0), stop=(kc == 7)),
                             reads=[kh32, "wrf"], writes=[PS(pl)], accum=(kc > 0))
                    P.op("dve", lambda e: e.tensor_tensor(out=logit[:], in0=psum[pl][:, 0:32], in1=brt[:], op=ALU.add), reads=[PS(pl), "brt"], writes=[PS(pl), kk("logit")])
                    P.op("dve", lambda e: e.max(out=mx8[:], in_=logit[:]), reads=[kk("logit")], writes=[kk("mx8")])
                    P.op("dve", lambda e: e.tensor_scalar(out=msk[:], in0=logit[:], scalar1=mx8[:, 3:4], scalar2=None, op0=ALU.is_ge), reads=[kk("logit"), kk("mx8")], writes=[kk("msk")])
                    P.op("dve", lambda e: e.tensor_scalar(out=negmax[:], in0=mx8[:, 0:1], scalar1=-1.0, scalar2=None, op0=ALU.mult), reads=[kk("mx8")], writes=[kk("negmax")])
                    P.op("act", lambda e: e.activation(out=ex[:], in_=logit[:], func=AF.Exp, bias=negmax[:, 0:1], scale=1.0), reads=[kk("logit"), kk("negmax")], writes=[kk("ex")])
                    P.op("dve", lambda e: e.tensor_tensor(out=ex[:], in0=ex[:], in1=msk[:], op=ALU.mult), reads=[kk("ex"), kk("msk")], writes=[kk("ex")])
                    P.op("dve", lambda e: e.reduce_sum(out=ssum[:], in_=ex[:], axis=AX.X), reads=[kk("ex")], writes=[kk("ssum")])
                    P.op("dve", lambda e: e.reciprocal(out=ssum[:], in_=ssum[:]), reads=[kk("ssum")], writes=[kk("ssum")])
                    P.op("dve", lambda e: e.tensor_scalar(out=gw[:], in0=ex[:], scalar1=ssum[:, 0:1], scalar2=None, op0=ALU.mult), reads=[kk("ex"), kk("ssum")], writes=[kk("gw")])
                    P.op("pool", lambda e, tt=tt: e.tensor_copy(out=logit_all[:, tt, :], in_=logit[:]), reads=[kk("logit")], writes=[("logit_all", tt)])
                    P.op("pool", lambda e, tt=tt: e.tensor_copy(out=mx8_all[:, tt, :], in_=mx8[:]), reads=[kk("mx8")], writes=[("mx8_all", tt)])
                    P.op("pool", lambda e: e.tensor_copy(out=mskb[:], in_=msk[:]), reads=[kk("msk")], writes=[kk("mskb")])
                    P.op("act", lambda e: e.activation(out=e4[:], in_=mx8[:, 0:4], func=AF.Exp, bias=negmax[:, 0:1], scale=1.0), reads=[kk("mx8"), kk("negmax")], writes=[kk("e4")])
                    P.op("dve", lambda e: e.reduce_sum(out=s4[:], in_=e4[:], axis=AX.X), reads=[kk("e4")], writes=[kk("s4")])
                    P.op("dve", lambda e: e.reciprocal(out=s4[:], in_=s4[:]), reads=[kk("s4")], writes=[kk("s4")])
                    P.op("dve", lambda e, tt=tt: e.tensor_scalar(out=gwk[:, tt, :], in0=e4[:], scalar1=s4[:, 0:1], scalar2=None, op0=ALU.mult), reads=[kk("e4"), kk("s4")], writes=[("gwk", tt)])
                    P.op("pe", lambda e: e.matmul(psum[pl][:, 32:64], lhsT=utri[:], rhs=mskb[:], start=True, stop=True), reads=["utri", kk("mskb")], writes=[PS(pl)])
                    P.op("pe", lambda e: e.matmul(psum[pl][:, 64:96], lhsT=ones_bf[:], rhs=mskb[:], start=True, stop=True), reads=["ones_bf", kk("mskb")], writes=[PS(pl)], accum=True)
                    P.op("dve", lambda e, tt=tt: e.tensor_tensor(out=rank_all[:, tt, :], in0=psum[pl][:, 32:64], in1=cum[:], op=ALU.add), reads=[PS(pl), "cum"], writes=[PS(pl), ("rank_all", tt)])
                    P.op("dve", lambda e: e.tensor_tensor(out=cum[:], in0=psum[pl][:, 64:96], in1=cum[:], op=ALU.add), reads=[PS(pl), "cum"], writes=[PS(pl), "cum"])
                    P.op("pe", lambda e: e.transpose(out=psum[pg][0:32, 0:128], in_=gw[:], identity=ident[:]), reads=[kk("gw"), "ident"], writes=[PS(pg)])
                    P.op("act", lambda e: e.copy(out=gwT[:], in_=psum[pg][0:32, 0:128]), reads=[PS(pg)], writes=[PS(pg), kk("gwT")])
                    for nh in range(2):
                        ns = slice(nh * 512, (nh + 1) * 512)
                        b = 2 + nh
                        P.op("pe", lambda e, b=b, ns=ns: e.matmul(psum[b][:], lhsT=gwT[:], rhs=bd32[:, ns], start=True, stop=True), reads=[kk("gwT"), "bd32"], writes=[PS(b)])
                        P.op("dve", lambda e, b=b, tt=tt, ns=ns: e.tensor_tensor(out=x1[:, tt, ns], in0=psum[b][:], in1=x1[:, tt, ns], op=ALU.add),
                             reads=[PS(b), ("x1", tt)], writes=[PS(b), ("x1", tt)])

                for tt in range(NT):
                    route_tile(tt)
                rk_keys = [("rank_all", tt) for tt in range(NT)]
                P.op("pool", lambda e: e.memset(padb[:], 0.0), writes=["padb"])
                for j in range(-(-S // BLK)):
                    P.op("dve", lambda e, j=j: e.scalar_tensor_tensor(out=padb[:], in0=cum[:], scalar=float(BLK * j), in1=padb[:], op0=ALU.is_gt, op1=ALU.add),
                         reads=["cum", "padb"], writes=["padb"])
                P.op("dve", lambda e: e.tensor_scalar(out=padded[:], in0=padb[:], scalar1=float(BLK), scalar2=None, op0=ALU.mult), reads=["padb"], writes=["padded"])
                P.op("dve", lambda e: e.tensor_copy(out=pada[:], in_=padded[:]), reads=["padded", "padb"], writes=["pada"])
                src_, dst_ = pada, padb
                for st_ in (1, 2, 4, 8, 16):
                    P.op("dve", lambda e, src_=src_, dst_=dst_, st_=st_: e.tensor_copy(out=dst_[:, 0:st_], in_=src_[:, 0:st_]), reads=["pada", "padb"], writes=["pada", "padb"])
                    P.op("dve", lambda e, src_=src_, dst_=dst_, st_=st_: e.tensor_tensor(out=dst_[:, st_:32], in0=src_[:, st_:32], in1=src_[:, 0:32 - st_], op=ALU.add),
                         reads=["pada", "padb"], writes=["pada", "padb"])
                    src_, dst_ = dst_, src_
                pend = src_
                P.op("dve", lambda e: e.tensor_tensor(out=pstart[:], in0=pend[:], in1=padded[:], op=ALU.subtract), reads=["pada", "padb", "padded"], writes=["pstart"])
                P.op("dve", lambda e: e.tensor_tensor(out=dest_all[:], in0=rank_all[:], in1=pstart[:].unsqueeze(1).broadcast_to([128, NT, 32]), op=ALU.add),
                     reads=rk_keys + ["pstart"], writes=["dest_all"])
                big = stg[0][:].rearrange("p (t k e) -> p t k e", t=NT, k=4)
                la_keys = [("logit_all", tt) for tt in range(NT)] + [("mx8_all", tt) for tt in range(NT)]
                P.op("dve", lambda e: e.tensor_tensor(out=big, in0=logit_all[:].unsqueeze(2).broadcast_to([128, NT, 4, 32]),
                                                      in1=mx8_all[:, :, 0:4].unsqueeze(3).broadcast_to([128, NT, 4, 32]), op=ALU.is_equal),
                     reads=la_keys + ["hn2a", "h32a"], writes=["big"])
                P.op("dve", lambda e: e.tensor_tensor(out=big, in0=big, in1=dest_all[:].unsqueeze(2).broadcast_to([128, NT, 4, 32]), op=ALU.mult),
                     reads=["big", "dest_all"], writes=["big"])
                P.op("dve", lambda e: e.reduce_sum(out=destk[:], in_=stg[0][:].rearrange("p (c e) -> p c e", e=32), axis=AX.X), reads=["big"], writes=["destk"])
                P.op("dve", lambda e: e.tensor_copy(out=desti[:], in_=destk[:]), reads=["destk"], writes=["desti"])
                big2 = stg[1][:, 0:NBLK * 32].rearrange("p (b e) -> p b e", e=32)
                P.op("dve", lambda e: e.tensor_tensor(out=big2, in0=pend[:].unsqueeze(1).broadcast_to([128, NBLK, 32]),
                                                      in1=blkthr[:].unsqueeze(2).broadcast_to([128, NBLK, 32]), op=ALU.is_le),
                     reads=["pada", "padb", "blkthr", "h32a", "h32b", "hn2b"], writes=["big2"])
                P.op("dve", lambda e: e.reduce_sum(out=be[:], in_=big2, axis=AX.X), reads=["big2"], writes=["be"])
                P.op("dve", lambda e: e.tensor_scalar(out=bidf[:], in0=be[:], scalar1=1024.0, scalar2=None, op0=ALU.mult), reads=["be"], writes=["bidf"])
                P.op("dve", lambda e: e.tensor_tensor(out=widf[:], in0=bidf[:].unsqueeze(2).broadcast_to([128, NBLK, 8]), in1=basekc[:].unsqueeze(1).broadcast_to([128, NBLK, 8]), op=ALU.add),
                     reads=["bidf", "basekc"], writes=["widf"])
                P.op("dve", lambda e: e.tensor_copy(out=widx[:], in_=widf[:]), reads=["widf"], writes=["widx"])
                P.op("dve", lambda e: e.tensor_scalar(out=bidf[:], in0=be[:], scalar1=128.0, scalar2=basekc[:, 0:1], op0=ALU.mult, op1=ALU.add), reads=["be", "basekc", "widf"], writes=["bidf"])
                P.op("dve", lambda e: e.tensor_copy(out=bidx[:], in_=bidf[:]), reads=["bidf"], writes=["bidx"])
                if STAGE == 5:
                    gwk_keys = [("gwk", tt) for tt in range(NT)]
                    final_ops.append(dma(dbg_d["destk"][:, :], destk[:], reads=["destk"]))
                    final_ops.append(dma(dbg_d["be"][:, :], be[:], reads=["be"]))
                    final_ops.append(dma(dbg_d["gwk"][:, :], gwk[:].rearrange("p a b -> p (a b)"), reads=gwk_keys))
                    final_ops.append(dma(dbg_d["widf"][:, :], widf[:].rearrange("p a b -> p (a b)"), reads=["widf"]))
            if STAGE == 5:
                P.emit(final_ops)
                return nc
            P.barrier()
            nrow = NBLK * BLK
            xs0_keys = [("xs0", i) for i in range(len(zero_ops))]
            sc_ops = []
            for tt in range(NT):
                for k in range(4):
                    c_ = tt * 4 + k
                    sc_ops.append(idma(xs_d[:, :], IOA(ap=desti[:, c_:c_ + 1], axis=0), h2tok[:, tt, :], None, None,
                                       reads=[("h2tok", tt), "desti"] + (xs0_keys if c_ == 0 else []), writes=[("xs", c_)]))
            xs_keys = [("xs", c_) for c_ in range(NT * 4)]
            P.barrier()
            with contextlib.ExitStack() as sb_:
                print("SBUF remaining before block scope", nc.sbuf_bytes_remaining)
                wgu2 = [obuf, hT]
                identb = sbt(sb_, "identb", [128, 128], BF16)
                xtok = sbt(sb_, "xtok", [128, NQ, D], BF16)
                xT = [sbt(sb_, "xT%d" % i, [128, 8, BLK], BF16) for i in range(2)]
                actT = [sbt(sb_, "actT%d" % i, [128, 8, BLK], BF16) for i in range(2)]
                gtb = [sbt(sb_, "gtb%d" % i, [128, BLK], F32) for i in range(2)]
                sgb = [sbt(sb_, "sgb%d" % i, [128, BLK], F32) for i in range(2)]
                u1b = [sbt(sb_, "u1b%d" % i, [128, BLK], F32) for i in range(2)]
                ysb = [sbt(sb_, "ysb%d" % i, [128, 512], F32) for i in range(3)]
                bblk = [sbt(sb_, "bblk%d" % i, [128, 16], F32) for i in range(2)]
                P.op("dve", lambda e: e.tensor_copy(out=identb[:], in_=ident[:]), reads=["ident"], writes=["identb"])
                for i in range(2):
                    P.op("dve", lambda e, i=i: e.memset(bblk[i][:], 0.0), writes=[("bblk", i)])
                itc = [0]
                ycnt = [0]

                half = NBLK - 32
                order = []
                for i in range(half):
                    order += [i, 32 + i]
                order += list(range(half, 32))
                assert sorted(order) == list(range(NBLK))

                def load_wgu(b):
                    blk_ = order[b]
                    wb_ = wgu2[b % 2]
                    for kc in range(8):
                        idma(wb_[:, kc, :], None, wgu_rows[:, :], IOA(ap=widx[:, blk_, kc:kc + 1], axis=0), "bw", reads=["widx"], writes=[("wgu", b % 2, kc)])
                    idma(bblk[b % 2][:], None, bgu_d[:, :], IOA(ap=bidx[:, blk_:blk_ + 1], axis=0), "bb", reads=["bidx"], writes=[("bblk", b % 2)])
                    P.op("dve", lambda e, b=b: e.tensor_scalar(out=bblk[b % 2][:, 8:16], in0=bblk[b % 2][:, 8:16], scalar1=1.0, scalar2=None, op0=ALU.add),
                         reads=[("bblk", b % 2)], writes=[("bblk", b % 2)])

                def load_wd(b):
                    blk_ = order[b]
                    for kc in range(8):
                        idma(wdbf[:, kc, :], None, wd_rows[:, :], IOA(ap=widx[:, blk_, kc:kc + 1], axis=0), "bw", reads=["widx"], writes=[("wd", kc)])

                def load_x_dma(b):
                    blk_ = order[b]
                    for q in range(NQ):
                        dma(xtok[:, q, :], xs_d[blk_ * BLK + q * 128: blk_ * BLK + (q + 1) * 128, :], reads=xs_keys if q == 0 else [], writes=[("xtok", q)])

                def load_x(b):
                    xt_ = xT[b % 2]
                    for kc in range(8):
                        bk = 6 + kc % 2
                        pv = psum[bk][:].bitcast(BF16)
                        for q in range(NQ):
                            P.op("pe", lambda e, kc=kc, q=q, pv=pv: e.transpose(out=pv[:, q * 128:(q + 1) * 128], in_=xtok[:, q, kc * 128:(kc + 1) * 128], identity=identb[:]),
                                 reads=[("xtok", q), "identb"], writes=[PS(bk)], accum=(q > 0))
                        P.op("act", lambda e, kc=kc, pv=pv, xt_=xt_: e.activation(out=xt_[:, kc, :], in_=pv[:, 0:BLK], func=AF.Copy, scale=gffn[:, kc:kc + 1]),
                             reads=[PS(bk), "gffn"], writes=[PS(bk), ("xT", b % 2, kc)])

                pend_fin = []

                def gu_phase(b):
                    wb_ = wgu2[b % 2]
                    xt_ = xT[b % 2]
                    ap_ = b % 2
                    for m in range(8):
                        bpar = itc[0] % 2
                        par = itc[0] % 2
                        itc[0] += 1
                        bg_, bu_ = (0, 1) if bpar == 0 else (2, 3)
                        for kc in range(8):
                            P.op("pe", lambda e, kc=kc, m=m, bg_=bg_: e.matmul(psum[bg_][:, 0:BLK], lhsT=wb_[:, kc, m * 128:(m + 1) * 128], rhs=xt_[:, kc, :], start=(kc == 0), stop=(kc == 7)),
                                 reads=[("wgu", b % 2, kc), ("xT", b % 2, kc)], writes=[PS(bg_)], accum=(kc > 0))
                        for kc in range(8):
                            P.op("pe", lambda e, kc=kc, m=m, bu_=bu_: e.matmul(psum[bu_][:, 0:BLK], lhsT=wb_[:, kc, 1024 + m * 128:1024 + (m + 1) * 128], rhs=xt_[:, kc, :], start=(kc == 0), stop=(kc == 7)),
                                 reads=[("wgu", b % 2, kc), ("xT", b % 2, kc)], writes=[PS(bu_)], accum=(kc > 0))
                        gt, sg, u1 = gtb[bpar], sgb[par], u1b[bpar]
                        bb_ = bblk[b % 2]
                        P.op("dve", lambda e, gt=gt, bg_=bg_, m=m, bb_=bb_: e.tensor_scalar(out=gt[:], in0=psum[bg_][:, 0:BLK], scalar1=bb_[:, m:m + 1], scalar2=7.0, op0=ALU.add, op1=ALU.min),
                             reads=[PS(bg_), ("bblk", b % 2)], writes=[PS(bg_), ("gt", bpar)])
                        P.op("act", lambda e, gt=gt, sg=sg: e.activation(out=sg[:], in_=gt[:], func=AF.Sigmoid, scale=1.702), reads=[("gt", bpar)], writes=[("sg", par)])
                        P.op("dve", lambda e, u1=u1, bu_=bu_, m=m, bb_=bb_: e.tensor_scalar(out=u1[:], in0=psum[bu_][:, 0:BLK], scalar1=bb_[:, 8 + m:9 + m], scalar2=8.0, op0=ALU.add, op1=ALU.min),
                             reads=[PS(bu_), ("bblk", b % 2)], writes=[PS(bu_), ("u1", bpar)])
                        P.op("dve", lambda e, gt=gt, u1=u1: e.scalar_tensor_tensor(out=u1[:], in0=u1[:], scalar=-6.0, in1=gt[:], op0=ALU.max, op1=ALU.mult),
                             reads=[("gt", bpar), ("u1", bpar)], writes=[("u1", bpar)])

                        def fin(sg=sg, u1=u1, ap_=ap_, m=m, par=par, bpar=bpar):
                            P.op("dve", lambda e: e.tensor_tensor(out=actT[ap_][:, m, :], in0=u1[:], in1=sg[:], op=ALU.mult),
                                 reads=[("sg", par), ("u1", bpar)], writes=[("actT", ap_, m)])
                        if pend_fin:
                            pend_fin.pop()()
                        pend_fin.append(fin)
                    if pend_fin:
                        pend_fin.pop()()

                def down_phase(b):
                    ap_ = b % 2
                    blk_ = order[b]
                    for q in range(NQ):
                        for nh in range(2):
                            yi = ycnt[0] % 3
                            ycnt[0] += 1
                            yb = ysb[yi]
                            yk = ("ysb", yi)
                            bk = 4 + nh
                            ns = slice(nh * 512, (nh + 1) * 512)
                            for m in range(8):
                                P.op("pe", lambda e, m=m, bk=bk, q=q, ns=ns: e.matmul(psum[bk][:], lhsT=actT[ap_][:, m, q * 128:(q + 1) * 128], rhs=wdbf[:, m, ns], start=(m == 0), stop=(m == 7)),
                                     reads=[("actT", ap_, m), ("wd", m)], writes=[PS(bk)], accum=(m > 0))
                            if nh == 0:
                                P.op("act", lambda e, bk=bk, yb=yb: e.copy(out=yb[:], in_=psum[bk][:]), reads=[PS(bk)], writes=[PS(bk), yk])
                            else:
                                P.op("dve", lambda e, bk=bk, yb=yb: e.tensor_copy(out=yb[:], in_=psum[bk][:]), reads=[PS(bk)], writes=[PS(bk), yk])
                            dma(y_d[blk_ * BLK + q * 128: blk_ * BLK + (q + 1) * 128, ns], yb[:], reads=[yk], writes=[("y", b, q, nh)])

                load_wgu(0)
                load_wgu(1)
                load_wd(0)
                load_x_dma(0)
                load_x(0)
                load_x_dma(1)
                gu_phase(0)
                for b in range(NBLK):
                    if b + 1 < NBLK:
                        load_x(b + 1)
                        if b + 2 < NBLK:
                            load_x_dma(b + 2)
                        gu_phase(b + 1)
                    if b + 2 < NBLK:
                        load_wgu(b + 2)
                    down_phase(b)
                    if b + 1 < NBLK:
                        load_wd(b + 1)
            P.barrier()
            with contextlib.ExitStack() as sc:
                yg = [sbt(sc, "yg%d" % i, [128, D], F32) for i in range(4)]
                for tt in range(NT):
                    for k in range(4):
                        c_ = tt * 4 + k
                        gi = c_ % 4
                        idma(yg[gi][:], None, y_d[:, :], IOA(ap=desti[:, c_:c_ + 1], axis=0), None, reads=["desti"], writes=[("yg", gi)])
                        P.op("dve", lambda e, tt=tt, k=k, gi=gi: e.scalar_tensor_tensor(out=x1[:, tt, :], in0=yg[gi][:], scalar=gwk[:, tt, k:k + 1], in1=x1[:, tt, :], op0=ALU.mult, op1=ALU.add),
                             reads=[("yg", gi), ("gwk", tt), ("x1", tt)], writes=[("x1", tt)])
                    final_ops.append(dma(out_d[tt * 128:(tt + 1) * 128, :], x1[:, tt, :], reads=[("x1", tt)]))
        P.emit(final_ops)
    return nc


def _in_maps(inputs):
    inp = {k: np.asarray(v) for k, v in inputs.items()}
    r = _relayout(inp)
    c = _consts()
    base = dict(r)
    if STAGE <= 4:
        base.pop("w_gate_up")
        base.pop("w_down")
    for k, v in c.items():
        base["c_" + k] = v
    maps = []
    for b in range(inp["x"].shape[0]):
        m = dict(base)
        m["x"] = np.ascontiguousarray(inp["x"][b])
        maps.append(m)
    return maps


def kernel(**inputs):
    maps = _in_maps(inputs)
    nc = build()
    res = run_bass_kernel_spmd(nc, maps, core_ids=list(range(len(maps))))
    return np.stack([r["out"] for r in res.results], axis=0).astype(np.float32)
```

```python
import contextlib
import os
import numpy as np
import concourse.bass as bass
import concourse.mybir as mybir
from concourse.bass_utils import run_bass_kernel_spmd

F32 = mybir.dt.float32
BF16 = mybir.dt.bfloat16
ALU = mybir.AluOpType
AF = mybir.ActivationFunctionType
AX = mybir.AxisListType

S = 2048
D = 1024
NT = 16
DIL = (1, 4, 16)
NE = 32
EPS = 1e-6
STAGE = int(os.environ.get("KSTAGE", "99"))
NEXP = int(os.environ.get("KNEXP", "32"))
SPARSE = int(os.environ.get("KSPARSE", "1"))
BLK = 384
NBLK = (4 * 2048 + 32 * (BLK - 1)) // BLK
NQ = BLK // 128

ENGS = ("pe", "act", "dve", "pool", "sp")
N_DMA_SEMS = 28
N_DMA_SEMS_HW = 16


class Op:
    __slots__ = ("idx", "eng", "fn", "deps", "dma", "sem", "val", "signal")


class Prog:
    def __init__(self, nc):
        self.nc = nc
        self.ops = []
        self.last_w = {}
        self.readers = {}
        self.dma_rr = 0
        self.dma_rr_sw = 0
        self.dma_last = [None] * N_DMA_SEMS
        self.dma_cnt = [0] * N_DMA_SEMS
        self.barrier_deps = set()
        self.last_on_eng = {}

    def op(self, eng, fn, reads=(), writes=(), dma=False, accum=False):
        o = Op()
        o.idx = len(self.ops)
        o.eng = eng
        o.fn = fn
        o.dma = dma
        o.signal = False
        o.sem = None
        o.val = None
        deps = set(self.barrier_deps)
        for k in reads:
            w = self.last_w.get(k)
            if w is not None:
                deps.add(w)
        for k in writes:
            w = self.last_w.get(k)
            if w is not None:
                if not (accum and self.ops[w].eng == "pe" and eng == "pe"):
                    deps.add(w)
            for r in self.readers.get(k, ()):
                deps.add(r)
        if dma:
            if eng == "sp":
                s = self.dma_rr % N_DMA_SEMS_HW
                self.dma_rr += 1
            else:
                s = N_DMA_SEMS_HW + self.dma_rr_sw % (N_DMA_SEMS - N_DMA_SEMS_HW)
                self.dma_rr_sw += 1
            prev = self.dma_last[s]
            if prev is not None:
                deps.add(prev)
            self.dma_last[s] = o.idx
            self.dma_cnt[s] += 1
            o.sem = ("dma", s)
            o.val = 16 * self.dma_cnt[s]
            o.signal = True
        o.deps = deps
        for k in reads:
            self.readers.setdefault(k, []).append(o.idx)
        for k in writes:
            self.last_w[k] = o.idx
            self.readers[k] = []
        self.ops.append(o)
        self.last_on_eng[("dma", o.sem) if dma else eng] = o.idx
        return o

    def barrier(self):
        self.barrier_deps = set(self.last_on_eng.values())

    def emit(self, final_ops):
        nc = self.nc
        ops = self.ops
        for o in ops:
            for d in o.deps:
                ops[d].signal = True
        cnt = {e: 0 for e in ENGS}
        for o in ops:
            if o.dma:
                continue
            if o.signal:
                cnt[o.eng] += 1
                o.sem = ("eng", o.eng)
                o.val = cnt[o.eng]
        with contextlib.ExitStack() as st:
            sems = {}
            for e in ENGS:
                sems[("eng", e)] = st.enter_context(nc.semaphore("s_" + e))
            for i in range(N_DMA_SEMS):
                sems[("dma", i)] = st.enter_context(nc.semaphore("s_dma%d" % i))
            block = st.enter_context(nc.Block())
            by_eng = {e: [o for o in ops if o.eng == e] for e in ENGS}

            def run(eng_name, engine):
                seen = {}
                for o in by_eng[eng_name]:
                    need = {}
                    for d in o.deps:
                        od = ops[d]
                        if seen.get(od.sem, 0) >= od.val:
                            continue
                        if need.get(od.sem, 0) < od.val:
                            need[od.sem] = od.val
                    for s, v in need.items():
                        engine.wait_ge(sems[s], v)
                        seen[s] = v
                    ins = o.fn(engine)
                    if o.signal:
                        ins.then_inc(sems[o.sem], 16 if o.dma else 1)
                if eng_name == "sp":
                    for fo in final_ops:
                        engine.wait_ge(sems[fo.sem], fo.val)

            @block.tensor
            def _(e):
                run("pe", e)

            @block.scalar
            def _(e):
                run("act", e)

            @block.vector
            def _(e):
                run("dve", e)

            @block.gpsimd
            def _(e):
                run("pool", e)

            @block.sync
            def _(e):
                run("sp", e)


def _consts():
    c = {}
    c["ident"] = np.eye(128, dtype=np.float32)
    bo = np.zeros((128, 128), np.float32)
    bo[:64, :64] = 1.0
    bo[64:, 64:] = 1.0
    c["blockones"] = bo
    pos = np.arange(S, dtype=np.float32)
    inv = (np.float32(500000.0) ** (-(np.arange(8, dtype=np.float32) / np.float32(8)))).astype(np.float32)
    ang = (pos[:, None] * inv[None, :]).astype(np.float32)
    cosA = np.ones((128, S), np.float32)
    sinA = np.zeros((128, S), np.float32)
    RA = np.zeros((128, 128), np.float32)
    for hh in range(2):
        for dd in range(16):
            p = hh * 64 + dd
            cosA[p] = np.cos(ang[:, dd % 8])
            sinA[p] = np.sin(ang[:, dd % 8])
            if dd < 8:
                RA[p + 8, p] = -1.0
            else:
                RA[p - 8, p] = 1.0
    c["cosA"], c["sinA"], c["RA"] = cosA, sinA, RA
    row = (np.arange(S) // 64).astype(np.float32)
    col = (np.arange(S) % 64).astype(np.float32)
    invb = (np.float32(10000.0) ** (-(np.arange(16, dtype=np.float32) / np.float32(16)))).astype(np.float32)
    cosB = np.ones((128, S), np.float32)
    sinB = np.zeros((128, S), np.float32)
    RB = np.zeros((128, 128), np.float32)
    for hh in range(2):
        for dd in range(64):
            p = hh * 64 + dd
            ps_ = row if dd < 32 else col
            a = (ps_ * invb[dd % 16]).astype(np.float32)
            cosB[p] = np.cos(a)
            sinB[p] = np.sin(a)
            if (dd % 32) < 16:
                RB[p + 16, p] = -1.0
            else:
                RB[p - 16, p] = 1.0
    c["cosB"], c["sinB"], c["RB"] = cosB, sinB, RB
    a = np.arange(128)[:, None]
    b = np.arange(128)[None, :]
    m = np.zeros((128, 3, 128), np.float32)
    m[:, 0, :] = (a - b >= 64)
    m[:, 1, :] = (np.abs(a - b) <= 64)
    m[:, 2, :] = (b - a >= 64)
    c["utri"] = np.triu(np.ones((128, 128), np.float32), 1)
    c["blkthr"] = np.tile((np.arange(NBLK, dtype=np.float32) * BLK)[None, :], (128, 1))
    c["basekc"] = (np.arange(8)[None, :] * 128 + np.arange(128)[:, None]).astype(np.float32)
    c["maskA"] = ((1.0 - m) * -30000.0).astype(np.float32).reshape(128, 384)
    return c


def _relayout(inp):
    r = {}
    w_in = inp["w_in"]
    r["w_in"] = w_in
    r["gmix"] = np.ascontiguousarray(inp["norm_mix_g"].reshape(8, 128).T)
    r["gffn"] = np.ascontiguousarray(inp["norm_ffn_g"].reshape(8, 128).T)
    gains = np.zeros((128, 30), np.float32)
    for g in range(3):
        for hp in range(4):
            gains[:, g * 4 + hp] = np.tile(inp["qn_a"][g], 2)
            gains[:, 12 + g * 4 + hp] = np.tile(inp["kn_a"][g], 2)
    for c in range(4):
        gains[:, 24 + c] = np.tile(inp["qn_b"], 2)
    for g in range(2):
        gains[:, 28 + g] = np.tile(inp["kn_b"], 2)
    r["gains"] = gains
    g0c = 3 * 1536 + 512 + 256
    bun = np.empty((8, 128, 24, 128), np.float32)
    for m in range(8):
        cs = slice(m * 128, (m + 1) * 128)
        bun[m, :, 0:4] = inp["w_proj_a"][:, cs].reshape(4, 128, 128).transpose(1, 0, 2)
        bun[m, :, 4:8] = inp["w_proj_b"][:, cs].reshape(4, 128, 128).transpose(1, 0, 2)
        bun[m, :, 8:16] = w_in[:, g0c + m * 128: g0c + (m + 1) * 128].reshape(8, 128, 128).transpose(1, 0, 2)
        bun[m, :, 16:24] = w_in[:, g0c + 1024 + m * 128: g0c + 1024 + (m + 1) * 128].reshape(8, 128, 128).transpose(1, 0, 2)
    r["mbundle"] = bun
    r["bgate"] = np.ascontiguousarray(inp["b_gate"].reshape(2, 8, 128).transpose(2, 0, 1).reshape(128, 16))
    r["w_out"] = inp["w_out"]
    r["w_router"] = inp["w_router"]
    r["b_router"] = inp["b_router"].reshape(1, 32)
    r["w_gate_up"] = inp["w_gate_up"]
    r["w_down"] = inp["w_down"]
    r["bgu"] = np.ascontiguousarray(inp["b_gate_up"].reshape(32, 16, 128).transpose(0, 2, 1).reshape(4096, 16))
    r["b_down"] = inp["b_down"]
    return r


def build(dbg=None):
    nc = bass.Bass("TRN2", target_bir_lowering=False)

    def din(name, shape):
        return nc.dram_tensor(name, list(shape), F32, kind="ExternalInput").ap()

    x_d = din("x", [S, D])
    w_in_d = din("w_in", [D, 7424])
    gmix_d = din("gmix", [128, 8])
    gffn_d = din("gffn", [128, 8])
    gains_d = din("gains", [128, 30])
    mb_d = din("mbundle", [8, 128, 24, 128])
    bgate_d = din("bgate", [128, 16])
    w_out_d = din("w_out", [D, D])
    w_router_d = din("w_router", [D, 32])
    b_router_d = din("b_router", [1, 32])
    if STAGE > 4:
        wgu_d = din("w_gate_up", [NE, D, 2048])
        wd_d = din("w_down", [NE, D, D])
    bgu_d = din("bgu", [4096, 16])
    bd_d = din("b_down", [NE, D])
    cd = {k: din("c_" + k, v.shape) for k, v in _consts().items()}
    out_d = nc.dram_tensor("out", [S, D], F32, kind="ExternalOutput").ap()
    dbg_d = {}
    if dbg:
        for k, shp in dbg.items():
            dbg_d[k] = nc.dram_tensor("dbg_" + k, list(shp), F32, kind="ExternalOutput").ap()

    w_in_v = w_in_d.rearrange("(kc p) n -> p kc n", p=128)

    P = Prog(nc)
    final_ops = []
    with contextlib.ExitStack() as top:
        def sbt(stk, name, shape, dt):
            return stk.enter_context(nc.sbuf_tensor("sb_" + name, list(shape), dt))

        x1 = sbt(top, "x1", [128, NT, D], F32)
        hT = sbt(top, "hT", [128, 8, S], BF16)
        obuf = sbt(top, "obuf", [128, 8, S], BF16)
        ident = sbt(top, "ident", [128, 128], F32)
        blockones = sbt(top, "blockones", [128, 128], F32)
        ones_bf = sbt(top, "ones_bf", [128, 128], BF16)
        gmix = sbt(top, "gmix", [128, 8], F32)
        gffn = sbt(top, "gffn", [128, 8], F32)
        gains = sbt(top, "gains", [128, 30], F32)
        bgate = sbt(top, "bgate", [128, 16], F32)
        epst = sbt(top, "epst", [128, 1], F32)
        ssq = sbt(top, "ssq", [128, NT], F32)
        rstd = sbt(top, "rstd", [128, NT], F32)
        psum = [top.enter_context(nc.psum_tensor("ps%d" % i, [128, 512], F32)) for i in range(8)]

        def PS(i):
            return ("ps", i)

        def dma(out, in_, reads=(), writes=(), eng="sp"):
            return P.op(eng, lambda e: e.dma_start(out=out, in_=in_), reads=reads, writes=writes, dma=True)

        def dbg_out(name, ap, key):
            if name in dbg_d:
                final_ops.append(dma(dbg_d[name], ap, reads=[key]))

        x1f = x1[:].rearrange("p a b -> p (a b)")

        def scr(off, n):
            return x1f[:, off:off + n]

        xs_d = nc.dram_tensor("xsorted", [NBLK * BLK, D], BF16).ap()
        y_d = nc.dram_tensor("ysorted", [NBLK * BLK, D], F32).ap()
        zero_ops = []
        dma(ident[:], cd["ident"][:, :], writes=["ident"])
        dma(blockones[:], cd["blockones"][:, :], writes=["blockones"])
        dma(gmix[:], gmix_d[:, :], writes=["gmix"])
        dma(gffn[:], gffn_d[:, :], writes=["gffn"])
        dma(gains[:], gains_d[:, :], writes=["gains"])
        dma(bgate[:], bgate_d[:, :], writes=["bgate"])
        P.op("pool", lambda e: e.memset(epst[:], EPS), writes=["eps"])
        P.op("pool", lambda e: e.memset(ones_bf[:], 1.0), writes=["ones_bf"])

        def norm_transpose(tt, src_ap, src_key, hn, hn_key, junk, junk_key, dstT, dst_key, psb, extra32=None, scale_ap=None):
            P.op("act", lambda e: e.activation(out=junk, in_=src_ap, func=AF.Square, accum_out=ssq[:, tt:tt + 1]),
                 reads=[src_key], writes=[junk_key, ("ssq", tt)])
            P.op("act", lambda e: e.activation(out=rstd[:, tt:tt + 1], in_=ssq[:, tt:tt + 1], func=AF.Sqrt,
                                               bias=epst[:, 0:1], scale=1.0 / D), reads=[("ssq", tt), "eps"], writes=[("rstd", tt)])
            P.op("dve", lambda e: e.reciprocal(out=rstd[:, tt:tt + 1], in_=rstd[:, tt:tt + 1]), reads=[("rstd", tt)], writes=[("rstd", tt)])
            P.op("dve", lambda e: e.tensor_scalar(out=hn, in0=src_ap, scalar1=rstd[:, tt:tt + 1], scalar2=None, op0=ALU.mult),
                 reads=[src_key, ("rstd", tt)], writes=[hn_key])
            for half in range(2):
                b = psb[half]
                for q in range(4):
                    kc = half * 4 + q
                    P.op("pe", lambda e, kc=kc, q=q, b=b: e.transpose(out=psum[b][:, q * 128:(q + 1) * 128], in_=hn[:, kc * 128:(kc + 1) * 128], identity=ident[:]),
                         reads=[hn_key, "ident"], writes=[PS(b)], accum=(q > 0))
                if dstT is not None and scale_ap is not None:
                    for q in range(4):
                        kc = half * 4 + q
                        P.op("act", lambda e, kc=kc, q=q, b=b: e.activation(out=dstT[:, kc, tt * 128:(tt + 1) * 128], in_=psum[b][:, q * 128:(q + 1) * 128],
                                                                            func=AF.Copy, scale=scale_ap[:, kc:kc + 1]),
                             reads=[PS(b), "gmix"], writes=[PS(b), (dst_key, tt)])
                elif dstT is not None:
                    P.op("act", lambda e, half=half, b=b: e.copy(out=dstT[:, half * 4:half * 4 + 4, tt * 128:(tt + 1) * 128],
                                                                  in_=psum[b][:].rearrange("p (a c) -> p a c", a=4)),
                         reads=[PS(b)], writes=[PS(b), (dst_key, tt)])
                if extra32 is not None:
                    e32, e32_key = extra32
                    P.op("act", lambda e, half=half, b=b: e.copy(out=e32[:, half * 4:half * 4 + 4, :], in_=psum[b][:].rearrange("p (a c) -> p a c", a=4)),
                         reads=[PS(b)], writes=[PS(b), e32_key])

        for tt in range(NT):
            xs = scr((tt % 2) * 1024, 1024)
            hn = scr(2048 + (tt % 2) * 1024, 1024)
            jk = scr(4096, 1024)
            dma(xs, x_d[tt * 128:(tt + 1) * 128, :], writes=[("xs", tt % 2)])
            norm_transpose(tt, xs, ("xs", tt % 2), hn, ("hn", tt % 2), jk, "junk", hT, "hT", (6, 7), scale_ap=gmix)
        hT_keys = [("hT", tt) for tt in range(NT)]
        if STAGE == 0:
            tmp = scr(8192, 2048)
            P.op("dve", lambda e: e.tensor_copy(out=tmp, in_=hT[:, 0, :]), reads=hT_keys, writes=["dbgtmp"])
            dbg_out("hT0", tmp, "dbgtmp")
        P.barrier()

        def load_w_piece(stage, stage_key, wbf, wbf_key, col_specs, fold):
            for (d0, s0, n) in col_specs:
                P.op("pool", lambda e, d0=d0, s0=s0, n=n: e.dma_start(out=wbf[:, :, d0:d0 + n], in_=w_in_v[:, :, s0:s0 + n]), writes=[wbf_key], dma=True)

        class RopeCtx:
            pass

        def qk_chunk(rc, wbf, wbf_key, gain_col, dst, dst_key, dil):
            def proj(tc):
                pb = rc.ps_proj[tc % 2]
                for kc in range(8):
                    P.op("pe", lambda e, kc=kc, pb=pb, tc=tc: e.matmul(psum[pb][:], lhsT=wbf[:, kc, :], rhs=hT[:, kc, tc * 512:(tc + 1) * 512],
                                                                        start=(kc == 0), stop=(kc == 7)),
                         reads=[wbf_key] + hT_keys[tc * 4:tc * 4 + 4], writes=[PS(pb)], accum=(kc > 0))
                sset = tc % 2
                u, sq = rc.u[sset], rc.sq[sset]
                ku, ksq = ("u", sset), ("sq", sset)
                P.op("act", lambda e, pb=pb, u=u: e.activation(out=u, in_=psum[pb][:], func=AF.Copy, scale=gains[:, gain_col:gain_col + 1]),
                     reads=[PS(pb), "gains"], writes=[PS(pb), ku])
                P.op("act", lambda e, pb=pb, sq=sq: e.activation(out=sq, in_=psum[pb][:], func=AF.Square), reads=[PS(pb)], writes=[PS(pb), ksq])

            def aux(tc):
                sset = tc % 2
                u, sq, rs, t1 = rc.u[sset], rc.sq[sset], rc.rs[sset], rc.t1[sset]
                ku, ksq, krs, kt1 = ("u", sset), ("sq", sset), ("rs", sset), ("t1", sset)
                pa = rc.ps_aux[0]
                pr = rc.ps_aux[1]
                P.op("pe", lambda e, pa=pa, sq=sq: e.matmul(psum[pa][:], lhsT=blockones[:], rhs=sq, start=True, stop=True),
                     reads=["blockones", ksq], writes=[PS(pa)])
                P.op("pe", lambda e, pr=pr, u=u: e.matmul(psum[pr][:], lhsT=rc.R[:], rhs=u, start=True, stop=True),
                     reads=[rc.R_key, ku], writes=[PS(pr)])
                P.op("act", lambda e, pa=pa, rs=rs: e.activation(out=rs, in_=psum[pa][:], func=AF.Sqrt, bias=epst[:, 0:1], scale=1.0 / 64),
                     reads=[PS(pa), "eps"], writes=[PS(pa), krs])
                P.op("dve", lambda e, rs=rs: e.reciprocal(out=rs, in_=rs), reads=[krs], writes=[krs])
                P.op("pool", lambda e, u=u, t1=t1, tc=tc: e.tensor_tensor(out=t1, in0=u, in1=rc.cos[:, tc * 512:(tc + 1) * 512], op=ALU.mult),
                     reads=[ku, rc.tab_key], writes=[kt1])
                P.op("dve", lambda e, pr=pr, sq=sq, tc=tc: e.tensor_tensor(out=sq, in0=psum[pr][:], in1=rc.sin[:, tc * 512:(tc + 1) * 512], op=ALU.mult),
                     reads=[PS(pr), rc.tab_key, ksq], writes=[PS(pr), ksq])
                P.op("dve", lambda e, t1=t1, sq=sq: e.tensor_tensor(out=t1, in0=t1, in1=sq, op=ALU.add), reads=[kt1, ksq], writes=[kt1])
                n = 512 // dil
                dv = dst.rearrange("p (r m) -> p r m", r=dil)[:, :, tc * n:(tc + 1) * n]
                P.op("dve", lambda e, t1=t1, rs=rs, dv=dv: e.tensor_tensor(out=dv, in0=t1.rearrange("p (m r) -> p r m", r=dil),
                                                                           in1=rs.rearrange("p (m r) -> p r m", r=dil), op=ALU.mult),
                     reads=[kt1, krs], writes=[dst_key])

            proj(0)
            for tc in range(4):
                if tc + 1 < 4:
                    proj(tc + 1)
                aux(tc)

        def v_tiles(wbf, wbf_key, vdst, v_key, dil, pbanks, evac=None):
            nb = (S // dil) // 128
            hv = hT[:].rearrange("p k (m r) -> p k r m", r=dil)
            for tg in range(4):
                pb = pbanks[tg % 2]
                for q in range(4):
                    ti = tg * 4 + q
                    r_, j = ti // nb, ti % nb
                    for kc in range(8):
                        P.op("pe", lambda e, kc=kc, q=q, r_=r_, j=j, pb=pb: e.matmul(psum[pb][:, q * 128:(q + 1) * 128], lhsT=hv[:, kc, r_, j * 128:(j + 1) * 128],
                                                                                     rhs=wbf[:, kc, :], start=(kc == 0), stop=(kc == 7)),
                             reads=[wbf_key] + hT_keys, writes=[PS(pb)], accum=not (q == 0 and kc == 0))
                if evac is not None:
                    evac(tg, pb)
                    continue
                P.op("act", lambda e, tg=tg, pb=pb: e.copy(out=vdst[:, tg * 4:tg * 4 + 4, :], in_=psum[pb][:].rearrange("p (a c) -> p a c", a=4)),
                     reads=[PS(pb)], writes=[PS(pb), v_key])

        o_aT = obuf[:, 0:4, :]
        o_bT = obuf[:, 4:8, :]

        with contextlib.ExitStack() as sa:
            rc = RopeCtx()
            rc.cos = scr(0, 2048)
            rc.sin = scr(2048, 2048)
            accn = scr(4096, 2048)
            accd = scr(6144, 2048)
            rc.u = [scr(8192 + i * 512, 512) for i in range(2)]
            rc.sq = [scr(9216 + i * 512, 512) for i in range(2)]
            rc.rs = [scr(10240 + i * 512, 512) for i in range(2)]
            rc.t1 = [scr(11264 + i * 512, 512) for i in range(2)]
            wst = [scr(12288 + i * 1024, 1024).rearrange("p (k c) -> p k c", k=8) for i in range(3)]
            rc.R = sbt(sa, "RA", [128, 128], F32)
            rc.R_key = "R"
            rc.tab_key = "tab"
            rc.ps_proj = (0, 1)
            rc.ps_aux = (2, 3)
            maskA = sbt(sa, "maskA", [128, 384], BF16)
            mstage = scr(15360, 384)
            wbf = [sbt(sa, "wbfA%d" % i, [128, 8, 128], BF16) for i in range(6)]
            qT = [sbt(sa, "qT%d" % i, [128, S], BF16) for i in range(2)]
            kT = [sbt(sa, "kT%d" % i, [128, S], BF16) for i in range(2)]
            vA = [[sbt(sa, "vA%d_%d" % (i, hh), [128, 16, 128], BF16) for hh in range(2)] for i in range(2)]
            pT = [sbt(sa, "pTA%d" % i, [128, 384], BF16) for i in range(4)]
            ident_bf = sbt(sa, "ident_bf", [128, 128], BF16)
            P.op("pool", lambda e: e.tensor_copy(out=ident_bf[:], in_=ident[:]), reads=["ident"], writes=["ident_bf"])
            for i in range(2):
                for hh in range(2):
                    P.op("pool", lambda e, i=i, hh=hh: e.memset(vA[i][hh][:], 1.0), writes=[("vA", i)])
            dma(rc.cos, cd["cosA"][:, :], writes=["tab"])
            dma(rc.sin, cd["sinA"][:, :], writes=["tab"])
            dma(rc.R[:], cd["RA"][:, :], writes=["R"])
            dma(mstage, cd["maskA"][:, :], writes=["mstage"])
            P.op("pool", lambda e: e.tensor_copy(out=maskA[:], in_=mstage), reads=["mstage"], writes=["maskA"])
            it = 0
            blk = 0
            itersA = [(hp, g) for hp in range(4) for g in range(3)]

            def loadsA(ix):
                hp_, g_ = itersA[ix]
                o3 = 3 * (ix % 2)
                for j, c0 in enumerate((g_ * 512 + hp_ * 128, 1536 + g_ * 512 + hp_ * 128, 3072 + g_ * 512 + hp_ * 128)):
                    load_w_piece(None, None, wbf[o3 + j][:], ("wbf", o3 + j), [(0, c0, 128)], None)
            loadsA(0)
            for hp in range(4):
                for g in range(3):
                    dil = DIL[g]
                    nb = (S // dil) // 128
                    buf = it % 2
                    if it + 1 < len(itersA):
                        loadsA(it + 1)
                    o3 = 3 * (it % 2)
                    it += 1
                    qk_chunk(rc, wbf[o3 + 0], ("wbf", o3 + 0), g * 4 + hp, qT[buf][:], ("qT", buf), dil)
                    qk_chunk(rc, wbf[o3 + 1], ("wbf", o3 + 1), 12 + g * 4 + hp, kT[buf][:], ("kT", buf), dil)
                    def evacA(tg, pb, buf=buf):
                        for hh in range(2):
                            P.op("act", lambda e, tg=tg, pb=pb, hh=hh, buf=buf: e.copy(out=vA[buf][hh][:, tg * 4:tg * 4 + 4, 64 * hh:64 * hh + 64],
                                                                                      in_=psum[pb][:].rearrange("p (a c) -> p a c", a=4)[:, :, 64 * hh:64 * hh + 64]),
                                 reads=[PS(pb)], writes=[PS(pb), ("vA", buf)])
                    v_tiles(wbf[o3 + 2], ("wbf", o3 + 2), None, ("vA", buf), dil, (0, 1), evac=evacA)
                    if STAGE == 1 and hp == 0:
                        tmp = scr(4096, 1024)
                        for nm, src_, key in (("qT_%d" % g, qT[buf], ("qT", buf)), ("kT_%d" % g, kT[buf], ("kT", buf))):
                            for hf in range(2):
                                P.op("dve", lambda e, src_=src_, hf=hf: e.tensor_copy(out=tmp, in_=src_[:, hf * 1024:(hf + 1) * 1024]), reads=[key], writes=["dbgtmp"])
                                if nm in dbg_d:
                                    final_ops.append(dma(dbg_d[nm][:, hf * 1024:(hf + 1) * 1024], tmp, reads=["dbgtmp"]))
                    if STAGE == 1:
                        continue
                    blocks = [(hh, bi) for hh in range(2) for bi in range(16)]

                    def s_block(hh, bi, blk_):
                        hs = slice(64 * hh, 64 * hh + 64)
                        r_, i = bi // nb, bi % nb
                        sb_ = (4, 5, 3)[blk_ % 3]
                        pbuf = blk_ % 4
                        js = [j for j in (i - 1, i, i + 1) if 0 <= j < nb]
                        qcols = slice(r_ * (S // dil) + i * 128, r_ * (S // dil) + (i + 1) * 128)
                        for j in js:
                            sl = j - i + 1
                            kcols = slice(r_ * (S // dil) + j * 128, r_ * (S // dil) + (j + 1) * 128)
                            P.op("pe", lambda e, sb_=sb_, sl=sl, kcols=kcols, qcols=qcols, hs=hs, buf=buf: e.matmul(
                                psum[sb_][:, sl * 128:(sl + 1) * 128], lhsT=kT[buf][hs, kcols], rhs=qT[buf][hs, qcols], start=True, stop=False),
                                reads=[("kT", buf), ("qT", buf)], writes=[PS(sb_)], accum=(j != js[0]))
                            P.op("pe", lambda e, sb_=sb_, sl=sl: e.matmul(
                                psum[sb_][:, sl * 128:(sl + 1) * 128], lhsT=ident_bf[:], rhs=maskA[:, sl * 128:(sl + 1) * 128], start=False, stop=True),
                                reads=["ident_bf", "maskA"], writes=[PS(sb_)], accum=True)
                        c0, c1 = (js[0] - i + 1) * 128, (js[-1] - i + 2) * 128
                        P.op("act", lambda e, sb_=sb_, pbuf=pbuf, c0=c0, c1=c1: e.activation(out=pT[pbuf][:, c0:c1], in_=psum[sb_][:, c0:c1], func=AF.Exp, scale=0.125),
                             reads=[PS(sb_)], writes=[PS(sb_), ("pT", pbuf)])

                    def pv_block(hh, bi, blk_):
                        hs = slice(64 * hh, 64 * hh + 64)
                        os_ = slice(64 * (1 - hh), 64 * (1 - hh) + 64)
                        r_, i = bi // nb, bi % nb
                        pbuf = blk_ % 4
                        js = [j for j in (i - 1, i, i + 1) if 0 <= j < nb]
                        pn = (6, 7)[(bi // 4) % 2]
                        oc = slice((bi % 4) * 128, (bi % 4 + 1) * 128)
                        for n_, j in enumerate(js):
                            sl = j - i + 1
                            vt = r_ * nb + j
                            first = (bi % 4 == 0 and n_ == 0)
                            P.op("pe", lambda e, pn=pn, oc=oc, vt=vt, sl=sl, pbuf=pbuf, buf=buf, hh=hh, n_=n_, nl=len(js) - 1: e.matmul(
                                psum[pn][:, oc], lhsT=vA[buf][hh][:, vt, :], rhs=pT[pbuf][:, sl * 128:(sl + 1) * 128], start=(n_ == 0), stop=(n_ == nl)),
                                reads=[("vA", buf), ("pT", pbuf)], writes=[PS(pn)], accum=not first)
                        if bi % 4 == 3:
                            b4 = bi // 4
                            if nb >= 4:
                                r0, nr, i0, ni = (4 * b4) // nb, 1, (4 * b4) % nb, 4
                            else:
                                r0, nr, i0, ni = 4 * b4, 4, 0, 1
                            for (acc, ps_rows, kk) in ((accn, hs, "accn"), (accd, os_, "accd")):
                                av = acc.rearrange("p (m r) -> p r m", r=dil)[hs, r0:r0 + nr, i0 * 128:(i0 + ni) * 128]
                                pv = psum[pn][ps_rows, :].rearrange("p (a c) -> p a c", a=nr)
                                if g == 0:
                                    P.op("dve", lambda e, av=av, pv=pv: e.tensor_copy(out=av, in_=pv), reads=[PS(pn)], writes=[PS(pn), (kk, hh)])
                                else:
                                    P.op("dve", lambda e, av=av, pv=pv: e.tensor_tensor(out=av, in0=pv, in1=av, op=ALU.add),
                                         reads=[PS(pn), (kk, hh)], writes=[PS(pn), (kk, hh)])

                    DEPTH = 2
                    for bx in range(min(DEPTH, len(blocks))):
                        s_block(blocks[bx][0], blocks[bx][1], blk + bx)
                    for bx, (hh, bi) in enumerate(blocks):
                        if bx + DEPTH < len(blocks):
                            s_block(blocks[bx + DEPTH][0], blocks[bx + DEPTH][1], blk + DEPTH)
                        pv_block(hh, bi, blk)
                        blk += 1
                if STAGE == 1:
                    continue
                P.op("dve", lambda e: e.reciprocal(out=accd, in_=accd), reads=[("accd", 0), ("accd", 1)], writes=[("accd", 0), ("accd", 1)])
                P.op("dve", lambda e, hp=hp: e.tensor_tensor(out=o_aT[:, hp, :], in0=accn, in1=accd, op=ALU.mult),
                     reads=[("accn", 0), ("accn", 1), ("accd", 0), ("accd", 1)], writes=[("o_aT", hp)])
            if STAGE == 2:
                tmp = scr(8192, 2048)
                for hp in range(4):
                    P.op("dve", lambda e, hp=hp: e.tensor_copy(out=tmp, in_=o_aT[:, hp, :]), reads=[("o_aT", hp)], writes=["dbgtmp"])
                    if "o_aT" in dbg_d:
                        final_ops.append(dma(dbg_d["o_aT"][hp], tmp, reads=["dbgtmp"]))
        P.barrier()

        if STAGE <= 2:
            P.emit(final_ops)
            return nc

        with contextlib.ExitStack() as sbk:
            rc = RopeCtx()
            rc.cos = scr(0, 2048)
            rc.sin = scr(2048, 2048)
            rden = [scr(4096 + i * 512, 512) for i in range(2)]
            rc.u = [scr(8192 + i * 512, 512) for i in range(2)]
            rc.sq = [scr(9216 + i * 512, 512) for i in range(2)]
            rc.rs = [scr(10240 + i * 512, 512) for i in range(2)]
            rc.t1 = [scr(11264 + i * 512, 512) for i in range(2)]
            wst = [scr(12288 + i * 1024, 1024).rearrange("p (k c) -> p k c", k=8) for i in range(2)]
            rc.R = sbt(sbk, "RB", [128, 128], F32)
            rc.R_key = "RBk"
            rc.tab_key = "tabB"
            rc.ps_proj = (0, 1)
            rc.ps_aux = (2, 3)
            wbf = [sbt(sbk, "wbfB%d" % i, [128, 8, 128], BF16) for i in range(3)]
            qbT = sbt(sbk, "qbT", [128, 4, S], BF16)
            kbd = [sbt(sbk, "kbd%d" % g, [128, S], BF16) for g in range(2)]
            vbd = [sbt(sbk, "vbd%d" % g, [128, 16, 128], BF16) for g in range(2)]
            vb1 = [[sbt(sbk, "vb1_%d_%d" % (g, hh), [128, 16, 128], BF16) for hh in range(2)] for g in range(2)]
            pTB = [sbt(sbk, "pTB%d" % i, [128, 512], BF16) for i in range(3)]
            dma(rc.cos, cd["cosB"][:, :], writes=["tabB"])
            dma(rc.sin, cd["sinB"][:, :], writes=["tabB"])
            dma(rc.R[:], cd["RB"][:, :], writes=["RBk"])
            cqb, ckb, cvb = 4608, 5120, 5248
            piecesB = [[(0, cqb + c * 128, 128)] for c in range(4)]
            for g in range(2):
                piecesB.append([(0, ckb + g * 64, 64), (64, ckb + g * 64, 64)])
                piecesB.append([(0, cvb + g * 64, 64), (64, cvb + g * 64, 64)])

            def loadB(j):
                load_w_piece(None, None, wbf[j % 3][:], ("wbfB", j % 3), piecesB[j], None)
            loadB(0)
            loadB(1)
            for c in range(4):
                if c + 2 < len(piecesB):
                    loadB(c + 2)
                qk_chunk(rc, wbf[c % 3], ("wbfB", c % 3), 24 + c, qbT[:, c, :], ("qbT", c), 1)
            for g in range(2):
                j = 4 + 2 * g
                if j + 2 < len(piecesB):
                    loadB(j + 2)
                qk_chunk(rc, wbf[j % 3], ("wbfB", j % 3), 28 + g, kbd[g][:], ("kbd", g), 1)
                if j + 3 < len(piecesB):
                    loadB(j + 3)
                v_tiles(wbf[(j + 1) % 3], ("wbfB", (j + 1) % 3), vbd[g], ("vbd", g), 1, (0, 1))
                for hh in range(2):
                    oh = 1 - hh
                    P.op("pool", lambda e, g=g, hh=hh, oh=oh: e.memset(vb1[g][hh][:, :, 64 * oh:64 * oh + 64], 1.0), writes=[("vb1", g, hh)])
                    P.op("pool", lambda e, g=g, hh=hh: e.tensor_copy(out=vb1[g][hh][:, :, 64 * hh:64 * hh + 64], in_=vbd[g][:, :, 64 * hh:64 * hh + 64]),
                         reads=[("vbd", g), ("vb1", g, hh)], writes=[("vb1", g, hh)])
            zt = scr(12288, 512).bitcast(BF16)
            P.op("pool", lambda e: e.memset(zt, 0.0), writes=["zt"])
            xs_flat = xs_d.rearrange("(p r) d -> p (r d)", p=128)
            ZR = NQ * 2 if (NBLK * BLK // 128) % (NQ * 2) == 0 else NQ
            assert (NBLK * BLK // 128) % ZR == 0
            for i in range(NBLK * BLK // 128 // ZR):
                zero_ops.append(P.op("pool", lambda e, i=i: e.dma_start(out=xs_flat[:, i * ZR * 1024:(i + 1) * ZR * 1024].rearrange("p (a c) -> p a c", a=ZR),
                                                                        in_=zt.unsqueeze(1).broadcast_to([128, ZR, 1024])), reads=["zt"], writes=[("xs0", i)], dma=True))
            tasks = [(h, qc, kt) for h in range(8) for qc in range(4) for kt in range(16)]

            def s_task(ix):
                h, qc, kt = tasks[ix]
                c, hh, g = h // 2, h % 2, h // 4
                hs = slice(64 * hh, 64 * hh + 64)
                qs = slice(qc * 512, (qc + 1) * 512)
                sb_ = 4 + ix % 2
                pb = ix % 3
                P.op("pe", lambda e, sb_=sb_, g=g, hs=hs, kt=kt, c=c, qs=qs: e.matmul(psum[sb_][:], lhsT=kbd[g][hs, kt * 128:(kt + 1) * 128], rhs=qbT[hs, c, qs], start=True, stop=True),
                     reads=[("kbd", g), ("qbT", c)], writes=[PS(sb_)])
                P.op("act", lambda e, sb_=sb_, pb=pb: e.activation(out=pTB[pb][:], in_=psum[sb_][:], func=AF.Exp, scale=0.125),
                     reads=[PS(sb_)], writes=[PS(sb_), ("pTB", pb)])

            def pv_task(ix):
                h, qc, kt = tasks[ix]
                c, hh, g = h // 2, h % 2, h // 4
                hs = slice(64 * hh, 64 * hh + 64)
                os_ = slice(64 * (1 - hh), 64 * (1 - hh) + 64)
                qs = slice(qc * 512, (qc + 1) * 512)
                par = (h * 4 + qc) % 2
                pn = (6, 7)[par]
                pb = ix % 3
                P.op("pe", lambda e, pn=pn, g=g, hh=hh, kt=kt, pb=pb: e.matmul(psum[pn][:], lhsT=vb1[g][hh][:, kt, :], rhs=pTB[pb][:], start=(kt == 0), stop=(kt == 15)),
                     reads=[("vb1", g, hh), ("pTB", pb)], writes=[PS(pn)], accum=(kt > 0))
                if kt == 15:
                    rd = rden[par]
                    P.op("dve", lambda e, rd=rd, hs=hs, os_=os_, pn=pn: e.reciprocal(out=rd[hs, :], in_=psum[pn][os_, :]), reads=[PS(pn)], writes=[PS(pn), ("rden", par)])
                    P.op("dve", lambda e, rd=rd, hs=hs, pn=pn, c=c, qs=qs: e.tensor_tensor(out=o_bT[hs, c, qs], in0=psum[pn][hs, :], in1=rd[hs, :], op=ALU.mult),
                         reads=[PS(pn), ("rden", par)], writes=[PS(pn), ("o_bT", c)])

            s_task(0)
            for ix in range(len(tasks)):
                if ix + 1 < len(tasks):
                    s_task(ix + 1)
                pv_task(ix)
            if STAGE == 3:
                tmp = scr(8192, 2048)
                for c in range(4):
                    P.op("dve", lambda e, c=c: e.tensor_copy(out=tmp, in_=o_bT[:, c, :]), reads=[("o_bT", c)], writes=["dbgtmp"])
                    if "o_bT" in dbg_d:
                        final_ops.append(dma(dbg_d["o_bT"][c], tmp, reads=["dbgtmp"]))
        P.barrier()
        if STAGE == 3:
            P.emit(final_ops)
            return nc

        o_keys_a = [("o_aT", c) for c in range(4)]
        o_keys_b = [("o_bT", c) for c in range(4)]
        with contextlib.ExitStack() as sm:
            bbf2 = [sbt(sm, "bbf%d" % i, [128, 24, 128], BF16) for i in range(2)]
            woutbf = sbt(sm, "woutbf", [128, 8, D], BF16)
            mergedT = sbt(sm, "mergedT", [128, 8, S], BF16)
            sg0 = [sbt(sm, "sg0_%d" % i, [128, 512], F32) for i in range(2)]
            sg1 = [sbt(sm, "sg1_%d" % i, [128, 512], F32) for i in range(2)]
            for tt in range(NT):
                dma(x1[:, tt, :], x_d[tt * 128:(tt + 1) * 128, :], writes=[("x1", tt)])
            w_out_v = w_out_d.rearrange("(kc p) n -> p kc n", p=128)
            for q in range(4):
                P.op("pool", lambda e, q=q: e.dma_start(out=woutbf[:, 2 * q:2 * q + 2, :], in_=w_out_v[:, 2 * q:2 * q + 2, :]), writes=[("woutbf", q)], dma=True)
            it = 0

            def loadM(m_):
                P.op("pool", lambda e: e.dma_start(out=bbf2[m_ % 2][:], in_=mb_d[m_]), writes=[("bbf", m_ % 2)], dma=True)
            loadM(0)
            for m in range(8):
                bbf = bbf2[m % 2]
                kbb = ("bbf", m % 2)
                for tc in range(4):
                    if tc == 1 and m + 1 < 8:
                        loadM(m + 1)
                    ts_ = slice(tc * 512, (tc + 1) * 512)
                    par = it % 2
                    it += 1
                    bA, bB, bG0, bG1 = (0, 1, 2, 3) if par == 0 else (4, 5, 6, 7)
                    for c in range(4):
                        P.op("pe", lambda e, c=c, bA=bA, ts_=ts_, bbf=bbf: e.matmul(psum[bA][:], lhsT=bbf[:, c, :], rhs=o_aT[:, c, ts_], start=(c == 0), stop=(c == 3)),
                             reads=[kbb] + o_keys_a, writes=[PS(bA)], accum=(c > 0))
                    for c in range(4):
                        P.op("pe", lambda e, c=c, bB=bB, ts_=ts_, bbf=bbf: e.matmul(psum[bB][:], lhsT=bbf[:, 4 + c, :], rhs=o_bT[:, c, ts_], start=(c == 0), stop=(c == 3)),
                             reads=[kbb] + o_keys_b, writes=[PS(bB)], accum=(c > 0))
                    for kc in range(8):
                        P.op("pe", lambda e, kc=kc, bG0=bG0, ts_=ts_, bbf=bbf: e.matmul(psum[bG0][:], lhsT=bbf[:, 8 + kc, :], rhs=hT[:, kc, ts_], start=(kc == 0), stop=(kc == 7)),
                             reads=[kbb] + hT_keys, writes=[PS(bG0)], accum=(kc > 0))
                    for kc in range(8):
                        P.op("pe", lambda e, kc=kc, bG1=bG1, ts_=ts_, bbf=bbf: e.matmul(psum[bG1][:], lhsT=bbf[:, 16 + kc, :], rhs=hT[:, kc, ts_], start=(kc == 0), stop=(kc == 7)),
                             reads=[kbb] + hT_keys, writes=[PS(bG1)], accum=(kc > 0))
                    P.op("act", lambda e, m=m, bG0=bG0, par=par: e.activation(out=sg0[par][:], in_=psum[bG0][:], func=AF.Sigmoid, bias=bgate[:, m:m + 1]),
                         reads=[PS(bG0), "bgate"], writes=[PS(bG0), ("sg0", par)])
                    P.op("act", lambda e, m=m, bG1=bG1, par=par: e.activation(out=sg1[par][:], in_=psum[bG1][:], func=AF.Sigmoid, bias=bgate[:, 8 + m:9 + m]),
                         reads=[PS(bG1), "bgate"], writes=[PS(bG1), ("sg1", par)])
                    P.op("dve", lambda e, bA=bA, par=par: e.tensor_tensor(out=sg0[par][:], in0=psum[bA][:], in1=sg0[par][:], op=ALU.mult),
                         reads=[PS(bA), ("sg0", par)], writes=[PS(bA), ("sg0", par)])
                    P.op("dve", lambda e, bB=bB, par=par: e.tensor_tensor(out=sg1[par][:], in0=psum[bB][:], in1=sg1[par][:], op=ALU.mult),
                         reads=[PS(bB), ("sg1", par)], writes=[PS(bB), ("sg1", par)])
                    P.op("pool", lambda e, m=m, par=par, ts_=ts_: e.tensor_tensor(out=mergedT[:, m, ts_], in0=sg0[par][:], in1=sg1[par][:], op=ALU.add),
                         reads=[("sg0", par), ("sg1", par)], writes=[("mergedT", m, tc)])
            for tt in range(NT):
                for nh in range(2):
                    b = (0, 1, 4, 5)[(tt * 2 + nh) % 4]
                    ns = slice(nh * 512, (nh + 1) * 512)
                    for m in range(8):
                        P.op("pe", lambda e, m=m, b=b, tt=tt, ns=ns: e.matmul(psum[b][:], lhsT=mergedT[:, m, tt * 128:(tt + 1) * 128], rhs=woutbf[:, m, ns], start=(m == 0), stop=(m == 7)),
                             reads=[("mergedT", m, tt // 4), ("woutbf", m // 2)], writes=[PS(b)], accum=(m > 0))
                    P.op("dve", lambda e, b=b, tt=tt, ns=ns: e.tensor_tensor(out=x1[:, tt, ns], in0=psum[b][:], in1=x1[:, tt, ns], op=ALU.add),
                         reads=[PS(b), ("x1", tt)], writes=[PS(b), ("x1", tt)])
        x1_keys = [("x1", tt) for tt in range(NT)]
        if STAGE == 4:
            for tt in range(NT):
                if "x1" in dbg_d:
                    final_ops.append(dma(dbg_d["x1"][tt * 128:(tt + 1) * 128, :], x1[:, tt, :], reads=[("x1", tt)]))
            P.emit(final_ops)
            return nc
        P.barrier()

        I32 = mybir.dt.int32
        wgu_rows = wgu_d.rearrange("e k n -> (e k) n")
        wd_rows = wd_d.rearrange("e k n -> (e k) n")
        IOA = bass.IndirectOffsetOnAxis

        def idma(out, out_off, in_, in_off, bound, reads=(), writes=()):
            def f(e):
                if bound is None:
                    return e.indirect_dma_start(out=out, out_offset=out_off, in_=in_, in_offset=in_off)
                return e.indirect_dma_start(out=out, out_offset=out_off, in_=in_, in_offset=in_off, bounds_check=pregs[bound], oob_is_err=False)
            return P.op("pool", f, reads=reads, writes=writes, dma=True)

        pregs = {}

        def mkreg(name, val):
            def f(e):
                pregs[name] = e.alloc_register(name)
                return e.reg_mov(pregs[name], val)
            P.op("pool", f)
        mkreg("bw", NE * D - 1)
        mkreg("bb", NE * 128 - 1)

        with contextlib.ExitStack() as se:
            print("SBUF remaining before moe scope", nc.sbuf_bytes_remaining)
            wgubf = obuf
            wdbf = sbt(se, "wdbf", [128, 8, D], BF16)
            gwk = sbt(se, "gwk", [128, NT, 4], F32)
            desti = sbt(se, "desti", [128, NT * 4], I32)
            be = sbt(se, "be", [128, NBLK], F32)
            widx = sbt(se, "widx", [128, NBLK, 8], I32)
            bidx = sbt(se, "bidx", [128, NBLK], I32)
            h2tok = hT[:].rearrange("p k s -> p (k s)").rearrange("p (t f) -> p t f", t=NT)
            with contextlib.ExitStack() as sr:
                stg = [sbt(sr, "stg%d" % i, [128, 2048], F32) for i in range(2)]
                hn2s = [stg[i][:, 0:1024] for i in range(2)]
                h32s = [stg[i][:, 1024:2048].rearrange("p (k c) -> p k c", k=8) for i in range(2)]
                wr = sbt(sr, "wr", [128, 8, 32], F32)
                wrf = sbt(sr, "wrf", [128, 8, 32], F32)
                brt = sbt(sr, "brt", [128, 32], F32)
                tiny = []
                for nm_, shp_, dt_ in (("logit", [128, 32], F32), ("mx8", [128, 8], F32), ("negmax", [128, 1], F32), ("msk", [128, 32], F32), ("ex", [128, 32], F32),
                                       ("ssum", [128, 1], F32), ("gw", [128, 32], F32), ("gwT", [32, 128], F32), ("mskb", [128, 32], BF16), ("e4", [128, 4], F32), ("s4", [128, 1], F32)):
                    tiny.append([sbt(sr, "%s_%d" % (nm_, i), shp_, dt_) for i in range(2)])
                bd32 = sbt(sr, "bd32", [32, D], F32)
                utri = sbt(sr, "utri", [128, 128], BF16)
                basekc = sbt(sr, "basekc", [128, 8], F32)
                cum = sbt(sr, "cum", [128, 32], F32)
                rank_all = sbt(sr, "rank_all", [128, NT, 32], F32)
                logit_all = sbt(sr, "logit_all", [128, NT, 32], F32)
                mx8_all = sbt(sr, "mx8_all", [128, NT, 8], F32)
                pada = sbt(sr, "pada", [128, 32], F32)
                padb = sbt(sr, "padb", [128, 32], F32)
                padded = sbt(sr, "padded", [128, 32], F32)
                pstart = sbt(sr, "pstart", [128, 32], F32)
                dest_all = sbt(sr, "dest_all", [128, NT, 32], F32)
                junk32 = sbt(sr, "junk32", [128, 32], F32)
                destk = sbt(sr, "destk", [128, NT * 4], F32)
                widf = sbt(sr, "widf", [128, NBLK, 8], F32)
                bidf = sbt(sr, "bidf", [128, NBLK], F32)
                dma(wr[:], w_router_d.rearrange("(kc p) n -> p kc n", p=128), writes=["wr"])
                dma(brt[:], b_router_d[0:1, :].broadcast_to([128, 32]), writes=["brt"])
                dma(bd32[:], bd_d[:, :], writes=["bd32"])
                dma(basekc[:], cd["basekc"][:, :], writes=["basekc"])
                blkthr = sbt(sr, "blkthr", [128, NBLK], F32)
                dma(blkthr[:], cd["blkthr"][:, :], writes=["blkthr"])
                dma(stg[1][:, 1024:1152], cd["utri"][:, :], writes=["h32b"])
                P.op("pool", lambda e: e.tensor_copy(out=utri[:], in_=stg[1][:, 1024:1152]), reads=["h32b"], writes=["utri"])
                P.op("pool", lambda e: e.memset(cum[:], 0.0), writes=["cum"])
                P.op("pool", lambda e: e.tensor_tensor(out=wrf[:], in0=wr[:], in1=gffn[:, 0:8].unsqueeze(2).broadcast_to([128, 8, 32]), op=ALU.mult),
                     reads=["wr", "gffn"], writes=["wrf"])
                def route_tile(tt):
                    par = tt % 2
                    hn2, junk2, h32 = hn2s[par], h32s[par].rearrange("p k c -> p (k c)"), h32s[par]
                    khn, kh32 = ("hn2a", "hn2b")[par], ("h32a", "h32b")[par]
                    logit, mx8, negmax, msk, ex, ssum, gw, gwT, mskb, e4, s4 = [t_[par] for t_ in tiny]
                    kk = lambda n: (n, par)
                    pl, pg = (5, 4) if par == 0 else (1, 0)
                    norm_transpose(tt, x1[:, tt, :], ("x1", tt), hn2, khn, junk2, kh32, None, None, (6, 7), extra32=(h32, kh32))
                    P.op("act", lambda e, tt=tt: e.copy(out=h2tok[:, tt, :], in_=hn2), reads=[khn], writes=[("h2tok", tt)])
                    for kc in range(8):
                        P.op("pe", lambda e, kc=kc: e.matmul(psum[pl][:, 0:32], lhsT=h32[:, kc, :], rhs=wrf[:, kc, :], start=(kc == 0), stop=(kc == 7)),
                             reads=[kh32, "wrf"], writes=[PS(pl)], accum=(kc > 0))
                    P.op("dve", lambda e: e.tensor_tensor(out=logit[:], in0=psum[pl][:, 0:32], in1=brt[:], op=ALU.add), reads=[PS(pl), "brt"], writes=[PS(pl), kk("logit")])
                    P.op("dve", lambda e: e.max(out=mx8[:], in_=logit[:]), reads=[kk("logit")], writes=[kk("mx8")])
                    P.op("dve", lambda e: e.tensor_scalar(out=msk[:], in0=logit[:], scalar1=mx8[:, 3:4], scalar2=None, op0=ALU.is_ge), reads=[kk("logit"), kk("mx8")], writes=[kk("msk")])
                    P.op("dve", lambda e: e.tensor_scalar(out=negmax[:], in0=mx8[:, 0:1], scalar1=-1.0, scalar2=None, op0=ALU.mult), reads=[kk("mx8")], writes=[kk("negmax")])
                    P.op("act", lambda e: e.activation(out=ex[:], in_=logit[:], func=AF.Exp, bias=negmax[:, 0:1], scale=1.0), reads=[kk("logit"), kk("negmax")], writes=[kk("ex")])
                    P.op("dve", lambda e: e.tensor_tensor(out=ex[:], in0=ex[:], in1=msk[:], op=ALU.mult), reads=[kk("ex"), kk("msk")], writes=[kk("ex")])
                    P.op("dve", lambda e: e.reduce_sum(out=ssum[:], in_=ex[:], axis=AX.X), reads=[kk("ex")], writes=[kk("ssum")])
                    P.op("dve", lambda e: e.reciprocal(out=ssum[:], in_=ssum[:]), reads=[kk("ssum")], writes=[kk("ssum")])
                    P.op("dve", lambda e: e.tensor_scalar(out=gw[:], in0=ex[:], scalar1=ssum[:, 0:1], scalar2=None, op0=ALU.mult), reads=[kk("ex"), kk("ssum")], writes=[kk("gw")])
                    P.op("pool", lambda e, tt=tt: e.tensor_copy(out=logit_all[:, tt, :], in_=logit[:]), reads=[kk("logit")], writes=[("logit_all", tt)])
                    P.op("pool", lambda e, tt=tt: e.tensor_copy(out=mx8_all[:, tt, :], in_=mx8[:]), reads=[kk("mx8")], writes=[("mx8_all", tt)])
                    P.op("pool", lambda e: e.tensor_copy(out=mskb[:], in_=msk[:]), reads=[kk("msk")], writes=[kk("mskb")])
                    P.op("act", lambda e: e.activation(out=e4[:], in_=mx8[:, 0:4], func=AF.Exp, bias=negmax[:, 0:1], scale=1.0), reads=[kk("mx8"), kk("negmax")], writes=[kk("e4")])
                    P.op("dve", lambda e: e.reduce_sum(out=s4[:], in_=e4[:], axis=AX.X), reads=[kk("e4")], writes=[kk("s4")])
                    P.op("dve", lambda e: e.reciprocal(out=s4[:], in_=s4[:]), reads=[kk("s4")], writes=[kk("s4")])
                    P.op("dve", lambda e, tt=tt: e.tensor_scalar(out=gwk[:, tt, :], in0=e4[:], scalar1=s4[:, 0:1], scalar2=None, op0=ALU.mult), reads=[kk("e4"), kk("s4")], writes=[("gwk", tt)])
                    P.op("pe", lambda e: e.matmul(psum[pl][:, 32:64], lhsT=utri[:], rhs=mskb[:], start=True, stop=True), reads=["utri", kk("mskb")], writes=[PS(pl)])
                    P.op("pe", lambda e: e.matmul(psum[pl][:, 64:96], lhsT=ones_bf[:], rhs=mskb[:], start=True, stop=True), reads=["ones_bf", kk("mskb")], writes=[PS(pl)], accum=True)
                    P.op("dve", lambda e, tt=tt: e.tensor_tensor(out=rank_all[:, tt, :], in0=psum[pl][:, 32:64], in1=cum[:], op=ALU.add), reads=[PS(pl), "cum"], writes=[PS(pl), ("rank_all", tt)])
                    P.op("dve", lambda e: e.tensor_tensor(out=cum[:], in0=psum[pl][:, 64:96], in1=cum[:], op=ALU.add), reads=[PS(pl), "cum"], writes=[PS(pl), "cum"])
                    P.op("pe", lambda e: e.transpose(out=psum[pg][0:32, 0:128], in_=gw[:], identity=ident[:]), reads=[kk("gw"), "ident"], writes=[PS(pg)])
                    P.op("act", lambda e: e.copy(out=gwT[:], in_=psum[pg][0:32, 0:128]), reads=[PS(pg)], writes=[PS(pg), kk("gwT")])
                    for nh in range(2):
                        ns = slice(nh * 512, (nh + 1) * 512)
                        b = 2 + nh
                        P.op("pe", lambda e, b=b, ns=ns: e.matmul(psum[b][:], lhsT=gwT[:], rhs=bd32[:, ns], start=True, stop=True), reads=[kk("gwT"), "bd32"], writes=[PS(b)])
                        P.op("dve", lambda e, b=b, tt=tt, ns=ns: e.tensor_tensor(out=x1[:, tt, ns], in0=psum[b][:], in1=x1[:, tt, ns], op=ALU.add),
                             reads=[PS(b), ("x1", tt)], writes=[PS(b), ("x1", tt)])

                for tt in range(NT):
                    route_tile(tt)
                rk_keys = [("rank_all", tt) for tt in range(NT)]
                P.op("pool", lambda e: e.memset(padb[:], 0.0), writes=["padb"])
                for j in range(-(-S // BLK)):
                    P.op("dve", lambda e, j=j: e.scalar_tensor_tensor(out=padb[:], in0=cum[:], scalar=float(BLK * j), in1=padb[:], op0=ALU.is_gt, op1=ALU.add),
                         reads=["cum", "padb"], writes=["padb"])
                P.op("dve", lambda e: e.tensor_scalar(out=padded[:], in0=padb[:], scalar1=float(BLK), scalar2=None, op0=ALU.mult), reads=["padb"], writes=["padded"])
                P.op("dve", lambda e: e.tensor_copy(out=pada[:], in_=padded[:]), reads=["padded", "padb"], writes=["pada"])
                src_, dst_ = pada, padb
                for st_ in (1, 2, 4, 8, 16):
                    P.op("dve", lambda e, src_=src_, dst_=dst_, st_=st_: e.tensor_copy(out=dst_[:, 0:st_], in_=src_[:, 0:st_]), reads=["pada", "padb"], writes=["pada", "padb"])
                    P.op("dve", lambda e, src_=src_, dst_=dst_, st_=st_: e.tensor_tensor(out=dst_[:, st_:32], in0=src_[:, st_:32], in1=src_[:, 0:32 - st_], op=ALU.add),
                         reads=["pada", "padb"], writes=["pada", "padb"])
                    src_, dst_ = dst_, src_
                pend = src_
                P.op("dve", lambda e: e.tensor_tensor(out=pstart[:], in0=pend[:], in1=padded[:], op=ALU.subtract), reads=["pada", "padb", "padded"], writes=["pstart"])
                P.op("dve", lambda e: e.tensor_tensor(out=dest_all[:], in0=rank_all[:], in1=pstart[:].unsqueeze(1).broadcast_to([128, NT, 32]), op=ALU.add),
                     reads=rk_keys + ["pstart"], writes=["dest_all"])
                big = stg[0][:].rearrange("p (t k e) -> p t k e", t=NT, k=4)
                la_keys = [("logit_all", tt) for tt in range(NT)] + [("mx8_all", tt) for tt in range(NT)]
                P.op("dve", lambda e: e.tensor_tensor(out=big, in0=logit_all[:].unsqueeze(2).broadcast_to([128, NT, 4, 32]),
                                                      in1=mx8_all[:, :, 0:4].unsqueeze(3).broadcast_to([128, NT, 4, 32]), op=ALU.is_equal),
                     reads=la_keys + ["hn2a", "h32a"], writes=["big"])
                P.op("dve", lambda e: e.tensor_tensor(out=big, in0=big, in1=dest_all[:].unsqueeze(2).broadcast_to([128, NT, 4, 32]), op=ALU.mult),
                     reads=["big", "dest_all"], writes=["big"])
                P.op("dve", lambda e: e.reduce_sum(out=destk[:], in_=stg[0][:].rearrange("p (c e) -> p c e", e=32), axis=AX.X), reads=["big"], writes=["destk"])
                P.op("dve", lambda e: e.tensor_copy(out=desti[:], in_=destk[:]), reads=["destk"], writes=["desti"])
                big2 = stg[1][:, 0:NBLK * 32].rearrange("p (b e) -> p b e", e=32)
                P.op("dve", lambda e: e.tensor_tensor(out=big2, in0=pend[:].unsqueeze(1).broadcast_to([128, NBLK, 32]),
                                                      in1=blkthr[:].unsqueeze(2).broadcast_to([128, NBLK, 32]), op=ALU.is_le),
                     reads=["pada", "padb", "blkthr", "h32a", "h32b", "hn2b"], writes=["big2"])
                P.op("dve", lambda e: e.reduce_sum(out=be[:], in_=big2, axis=AX.X), reads=["big2"], writes=["be"])
                P.op("dve", lambda e: e.tensor_scalar(out=bidf[:], in0=be[:], scalar1=1024.0, scalar2=None, op0=ALU.mult), reads=["be"], writes=["bidf"])
                P.op("dve", lambda e: e.tensor_tensor(out=widf[:], in0=bidf[:].unsqueeze(2).broadcast_to([128, NBLK, 8]), in1=basekc[:].unsqueeze(1).broadcast_to([128, NBLK, 8]), op=ALU.add),
                     reads=["bidf", "basekc"], writes=["widf"])
                P.op("dve", lambda e: e.tensor_copy(out=widx[:], in_=widf[:]), reads=["widf"], writes=["widx"])
                P.op("dve", lambda e: e.tensor_scalar(out=bidf[:], in0=be[:], scalar1=128.0, scalar2=basekc[:, 0:1], op0=ALU.mult, op1=ALU.add), reads=["be", "basekc", "widf"], writes=["bidf"])
                P.op("dve", lambda e: e.tensor_copy(out=bidx[:], in_=bidf[:]), reads=["bidf"], writes=["bidx"])
                if STAGE == 5:
                    gwk_keys = [("gwk", tt) for tt in range(NT)]
                    final_ops.append(dma(dbg_d["destk"][:, :], destk[:], reads=["destk"]))
                    final_ops.append(dma(dbg_d["be"][:, :], be[:], reads=["be"]))
                    final_ops.append(dma(dbg_d["gwk"][:, :], gwk[:].rearrange("p a b -> p (a b)"), reads=gwk_keys))
                    final_ops.append(dma(dbg_d["widf"][:, :], widf[:].rearrange("p a b -> p (a b)"), reads=["widf"]))
            if STAGE == 5:
                P.emit(final_ops)
                return nc
            P.barrier()
            nrow = NBLK * BLK
            xs0_keys = [("xs0", i) for i in range(len(zero_ops))]
            sc_ops = []
            for tt in range(NT):
                for k in range(4):
                    c_ = tt * 4 + k
                    sc_ops.append(idma(xs_d[:, :], IOA(ap=desti[:, c_:c_ + 1], axis=0), h2tok[:, tt, :], None, None,
                                       reads=[("h2tok", tt), "desti"] + (xs0_keys if c_ == 0 else []), writes=[("xs", c_)]))
            xs_keys = [("xs", c_) for c_ in range(NT * 4)]
            P.barrier()
            with contextlib.ExitStack() as sb_:
                print("SBUF remaining before block scope", nc.sbuf_bytes_remaining)
                wgu2 = [obuf, hT]
                identb = sbt(sb_, "identb", [128, 128], BF16)
                xtok = sbt(sb_, "xtok", [128, NQ, D], BF16)
                xT = [sbt(sb_, "xT%d" % i, [128, 8, BLK], BF16) for i in range(2)]
                actT = [sbt(sb_, "actT%d" % i, [128, 8, BLK], BF16) for i in range(2)]
                gtb = [sbt(sb_, "gtb%d" % i, [128, BLK], F32) for i in range(2)]
                sgb = [sbt(sb_, "sgb%d" % i, [128, BLK], F32) for i in range(2)]
                u1b = [sbt(sb_, "u1b%d" % i, [128, BLK], F32) for i in range(2)]
                ysb = [sbt(sb_, "ysb%d" % i, [128, 512], F32) for i in range(3)]
                bblk = [sbt(sb_, "bblk%d" % i, [128, 16], F32) for i in range(2)]
                P.op("dve", lambda e: e.tensor_copy(out=identb[:], in_=ident[:]), reads=["ident"], writes=["identb"])
                for i in range(2):
                    P.op("dve", lambda e, i=i: e.memset(bblk[i][:], 0.0), writes=[("bblk", i)])
                itc = [0]
                ycnt = [0]

                order = []
                nt_ = NBLK - 32
                acc_ = 0
                tail_ = 32
                for i in range(32):
                    order.append(i)
                    acc_ += nt_
                    if acc_ >= 32:
                        acc_ -= 32
                        order.append(tail_)
                        tail_ += 1
                order += list(range(tail_, NBLK))
                assert sorted(order) == list(range(NBLK))

                def load_wgu(b):
                    blk_ = order[b]
                    wb_ = wgu2[b % 2]
                    for kc in range(8):
                        idma(wb_[:, kc, :], None, wgu_rows[:, :], IOA(ap=widx[:, blk_, kc:kc + 1], axis=0), "bw", reads=["widx"], writes=[("wgu", b % 2, kc)])
                    idma(bblk[b % 2][:], None, bgu_d[:, :], IOA(ap=bidx[:, blk_:blk_ + 1], axis=0), "bb", reads=["bidx"], writes=[("bblk", b % 2)])
                    P.op("dve", lambda e, b=b: e.tensor_scalar(out=bblk[b % 2][:, 8:16], in0=bblk[b % 2][:, 8:16], scalar1=1.0, scalar2=None, op0=ALU.add),
                         reads=[("bblk", b % 2)], writes=[("bblk", b % 2)])

                def load_wd(b):
                    blk_ = order[b]
                    for kc in range(8):
                        idma(wdbf[:, kc, :], None, wd_rows[:, :], IOA(ap=widx[:, blk_, kc:kc + 1], axis=0), "bw", reads=["widx"], writes=[("wd", kc)])

                def load_x_dma(b):
                    blk_ = order[b]
                    for q in range(NQ):
                        dma(xtok[:, q, :], xs_d[blk_ * BLK + q * 128: blk_ * BLK + (q + 1) * 128, :], reads=xs_keys if q == 0 else [], writes=[("xtok", q)])

                def load_x(b):
                    xt_ = xT[b % 2]
                    for kc in range(8):
                        bk = 6 + kc % 2
                        pv = psum[bk][:].bitcast(BF16)
                        for q in range(NQ):
                            P.op("pe", lambda e, kc=kc, q=q, pv=pv: e.transpose(out=pv[:, q * 128:(q + 1) * 128], in_=xtok[:, q, kc * 128:(kc + 1) * 128], identity=identb[:]),
                                 reads=[("xtok", q), "identb"], writes=[PS(bk)], accum=(q > 0))
                        P.op("act", lambda e, kc=kc, pv=pv, xt_=xt_: e.activation(out=xt_[:, kc, :], in_=pv[:, 0:BLK], func=AF.Copy, scale=gffn[:, kc:kc + 1]),
                             reads=[PS(bk), "gffn"], writes=[PS(bk), ("xT", b % 2, kc)])

                pend_fin = []

                def gu_phase(b):
                    wb_ = wgu2[b % 2]
                    xt_ = xT[b % 2]
                    ap_ = b % 2
                    for m in range(8):
                        bpar = itc[0] % 2
                        par = itc[0] % 2
                        itc[0] += 1
                        bg_, bu_ = (0, 1) if bpar == 0 else (2, 3)
                        for kc in range(8):
                            P.op("pe", lambda e, kc=kc, m=m, bg_=bg_: e.matmul(psum[bg_][:, 0:BLK], lhsT=wb_[:, kc, m * 128:(m + 1) * 128], rhs=xt_[:, kc, :], start=(kc == 0), stop=(kc == 7)),
                                 reads=[("wgu", b % 2, kc), ("xT", b % 2, kc)], writes=[PS(bg_)], accum=(kc > 0))
                        for kc in range(8):
                            P.op("pe", lambda e, kc=kc, m=m, bu_=bu_: e.matmul(psum[bu_][:, 0:BLK], lhsT=wb_[:, kc, 1024 + m * 128:1024 + (m + 1) * 128], rhs=xt_[:, kc, :], start=(kc == 0), stop=(kc == 7)),
                                 reads=[("wgu", b % 2, kc), ("xT", b % 2, kc)], writes=[PS(bu_)], accum=(kc > 0))
                        gt, sg, u1 = gtb[bpar], sgb[par], u1b[bpar]
                        bb_ = bblk[b % 2]
                        P.op("dve", lambda e, gt=gt, bg_=bg_, m=m, bb_=bb_: e.tensor_scalar(out=gt[:], in0=psum[bg_][:, 0:BLK], scalar1=bb_[:, m:m + 1], scalar2=7.0, op0=ALU.add, op1=ALU.min),
                             reads=[PS(bg_), ("bblk", b % 2)], writes=[PS(bg_), ("gt", bpar)])
                        P.op("act", lambda e, gt=gt, sg=sg: e.activation(out=sg[:], in_=gt[:], func=AF.Sigmoid, scale=1.702), reads=[("gt", bpar)], writes=[("sg", par)])
                        P.op("dve", lambda e, u1=u1, bu_=bu_, m=m, bb_=bb_: e.tensor_scalar(out=u1[:], in0=psum[bu_][:, 0:BLK], scalar1=bb_[:, 8 + m:9 + m], scalar2=8.0, op0=ALU.add, op1=ALU.min),
                             reads=[PS(bu_), ("bblk", b % 2)], writes=[PS(bu_), ("u1", bpar)])
                        P.op("dve", lambda e, gt=gt, u1=u1: e.scalar_tensor_tensor(out=u1[:], in0=u1[:], scalar=-6.0, in1=gt[:], op0=ALU.max, op1=ALU.mult),
                             reads=[("gt", bpar), ("u1", bpar)], writes=[("u1", bpar)])

                        def fin(sg=sg, u1=u1, ap_=ap_, m=m, par=par, bpar=bpar):
                            P.op("dve", lambda e: e.tensor_tensor(out=actT[ap_][:, m, :], in0=u1[:], in1=sg[:], op=ALU.mult),
                                 reads=[("sg", par), ("u1", bpar)], writes=[("actT", ap_, m)])
                        if pend_fin:
                            pend_fin.pop()()
                        pend_fin.append(fin)
                    if pend_fin:
                        pend_fin.pop()()

                def down_phase(b):
                    ap_ = b % 2
                    blk_ = order[b]
                    for q in range(NQ):
                        for nh in range(2):
                            yi = ycnt[0] % 3
                            ycnt[0] += 1
                            yb = ysb[yi]
                            yk = ("ysb", yi)
                            bk = 4 + nh
                            ns = slice(nh * 512, (nh + 1) * 512)
                            for m in range(8):
                                P.op("pe", lambda e, m=m, bk=bk, q=q, ns=ns: e.matmul(psum[bk][:], lhsT=actT[ap_][:, m, q * 128:(q + 1) * 128], rhs=wdbf[:, m, ns], start=(m == 0), stop=(m == 7)),
                                     reads=[("actT", ap_, m), ("wd", m)], writes=[PS(bk)], accum=(m > 0))
                            if nh == 0:
                                P.op("act", lambda e, bk=bk, yb=yb: e.copy(out=yb[:], in_=psum[bk][:]), reads=[PS(bk)], writes=[PS(bk), yk])
                            else:
                                P.op("dve", lambda e, bk=bk, yb=yb: e.tensor_copy(out=yb[:], in_=psum[bk][:]), reads=[PS(bk)], writes=[PS(bk), yk])
                            dma(y_d[blk_ * BLK + q * 128: blk_ * BLK + (q + 1) * 128, ns], yb[:], reads=[yk], writes=[("y", b, q, nh)])

                load_wgu(0)
                load_wgu(1)
                load_wd(0)
                load_x_dma(0)
                load_x(0)
                load_x_dma(1)
                gu_phase(0)
                for b in range(NBLK):
                    if b + 1 < NBLK:
                        load_x(b + 1)
                        if b + 2 < NBLK:
                            load_x_dma(b + 2)
                        gu_phase(b + 1)
                    if b + 2 < NBLK:
                        load_wgu(b + 2)
                    down_phase(b)
                    if b + 1 < NBLK:
                        load_wd(b + 1)
            P.barrier()
            with contextlib.ExitStack() as sc:
                yg = [sbt(sc, "yg%d" % i, [128, D], F32) for i in range(4)]
                for tt in range(NT):
                    for k in range(4):
                        c_ = tt * 4 + k
                        gi = c_ % 4
                        idma(yg[gi][:], None, y_d[:, :], IOA(ap=desti[:, c_:c_ + 1], axis=0), None, reads=["desti"], writes=[("yg", gi)])
                        P.op("dve", lambda e, tt=tt, k=k, gi=gi: e.scalar_tensor_tensor(out=x1[:, tt, :], in0=yg[gi][:], scalar=gwk[:, tt, k:k + 1], in1=x1[:, tt, :], op0=ALU.mult, op1=ALU.add),
                             reads=[("yg", gi), ("gwk", tt), ("x1", tt)], writes=[("x1", tt)])
                    final_ops.append(dma(out_d[tt * 128:(tt + 1) * 128, :], x1[:, tt, :], reads=[("x1", tt)]))
        P.emit(final_ops)
    return nc


def _in_maps(inputs):
    inp = {k: np.asarray(v) for k, v in inputs.items()}
    r = _relayout(inp)
    c = _consts()
    base = dict(r)
    if STAGE <= 4:
        base.pop("w_gate_up")
        base.pop("w_down")
    for k, v in c.items():
        base["c_" + k] = v
    maps = []
    for b in range(inp["x"].shape[0]):
        m = dict(base)
        m["x"] = np.ascontiguousarray(inp["x"][b])
        maps.append(m)
    return maps


def kernel(**inputs):
    maps = _in_maps(inputs)
    nc = build()
    res = run_bass_kernel_spmd(nc, maps, core_ids=list(range(len(maps))))
    return np.stack([r["out"] for r in res.results], axis=0).astype(np.float32)
```
